# Optimizing a Trainium2 kernel written in Bass

```python
import jax, jax.numpy as jnp
from jax import lax
import numpy as np

D_MODEL = 2048
BATCH = 4
SEQ = 2048
DEPTH = 1

MIX_W = D_MODEL
POOL_W = MIX_W // 2
POOL_WINDOWS = (2, 4, 8, 16)
POOL_GROUPS = len(POOL_WINDOWS)
POOL_CH = POOL_W // POOL_GROUPS
ATTN_W = MIX_W - POOL_W
HEAD_DIM = 128
N_HEADS = ATTN_W // HEAD_DIM
KV_HEADS = 2
Q_PER_KV = N_HEADS // KV_HEADS
KV_W = KV_HEADS * HEAD_DIM
IDX_HEADS = 16
IDX_DIM = 128
INDEX_TOPK = 256
Q_BLOCK = 128
D_FF = 4 * D_MODEL
RMS_EPS = 1e-6
IN_SIZES = (POOL_W, ATTN_W, KV_W, KV_W, IDX_HEADS * IDX_DIM, IDX_DIM, IDX_HEADS)
IN_W = sum(IN_SIZES)
IN_SPLITS = tuple(int(v) for v in np.cumsum(IN_SIZES)[:-1])

kernel_name = "hymba_pool_dsa_hybrid_layer"


def rms_norm(x, g):
    xf = x.astype(jnp.float32)
    y = xf * lax.rsqrt(jnp.mean(xf * xf, axis=-1, keepdims=True) + RMS_EPS)
    return (y * g.astype(jnp.float32)).astype(x.dtype)


def modulate(h, shift, scale):
    return h * (1 + scale[:, None, :]) + shift[:, None, :]


def multiscale_pool_mixer(u, w_pool, pool_scale):
    B, S, _ = u.shape
    ug = u.reshape(B, S, POOL_GROUPS, POOL_CH)
    cs = jnp.cumsum(ug.astype(jnp.float32), axis=1)
    cs = jnp.pad(cs, ((0, 0), (1, 0), (0, 0), (0, 0)))
    t = jnp.arange(S)
    outs = []
    for gi, win in enumerate(POOL_WINDOWS):
        lo = jnp.maximum(t + 1 - win, 0)
        total = cs[:, 1:, gi] - cs[:, lo, gi]
        count = jnp.minimum(t + 1, win).astype(jnp.float32)
        outs.append(total / count[None, :, None] - ug[:, :, gi].astype(jnp.float32))
    pooled = jnp.stack(outs, axis=2).astype(u.dtype)
    mixed = jnp.einsum('bsgc,gcd->bsgd', pooled, w_pool)
    return mixed.reshape(B, S, POOL_W) * pool_scale


def dsa_sparse_attention(q, k, v, q_idx, k_idx, w_idx):
    B, S = q.shape[0], q.shape[1]
    topk = min(INDEX_TOPK, S // 4)
    nb = S // Q_BLOCK
    idx_scale = (IDX_HEADS ** -0.5) * (IDX_DIM ** -0.5)
    key_pos = jnp.arange(S)
    gather = jax.vmap(lambda a, i: a[i])

    def to_blocks(a):
        return a.reshape((B, nb, Q_BLOCK) + a.shape[2:]).swapaxes(0, 1)

    def one_block(args):
        qb, qib, wb, t0 = args
        tpos = t0 + jnp.arange(Q_BLOCK)
        rel = jax.nn.relu(jnp.einsum('bthd,bsd->bths', qib, k_idx).astype(jnp.float32))
        score = jnp.einsum('bth,bths->bts', wb.astype(jnp.float32), rel) * idx_scale
        causal = key_pos[None, :] <= tpos[:, None]
        score = jnp.where(causal[None], score, -jnp.inf)
        _, sel = lax.top_k(score, topk)
        k_sel = gather(k, sel)
        v_sel = gather(v, sel)
        qg = qb.reshape(B, Q_BLOCK, KV_HEADS, Q_PER_KV, HEAD_DIM)
        logits = jnp.einsum('btgrd,btkgd->btgrk', qg, k_sel).astype(jnp.float32) * (HEAD_DIM ** -0.5)
        valid = (sel <= tpos[None, :, None])[:, :, None, None, :]
        logits = jnp.where(valid, logits, -jnp.inf)
        p = jax.nn.softmax(logits, axis=-1).astype(v.dtype)
        o = jnp.einsum('btgrk,btkgd->btgrd', p, v_sel)
        return o.reshape(B, Q_BLOCK, ATTN_W)

    starts = jnp.arange(nb) * Q_BLOCK
    out = lax.map(one_block, (to_blocks(q), to_blocks(q_idx), to_blocks(w_idx), starts))
    return out.swapaxes(0, 1).reshape(B, S, ATTN_W)


def setup_inputs(seed: int = 0) -> dict:
    key = jax.random.key(seed)
    ks = jax.random.split(key, 20)
    f32 = jnp.float32
    D = D_MODEL

    def nrm(k, shape, scale):
        return jax.random.normal(k, shape, f32) * scale

    def gain(k, shape):
        return 1.0 + 0.02 * jax.random.normal(k, shape, f32)

    return {
        "x": jax.random.normal(ks[0], (BATCH, SEQ, D), f32),
        "c": jax.random.normal(ks[1], (BATCH, D), f32),
        "w_ada": nrm(ks[2], (DEPTH, D, 6 * D), D ** -0.5),
        "b_ada": nrm(ks[3], (DEPTH, 6 * D), 0.01),
        "g_pre_mix": gain(ks[4], (DEPTH, D)),
        "g_post_mix": gain(ks[5], (DEPTH, D)),
        "g_pre_ffn": gain(ks[6], (DEPTH, D)),
        "g_post_ffn": gain(ks[7], (DEPTH, D)),
        "w_in": nrm(ks[8], (DEPTH, D, IN_W), D ** -0.5),
        "w_pool": nrm(ks[9], (DEPTH, POOL_GROUPS, POOL_CH, POOL_CH), POOL_CH ** -0.5),
        "pool_scale": gain(ks[10], (DEPTH, POOL_W)),
        "g_pool_out": gain(ks[11], (DEPTH, POOL_W)),
        "g_attn_out": gain(ks[12], (DEPTH, ATTN_W)),
        "w_out": nrm(ks[13], (DEPTH, MIX_W, D), MIX_W ** -0.5),
        "w_ff1": nrm(ks[14], (DEPTH, D, D_FF), D ** -0.5),
        "w_ff2": nrm(ks[15], (DEPTH, D_FF, D), D_FF ** -0.5),
    }


def reference(x, c, w_ada, b_ada, g_pre_mix, g_post_mix, g_pre_ffn, g_post_ffn,
              w_in, w_pool, pool_scale, g_pool_out, g_attn_out, w_out, w_ff1, w_ff2):
    B, S, D = x.shape
    c_act = jax.nn.silu(c)
    for l in range(DEPTH):
        mod = jnp.einsum('bd,de->be', c_act, w_ada[l]) + b_ada[l]
        shift1, scale1, gate1, shift2, scale2, gate2 = jnp.split(mod, 6, axis=-1)

        h = modulate(rms_norm(x, g_pre_mix[l]), shift1, scale1)
        z = jnp.einsum('bsd,de->bse', h, w_in[l])
        z_pool, z_q, z_k, z_v, z_iq, z_ik, z_iw = jnp.split(z, IN_SPLITS, axis=-1)
        pool_out = multiscale_pool_mixer(z_pool, w_pool[l], pool_scale[l])
        attn_out = dsa_sparse_attention(
            z_q.reshape(B, S, N_HEADS, HEAD_DIM),
            z_k.reshape(B, S, KV_HEADS, HEAD_DIM),
            z_v.reshape(B, S, KV_HEADS, HEAD_DIM),
            z_iq.reshape(B, S, IDX_HEADS, IDX_DIM),
            z_ik, z_iw)
        merged = jnp.concatenate(
            [rms_norm(pool_out, g_pool_out[l]), rms_norm(attn_out, g_attn_out[l])], axis=-1)
        mix = jnp.einsum('bse,ed->bsd', merged, w_out[l])
        x = x + gate1[:, None, :] * rms_norm(mix, g_post_mix[l])

        h2 = modulate(rms_norm(x, g_pre_ffn[l]), shift2, scale2)
        f = jnp.square(jax.nn.relu(jnp.einsum('bsd,df->bsf', h2, w_ff1[l])))
        f = jnp.einsum('bsf,fd->bsd', f, w_ff2[l])
        x = x + gate2[:, None, :] * rms_norm(f, g_post_ffn[l])
    return x
```

```python
import bisect
from contextlib import ExitStack

import numpy as np
import concourse.bass as bass
import concourse.mybir as mybir
from concourse.bass_utils import run_bass_kernel_spmd

F32 = mybir.dt.float32
BF16 = mybir.dt.bfloat16
ALU = mybir.AluOpType
AF = mybir.ActivationFunctionType
AX = mybir.AxisListType

D = 2048
S = 2048
DFF = 8192
IN_W = 4752
NCORES = 8
NIT = 24
TOPK = 256.0
EPS = 1e-6
MASKV = -30000.0
BIG = 1e30
IDX_SCALE = (16 ** -0.5) * (128 ** -0.5)
ATT_SCALE = 128 ** -0.5
NDSEM = 24
NRING = 2


class Prog:
    ENG = ("pe", "act", "dve", "pool", "sp")

    def __init__(self, nc):
        self.nc = nc
        self.ops = {e: [] for e in self.ENG}
        self.sig_seq = {e: [] for e in self.ENG}
        self.nseq = {e: 0 for e in self.ENG}
        self.wr = {}
        self.rd = {}
        self.dslot_next = 0
        self.dslot_val = [0] * NDSEM
        self.floor = []

    def _deps(self, eng, reads, writes):
        deps = list(self.floor)
        for k in reads:
            deps.extend(self.wr.get(k, {}).values())
        for k in writes:
            deps.extend(self.wr.get(k, {}).values())
            deps.extend(self.rd.get(k, {}).values())
        return [t for t in deps if not (t[0] == "c" and t[1] == eng and eng == "pe")]

    def _record(self, tok, who, reads, writes):
        for k in reads:
            self.rd.setdefault(k, {})[who] = tok
        for k in writes:
            self.wr.setdefault(k, {})[who] = tok

    def op(self, eng, fn, reads=(), writes=(), signal=True):
        deps = self._deps(eng, reads, writes)
        seq = self.nseq[eng]
        self.nseq[eng] += 1
        if signal:
            self.sig_seq[eng].append(seq)
        tok = ("c", eng, seq)
        self.ops[eng].append(dict(kind="c", fn=fn, deps=deps, seq=seq, signal=signal))
        self._record(tok, eng, reads, writes)
        return tok

    def dma(self, eng, out, in_, reads=(), writes=(), **kw):
        deps = self._deps(eng, reads, writes)
        slot = self.dslot_next
        self.dslot_next = (slot + 1) % NDSEM
        prev = self.dslot_val[slot]
        self.dslot_val[slot] = prev + 16
        tok = ("d", slot, prev + 16)
        if prev:
            deps.append(("d", slot, prev))
        self.ops[eng].append(dict(kind="d", out=out, in_=in_, deps=deps, slot=slot, kw=kw,
                                  seq=self.nseq[eng]))
        self._record(tok, "dma%d" % slot, reads, writes)
        return tok

    def barrier(self):
        fl = []
        for e in ("pe", "act", "dve", "pool"):
            if self.nseq[e]:
                assert self.sig_seq[e] and self.sig_seq[e][-1] == self.nseq[e] - 1, e
                fl.append(("c", e, self.nseq[e] - 1))
        for s in range(NDSEM):
            if self.dslot_val[s]:
                fl.append(("d", s, self.dslot_val[s]))
        self.floor = fl

    def emit(self, final_tokens=()):
        nc = self.nc
        with ExitStack() as es:
            csem = {e: es.enter_context(nc.semaphore("s_" + e)) for e in self.ENG if e != "sp"}
            dsem = [es.enter_context(nc.semaphore("d%d" % i)) for i in range(NDSEM)]
            block = es.enter_context(nc.Block())
            sig_seq = self.sig_seq

            def resolve(tok):
                if tok[0] == "d":
                    return dsem[tok[1]], tok[2]
                _, e, seq = tok
                i = bisect.bisect_left(sig_seq[e], seq)
                assert i < len(sig_seq[e]), ("no signalling op after", tok)
                return csem[e], i + 1

            def run(ename):
                def body(engine):
                    waited = {}
                    for o in self.ops[ename]:
                        for t in o["deps"]:
                            if t[0] == "c" and t[1] == ename:
                                i = bisect.bisect_left(sig_seq[ename], t[2])
                                assert i < len(sig_seq[ename]) and sig_seq[ename][i] < o["seq"], \
                                    ("same-engine dep on unsignalled op", ename, t)
                            sem, val = resolve(t)
                            if waited.get(id(sem), 0) >= val:
                                continue
                            waited[id(sem)] = val
                            engine.wait_ge(sem, val)
                        if o["kind"] == "c":
                            ins = o["fn"](engine)
                            if o["signal"]:
                                ins.then_inc(csem[ename], 1)
                        else:
                            engine.dma_start(out=o["out"], in_=o["in_"], **o["kw"]).then_inc(
                                dsem[o["slot"]], 16)
                    if ename == "sp":
                        for t in final_tokens:
                            sem, val = resolve(t)
                            engine.wait_ge(sem, val)
                return body

            block.tensor(run("pe"))
            block.scalar(run("act"))
            block.vector(run("dve"))
            block.gpsimd(run("pool"))
            block.sync(run("sp"))


def build_program(debug=False, stage=None):
    nc = bass.Bass("TRN2", target_bir_lowering=False)
    dt_in = lambda name, shape: nc.dram_tensor(name, shape, F32, kind="ExternalInput").ap()
    x_own = dt_in("x_own", [1024, D])
    x_oth = dt_in("x_oth", [1024, D])
    x_halo = dt_in("x_halo", [128, D])
    c_col = dt_in("c_col", [128, 16])
    vcols = dt_in("vcols", [128, 32])
    nkB_d = dt_in("nkB", [128, S])
    qB_d = dt_in("qB", [128, 8])
    pw2_d = dt_in("pw2", [128, 32])
    Mm_d = dt_in("Mm", [128, 2 * 4 * 128])
    Hf_d = dt_in("Hf", [128, 8 * 4 * 16])
    I4_d = dt_in("I4", [128, 512])
    w_ada = dt_in("w_ada", [D, 6 * D])
    b_ada = dt_in("b_ada", [1, 6 * D])
    g_post_mix = dt_in("g_post_mix", [1, D])
    g_post_ffn = dt_in("g_post_ffn", [1, D])
    w_in = dt_in("w_in", [D, IN_W])
    w_pool = dt_in("w_pool", [4, 256, 256])
    pool_scale = dt_in("pool_scale", [1, 1024])
    g_pool_out = dt_in("g_pool_out", [1, 1024])
    g_attn_out = dt_in("g_attn_out", [1, 1024])
    w_out = dt_in("w_out", [D, D])
    w_ff1 = dt_in("w_ff1", [D, DFF])
    w_ff2 = dt_in("w_ff2", [DFF, D])
    y = nc.dram_tensor("y", [1024, D], F32, kind="ExternalOutput").ap()
    modD = nc.dram_tensor("modD", [1, 6 * D], F32).ap()
    x1D = nc.dram_tensor("x1D", [1024, D], F32).ap()
    dbg = {}
    if debug:
        for nm, shp in debug.items():
            dbg[nm] = nc.dram_tensor("dbg_" + nm, shp, F32, kind="ExternalOutput").ap()

    p = Prog(nc)
    final = []

    def finish():
        p.emit(final_tokens=final)
        return nc

    ARENA_BYTES = 210944
    arena_cm = nc.sbuf_tensor("arena", [128, ARENA_BYTES // 2], BF16)
    arena_t = arena_cm.__enter__()
    astate = dict(off=0, peak=0)

    def T(es, name, shape, dt):
        n = 1
        for s_ in shape[1:]:
            n *= s_
        nb = n * (4 if dt == F32 else 2)
        off = (astate["off"] + 63) // 64 * 64
        assert off + nb <= ARENA_BYTES, ("SBUF arena overflow", name, off + nb)
        astate["off"] = off + nb
        astate["peak"] = max(astate["peak"], off + nb)
        ap = arena_t[:, off // 2:(off + nb) // 2]
        if dt == F32:
            ap = ap.bitcast(F32)
        if len(shape) == 3:
            ap = ap.rearrange("p (a b) -> p a b", a=shape[1])
        elif len(shape) == 4:
            ap = ap.rearrange("p (a b c) -> p a b c", a=shape[1], b=shape[2])
        return ap

    def scope():
        es = ExitStack()
        m = astate["off"]

        def rel():
            astate["off"] = m
        es.callback(rel)
        return es

    def bank(b, n=1):
        return ["pb%d" % i for i in range(b, b + n)]

    with ExitStack() as G:
        ps = G.enter_context(nc.psum_tensor("ps", [128, 8, 512], F32))
        psf = ps[:].rearrange("p a b -> p (a b)")
        psb = ps[:].bitcast(BF16).rearrange("p a b -> p (a b)")
        ring = [T(G, "ring%d" % i, [128, 16, 512], BF16) for i in range(NRING)]
        st = T(G, "st", [128, 256], F32)
        cols = T(G, "cols", [128, 96], F32)
        identb = T(G, "identb", [128, 128], BF16)
        p.op("dve", lambda e: e.memset(st[:], 0.0), writes=["st"])
        p.dma("sp", cols[:, 0:32], vcols[:, :], writes=["cols_g"])
        p.dma("pool", identb[:], I4_d[:, 0:128], writes=["identb"])

        sched = []
        for cg in range(24):
            sched.append(("ada", w_ada[:, cg * 512:(cg + 1) * 512], 512))
        sched.append(("inA", w_in[:, 2048:2560], 512))
        sched.append(("inB", w_in[:, 4608:4752], 144))
        for i in range(2):
            sched.append(("q", w_in[:, 1024 + 512 * i:1536 + 512 * i], 512))
        for i in range(4):
            sched.append(("iq", w_in[:, 2560 + 512 * i:3072 + 512 * i], 512))
        for i in range(2):
            sched.append(("pool", w_in[:, 512 * i:512 * (i + 1)], 512))
        for cg in range(4):
            sched.append(("wout", w_out[:, cg * 512:(cg + 1) * 512], 512))
        for fg in range(16):
            sched.append(("ff1", w_ff1[:, fg * 512:(fg + 1) * 512], 512))
        for cg in range(4):
            for jg in range(4):
                sched.append(("ff2", w_ff2[jg * 2048:(jg + 1) * 2048, cg * 512:(cg + 1) * 512], 512))
        rstate = dict(loaded=0, used=0)

        def ring_load():
            i = rstate["loaded"]
            if i >= len(sched):
                return
            name, src, ncol = sched[i]
            buf = ring[i % NRING]
            p.dma("pool", buf[:, :, 0:ncol], src.rearrange("(k p) c -> p k c", p=128),
                  writes=[("ring", i % NRING)])
            rstate["loaded"] += 1

        def ring_get(name):
            i = rstate["used"]
            assert sched[i][0] == name, (sched[i][0], name)
            while rstate["loaded"] <= i:
                ring_load()
            return ring[i % NRING], ("ring", i % NRING)

        def ring_done():
            rstate["used"] += 1
            ring_load()

        for _ in range(NRING):
            ring_load()

        def mm(out, lhsT, rhs, start, stop, reads, writes, signal):
            p.op("pe", lambda e: e.matmul(out, lhsT=lhsT, rhs=rhs, start=start, stop=stop),
                 reads=reads, writes=writes, signal=signal)

        def rstd_batch(ss_ap, out_ap, n, key_in, key_out, tmp_ap):
            p.op("dve", lambda e: e.tensor_scalar(out=tmp_ap, in0=ss_ap, scalar1=1.0 / n, scalar2=EPS,
                                                   op0=ALU.mult, op1=ALU.add),
                 reads=(key_in if isinstance(key_in, list) else [key_in]), writes=[key_out + "_t"])
            p.op("act", lambda e: e.activation(out=tmp_ap, in_=tmp_ap, func=AF.Sqrt),
                 reads=[key_out + "_t"], writes=[key_out + "_t"])
            p.op("dve", lambda e: e.reciprocal(out=out_ap, in_=tmp_ap),
                 reads=[key_out + "_t"], writes=[key_out])

        def norm_transpose_pair(srcs, xt, xnb, dst, gcol, scol, tagbase, pbase, rstd_known=None):
            n = len(srcs)
            pv = psb[:, pbase * 1024:(pbase + 4) * 1024].rearrange("p (k t) -> p k t", k=16)
            for i, (kind, src, sc) in enumerate(srcs):
                if kind == "dram":
                    xa = xt[i]
                    xkey = ("xt", i)
                    p.dma("sp", xa[:], src, writes=[xkey])
                    xin = xa[:]
                else:
                    xin, xkey = src
                nb = xnb[i]
                nkey = ("xnb", i)
                if rstd_known is None:
                    p.op("act", lambda e, xin=xin, nb=nb, sc=sc: e.activation(
                        out=nb[:], in_=xin, func=AF.Square, accum_out=st[:, sc:sc + 1]),
                        reads=[xkey], writes=[nkey, ("st", sc)])
                    rstd_batch(st[:, sc:sc + 1], st[:, sc + 32:sc + 33], D, ("st", sc), "rs%s%d" % (tagbase, sc),
                               st[:, sc + 64:sc + 65])
                    rkey = "rs%s%d" % (tagbase, sc)
                    rap = st[:, sc + 32:sc + 33]
                else:
                    rap, rkey = rstd_known[i]
                p.op("dve", lambda e, xin=xin, nb=nb, rap=rap: e.tensor_scalar(
                    out=nb[:], in0=xin, scalar1=rap, scalar2=None, op0=ALU.mult),
                    reads=[xkey, rkey], writes=[nkey])
                for k in range(16):
                    p.op("pe", lambda e, k=k, i=i, nb=nb: e.transpose(
                        out=pv[:, k, i * 128:(i + 1) * 128], in_=nb[:, k * 128:(k + 1) * 128], identity=identb[:]),
                        reads=[nkey, "identb"], writes=bank(pbase, 4), signal=(k == 15))
            w = n * 128
            for k in range(16):
                if (k // 4) % 2 == 0:
                    p.op("act", lambda e, k=k: e.activation(
                        out=dst[:, k, 0:w], in_=pv[:, k, 0:w], func=AF.Identity,
                        scale=cols[:, gcol + k:gcol + k + 1], bias=cols[:, scol + k:scol + k + 1]),
                        reads=bank(pbase, 4) + ["cols_m"], writes=[dst_key(dst)])
                else:
                    p.op("dve", lambda e, k=k: e.tensor_scalar(
                        out=dst[:, k, 0:w], in0=pv[:, k, 0:w], scalar1=cols[:, gcol + k:gcol + k + 1],
                        scalar2=cols[:, scol + k:scol + k + 1], op0=ALU.mult, op1=ALU.add),
                        reads=bank(pbase, 4) + ["cols_m"], writes=[dst_key(dst)])

        dkeys = {}

        def dst_key(ap):
            return dkeys[id(ap)]

        def dbg_dump(name, ap_sb, key):
            if debug and name in dbg:
                final.append(p.dma("pool", dbg[name], ap_sb, reads=[key]))

        with scope() as P0:
            modrow = T(P0, "modrow", [1, 6 * D], F32)[0:1, :]
            brow = T(P0, "brow", [1, 6 * D], F32)[0:1, :]
            ctmp = T(P0, "ctmp", [128, 32], F32)
            caT = T(P0, "caT", [128, 16], BF16)
            p.dma("sp", ctmp[:, 0:16], c_col[:, :], writes=["ccol"])
            p.dma("sp", brow[:], b_ada[:, :], writes=["brow"])
            p.op("act", lambda e: e.activation(out=ctmp[:, 16:32], in_=ctmp[:, 0:16], func=AF.Exp, scale=-1.0),
                 reads=["ccol"], writes=["cexp"])
            p.op("dve", lambda e: e.tensor_scalar(out=ctmp[:, 16:32], in0=ctmp[:, 16:32], scalar1=1.0, scalar2=None,
                                                   op0=ALU.add), reads=["cexp"], writes=["cexp"])
            p.op("dve", lambda e: e.reciprocal(out=ctmp[:, 16:32], in_=ctmp[:, 16:32]), reads=["cexp"], writes=["cexp"])
            p.op("dve", lambda e: e.tensor_tensor(out=caT[:], in0=ctmp[:, 0:16], in1=ctmp[:, 16:32], op=ALU.mult),
                 reads=["cexp", "ccol"], writes=["caT"])
            for cg in range(24):
                W, wkey = ring_get("ada")
                b = cg % 2
                for k in range(16):
                    mm(ps[0:1, b, :], caT[:, k:k + 1], W[:, k, :], k == 0, k == 15,
                       [wkey, "caT"], bank(b), k == 15)
                ring_done()
                p.op("dve", lambda e, b=b, cg=cg: e.tensor_tensor(
                    out=modrow[0:1, cg * 512:(cg + 1) * 512], in0=ps[0:1, b, :],
                    in1=brow[0:1, cg * 512:(cg + 1) * 512], op=ALU.add),
                    reads=bank(b) + ["brow"], writes=["modrow"])
            p.dma("sp", modD[:, :], modrow[:], reads=["modrow"], writes=["modD"])
            def colload(dstc, off, key):
                p.dma("sp", cols[:, dstc:dstc + 16],
                      modD[0, off:off + D].rearrange("(k p) -> p k", p=128),
                      reads=["modD"], writes=[key], allow_slow_non_contiguous=True)
            colload(48, 0, "c_sh1")
            colload(32, D, "c_sc1")
            colload(80, 3 * D, "c_sh2")
            colload(64, 4 * D, "c_sc2")
            p.op("dve", lambda e: e.scalar_tensor_tensor(out=cols[:, 32:48], in0=cols[:, 32:48], scalar=1.0,
                                                          in1=cols[:, 0:16], op0=ALU.add, op1=ALU.mult),
                 reads=["c_sc1", "cols_g"], writes=["c_sc1"])
            p.op("dve", lambda e: e.scalar_tensor_tensor(out=cols[:, 64:80], in0=cols[:, 64:80], scalar=1.0,
                                                          in1=cols[:, 16:32], op0=ALU.add, op1=ALU.mult),
                 reads=["c_sc2", "cols_g", "c_sh1", "c_sh2"], writes=["c_sc2", "cols_m"])
            if debug:
                dbg_dump("cols", cols[:], "cols_m")
            p.barrier()
            if stage == "p0":
                return finish()

        with scope() as S1:
            big16 = T(S1, "big16", [128, 16, 1024], BF16)
            dkeys[id(big16)] = "big16"
            with scope() as S2:
                KT = T(S2, "KT", [128, 2, S], BF16)
                Vaug = T(S2, "Vaug", [128, 16, 2, 129], BF16)
                kiT = T(S2, "kiT", [128, S], BF16)
                wq = T(S2, "wq", [128, 8, 16], F32)
                with scope() as S4:
                    qT = T(S4, "qT", [128, 8, 8, 128], BF16)
                    u = T(S4, "u", [128, 8, 1024], BF16)
                    uh = T(S4, "uh", [128, 1024], BF16)
                    with scope() as S3:
                        hT_own = T(S3, "hT_own", [128, 16, 1024], BF16)
                        hT_halo = T(S3, "hT_halo", [128, 16, 128], BF16)
                        dkeys[id(hT_own)] = "hT_own"
                        dkeys[id(hT_halo)] = "hT_halo"
                        xt = [T(S3, "xt%d" % i, [128, D], F32) for i in range(2)]
                        xnb = [T(S3, "xnb%d" % i, [128, D], BF16) for i in range(2)]
                        grp = 0
                        for which, src, dstT in (("own", x_own, hT_own), ("oth", x_oth, big16)):
                            for j0 in range(0, 8, 2):
                                srcs = [("dram", src[(j0 + i) * 128:(j0 + i + 1) * 128, :],
                                         (0 if which == "own" else 8) + j0 + i) for i in range(2)]
                                dview = dstT[:, :, j0 * 128:(j0 + 2) * 128]
                                dkeys[id(dview)] = dkeys[id(dstT)]
                                norm_transpose_pair(srcs, xt, xnb, dview, 32, 48, "a", 4 * (grp % 2))
                                grp += 1
                        dview = hT_halo[:, :, :]
                        dkeys[id(dview)] = "hT_halo"
                        norm_transpose_pair([("dram", x_halo[:, :], 16)], xt, xnb, dview, 32, 48, "a", 4 * (grp % 2))
                        if debug:
                            dbg_dump("hT_own", hT_own[:], "hT_own")
                            dbg_dump("hT_oth", big16[:], "big16")
                            dbg_dump("hT_halo", hT_halo[:], "hT_halo")
                        if stage == "p1":
                            return finish()
                        WA, keyA = ring_get("inA")
                        ring_done_A = False
                        pb = [0]

                        def nextbank():
                            b = pb[0]
                            pb[0] = (b + 1) % 8
                            return b

                        p.op("dve", lambda e: e.memset(Vaug[:, :, :, 128:129], 1.0), writes=["Vaug"])
                        KTv = KT[:].rearrange("p g (j two t) -> p g j two t", two=2, t=128)
                        kiTv = kiT[:].rearrange("p (j two t) -> p j two t", two=2, t=128)
                        cpy = [0]

                        def evac_copy(out, in_, reads, writes):
                            cpy[0] += 1
                            if cpy[0] % 2:
                                p.op("act", lambda e: e.activation(out=out, in_=in_, func=AF.Copy),
                                     reads=reads, writes=writes)
                            else:
                                p.op("dve", lambda e: e.tensor_copy(out=out, in_=in_), reads=reads, writes=writes)

                        for par, hsrc, hkey in ((0, hT_own, "hT_own"), (1, big16, "big16")):
                            for tg in range(2):
                                for c in range(2):
                                    b = nextbank()
                                    for k in range(16):
                                        mm(ps[:, b, :], WA[:, k, c * 128:(c + 1) * 128], hsrc[:, k, tg * 512:(tg + 1) * 512],
                                           k == 0, k == 15, [keyA, hkey], bank(b), k == 15)
                                    evac_copy(KTv[:, c, tg * 4:(tg + 1) * 4, par, :],
                                              ps[:, b, :].rearrange("p (j t) -> p j t", t=128), bank(b), ["KT"])
                                for tt in range(4):
                                    jt = tg * 4 + tt
                                    b = nextbank()
                                    for k in range(16):
                                        mm(ps[:, b, 0:256], hsrc[:, k, jt * 128:(jt + 1) * 128], WA[:, k, 256:512],
                                           k == 0, k == 15, [keyA, hkey], bank(b), k == 15)
                                    evac_copy(Vaug[:, 2 * jt + par, :, 0:128],
                                              ps[:, b, 0:256].rearrange("p (g d) -> p g d", d=128), bank(b), ["Vaug"])
                        ring_done()
                        WB, keyB = ring_get("inB")
                        for par, hsrc, hkey in ((0, hT_own, "hT_own"), (1, big16, "big16")):
                            for tg in range(2):
                                b = nextbank()
                                for k in range(16):
                                    mm(ps[:, b, :], WB[:, k, 0:128], hsrc[:, k, tg * 512:(tg + 1) * 512],
                                       k == 0, k == 15, [keyB, hkey], bank(b), k == 15)
                                evac_copy(kiTv[:, tg * 4:(tg + 1) * 4, par, :],
                                          ps[:, b, :].rearrange("p (j t) -> p j t", t=128), bank(b), ["kiT"])
                        for j in range(8):
                            b = nextbank()
                            for k in range(16):
                                mm(ps[:, b, 0:16], hT_own[:, k, j * 128:(j + 1) * 128], WB[:, k, 128:144],
                                   k == 0, k == 15, [keyB, "hT_own"], bank(b), k == 15)
                            p.op("dve", lambda e, b=b, j=j: e.tensor_scalar(
                                out=wq[:, j, :], in0=ps[:, b, 0:16], scalar1=IDX_SCALE, scalar2=None, op0=ALU.mult),
                                reads=bank(b), writes=["wq"])
                        ring_done()
                        if debug:
                            dbg_dump("KT", KT[:], "KT")
                            dbg_dump("kiT", kiT[:], "kiT")
                            dbg_dump("Vaug", Vaug[:], "Vaug")
                            dbg_dump("wq", wq[:], "wq")
                        p.barrier()
                        if stage == "p2a":
                            return finish()
                        for i in range(2):
                            W, wkey = ring_get("q")
                            for hh in range(4):
                                h = 4 * i + hh
                                for tg in range(2):
                                    b = nextbank()
                                    for k in range(16):
                                        mm(ps[:, b, :], W[:, k, hh * 128:(hh + 1) * 128], hT_own[:, k, tg * 512:(tg + 1) * 512],
                                           k == 0, k == 15, [wkey, "hT_own"], bank(b), k == 15)
                                    evac_copy(qT[:, tg * 4:(tg + 1) * 4, h, :],
                                              ps[:, b, :].rearrange("p (j t) -> p j t", t=128), bank(b), ["qT"])
                            ring_done()
                        for i in range(4):
                            W, wkey = ring_get("iq")
                            for hh in range(4):
                                h = 4 * i + hh
                                for tg in range(2):
                                    b = nextbank()
                                    for k in range(16):
                                        mm(ps[:, b, :], W[:, k, hh * 128:(hh + 1) * 128], hT_own[:, k, tg * 512:(tg + 1) * 512],
                                           k == 0, k == 15, [wkey, "hT_own"], bank(b), k == 15)
                                    evac_copy(big16[:, h, tg * 512:(tg + 1) * 512], ps[:, b, :], bank(b),
                                              [("b16", tg * 4 + jj) for jj in range(4)])
                            ring_done()
                        for i in range(2):
                            W, wkey = ring_get("pool")
                            for j in range(9):
                                b = nextbank()
                                for k in range(16):
                                    lhsT = hT_own[:, k, j * 128:(j + 1) * 128] if j < 8 else hT_halo[:, k, :]
                                    mm(ps[:, b, :], lhsT, W[:, k, :], k == 0, k == 15,
                                       [wkey, "hT_own", "hT_halo"], bank(b), k == 15)
                                dst = u[:, j, i * 512:(i + 1) * 512] if j < 8 else uh[:, i * 512:(i + 1) * 512]
                                evac_copy(dst, ps[:, b, :], bank(b), ["u"])
                            ring_done()
                        if debug:
                            dbg_dump("qT", qT[:], "qT")
                            dbg_dump("qiT", big16[:], ("b16", 7))
                            dbg_dump("u", u[:], "u")
                            dbg_dump("uh", uh[:], "u")
                        p.barrier()
                        if stage == "p2b":
                            return finish()
                    with scope() as P3:
                        nkB = T(P3, "nkB", [128, S], F32)
                        qB = T(P3, "qB", [128, 8], F32)
                        pw2 = T(P3, "pw2", [128, 32], F32)
                        Mm = T(P3, "Mm", [128, 2, 4, 128], BF16)
                        Hf = T(P3, "Hf", [128, 8, 4, 16], BF16)
                        I4 = T(P3, "I4", [128, 512], BF16)
                        wps = T(P3, "wps", [128, 4, 2, 256], BF16)
                        wpf = T(P3, "wpf", [128, 4, 2, 256], F32)
                        gb = T(P3, "gb", [128, 2048], F32)
                        psb_t = T(P3, "psb_t", [128, 1024], F32)
                        score = T(P3, "score", [128, S], F32)
                        Rb = [T(P3, "Rb%d" % i, [128, 1024], F32) for i in range(2)]
                        maskb = T(P3, "maskb", [128, S], BF16)
                        PT = [T(P3, "PT%d" % i, [128, 512], BF16) for i in range(2)]
                        attn32 = T(P3, "attn32", [128, 1024], F32)
                        mg = T(P3, "mg", [128, 2048], BF16)
                        pooledT = T(P3, "pooledT", [128, 8, 128], BF16)
                        bs = T(P3, "bs", [128, 64], F32)
                        p.dma("sp", nkB[:], nkB_d[:, :], writes=["nkB"])
                        p.dma("sp", qB[:], qB_d[:, :], writes=["qB"])
                        p.dma("sp", pw2[:], pw2_d[:, :], writes=["pw2"])
                        p.dma("pool", Mm[:].rearrange("p a g t -> p (a g t)"), Mm_d[:, :], writes=["Mm"])
                        p.dma("pool", Hf[:].rearrange("p a g t -> p (a g t)"), Hf_d[:, :], writes=["Hf"])
                        p.dma("pool", I4[:], I4_d[:, :], writes=["I4"])
                        p.dma("sp", wpf[:], w_pool.rearrange("g (cc p) d -> p g cc d", p=128), writes=["wpf"])
                        p.dma("sp", psb_t[:], pool_scale[0:1, :].partition_broadcast(128), writes=["psb_t"])
                        p.dma("sp", gb[:, 0:1024], g_pool_out[0:1, :].partition_broadcast(128), writes=["gb0"])
                        p.dma("sp", gb[:, 1024:2048], g_attn_out[0:1, :].partition_broadcast(128), writes=["gb1"])
                        for g in range(4):
                            p.op("dve", lambda e, g=g: e.tensor_tensor(
                                out=wps[:, g, :, :], in0=wpf[:, g, :, :],
                                in1=psb_t[:, g * 256:(g + 1) * 256].unsqueeze(1).to_broadcast([128, 2, 256]),
                                op=ALU.mult), reads=["wpf", "psb_t"], writes=["wps"])
                        for j in range(8):
                            L = 2 * j + 2
                            N = 128 * L
                            chunks = [(c0, min(1024, N - c0)) for c0 in range(0, N, 1024)]
                            ci = 0
                            for h in range(16):
                                for (c0, cw) in chunks:
                                    bb = 2 * (ci % 2)
                                    rb = Rb[ci % 2]
                                    rkey = ("Rb", ci % 2)
                                    ci += 1
                                    nmm = (cw + 511) // 512
                                    for m in range(nmm):
                                        w = min(512, cw - m * 512)
                                        mm(ps[:, bb + m, 0:w], big16[:, h, j * 128:(j + 1) * 128],
                                           kiT[:, c0 + m * 512:c0 + m * 512 + w], True, True,
                                           [("b16", j), "kiT"], bank(bb + m), m == nmm - 1)
                                    pin = psf[:, bb * 512:bb * 512 + cw]
                                    p.op("act", lambda e, rb=rb, pin=pin, cw=cw: e.activation(
                                        out=rb[:, 0:cw], in_=pin, func=AF.Relu),
                                        reads=bank(bb, nmm), writes=[rkey])
                                    if h == 0:
                                        p.op("dve", lambda e, rb=rb, c0=c0, cw=cw, j=j: e.tensor_scalar(
                                            out=score[:, c0:c0 + cw], in0=rb[:, 0:cw], scalar1=wq[:, j, 0:1],
                                            scalar2=None, op0=ALU.mult), reads=[rkey, "wq"], writes=["score"])
                                    else:
                                        p.op("dve", lambda e, rb=rb, c0=c0, cw=cw, j=j, h=h: e.scalar_tensor_tensor(
                                            out=score[:, c0:c0 + cw], in0=rb[:, 0:cw], scalar=wq[:, j, h:h + 1],
                                            in1=score[:, c0:c0 + cw], op0=ALU.mult, op1=ALU.add),
                                            reads=[rkey, "wq", "score"], writes=["score"])
                            if debug and j == 1:
                                dbg_dump("score1", score[:, 0:512], "score")
                            p.op("dve", lambda e, N=N: e.tensor_reduce(out=bs[:, 0:1], in_=score[:, 0:N], axis=AX.X, op=ALU.max),
                                 reads=["score"], writes=["bsM"])
                            p.op("dve", lambda e, N=N: e.tensor_reduce(out=bs[:, 1:2], in_=score[:, 0:N], axis=AX.X, op=ALU.min),
                                 reads=["score"], writes=["bsm"])
                            p.op("dve", lambda e, N=N, j=j: e.scalar_tensor_tensor(
                                out=score[:, 0:N], in0=nkB[:, 0:N], scalar=qB[:, j:j + 1], in1=score[:, 0:N],
                                op0=ALU.add, op1=ALU.min), reads=["nkB", "qB", "score"], writes=["score"])
                            p.op("dve", lambda e: e.tensor_scalar(out=bs[:, 2:3], in0=bs[:, 1:2], scalar1=-1.0, scalar2=None,
                                                                   op0=ALU.add), reads=["bsm"], writes=["bslo"])
                            p.op("dve", lambda e: e.tensor_tensor(out=bs[:, 3:4], in0=bs[:, 0:1], in1=bs[:, 2:3], op=ALU.subtract),
                                 reads=["bsM", "bslo"], writes=["bsW"])
                            p.op("dve", lambda e: e.tensor_scalar(out=bs[:, 32:64], in0=pw2[:, :], scalar1=bs[:, 3:4], scalar2=None,
                                                                   op0=ALU.mult), reads=["bsW", "pw2"], writes=["bswd"])
                            p.op("dve", lambda e: e.tensor_tensor(out=bs[:, 4:5], in0=bs[:, 2:3], in1=bs[:, 32:33], op=ALU.add),
                                 reads=["bslo", "bswd"], writes=["bsmid"])
                            for it in range(NIT):
                                p.op("dve", lambda e, N=N: e.tensor_scalar(
                                    out=maskb[:, 0:N], in0=score[:, 0:N], scalar1=bs[:, 4:5], scalar2=None,
                                    op0=ALU.is_ge, op1=ALU.add, accum_out=bs[:, 5:6]),
                                    reads=["score", "bsmid", "maskb"], writes=["maskb", "bscnt"])
                                last = it == NIT - 1
                                p.op("dve", lambda e, last=last: e.tensor_scalar(
                                    out=bs[:, 6:7], in0=bs[:, 5:6], scalar1=TOPK, scalar2=(-1.0 if last else -0.5),
                                    op0=ALU.is_ge, op1=ALU.add), reads=["bscnt"], writes=["bsge"])
                                p.op("dve", lambda e, it=it: e.scalar_tensor_tensor(
                                    out=bs[:, 4:5], in0=bs[:, 6:7], scalar=bs[:, 32 + it:33 + it], in1=bs[:, 4:5],
                                    op0=ALU.mult, op1=ALU.add), reads=["bsge", "bswd", "bsmid"], writes=["bsmid"])
                            if debug and j == 1:
                                dbg_dump("tau1", bs[:, 0:8], "bsmid")
                            p.op("dve", lambda e, N=N: e.tensor_scalar(
                                out=maskb[:, 0:N], in0=score[:, 0:N], scalar1=bs[:, 4:5], scalar2=MASKV,
                                op0=ALU.is_lt, op1=ALU.mult), reads=["score", "bsmid", "maskb"], writes=["maskb"])
                            def oacc(h, n=129):
                                return ps[:, 5 + h // 3, (h % 3) * 129:(h % 3) * 129 + n]
                            ui = 0
                            for kt in range(L):
                                for g in range(2):
                                    mm(ps[:, 4, :], KT[:, g, kt * 128:(kt + 1) * 128],
                                       qT[:, j, 4 * g:4 * g + 4, :], True, False,
                                       ["KT", "qT"], bank(4), False)
                                    mm(ps[:, 4, :], maskb[:, kt * 128:(kt + 1) * 128], I4[:, :], False, True,
                                       ["maskb", "I4"], bank(4), True)
                                    pt = PT[ui % 2]
                                    pkey = ("PT", ui % 2)
                                    ui += 1
                                    p.op("act", lambda e, pt=pt: e.activation(out=pt[:], in_=ps[:, 4, :], func=AF.Exp,
                                                                              scale=ATT_SCALE),
                                         reads=bank(4), writes=[pkey])
                                    for hh in range(4):
                                        h = 4 * g + hh
                                        lastmm = (kt == L - 1) and (g == 1) and (hh == 3)
                                        mm(oacc(h), pt[:, hh * 128:(hh + 1) * 128], Vaug[:, kt, g, :],
                                           kt == 0 and h % 3 == 0, kt == L - 1, [pkey, "Vaug"], bank(5, 3),
                                           lastmm or hh == 3)
                            for bk in range(3):
                                nh = 3 if bk < 2 else 2
                                den = ps[:, 5 + bk, 0:nh * 129].rearrange("p (h c) -> p h c", c=129)
                                p.op("dve", lambda e, den=den, bk=bk, nh=nh: e.reciprocal(
                                    out=bs[:, 8 + 3 * bk:8 + 3 * bk + nh], in_=den[:, :, 128]),
                                    reads=bank(5, 3), writes=[("rc", bk)])
                                p.op("dve", lambda e, den=den, bk=bk, nh=nh: e.tensor_tensor(
                                    out=attn32[:, 384 * bk:384 * bk + 128 * nh].rearrange("p (h d) -> p h d", d=128),
                                    in0=den[:, :, 0:128],
                                    in1=bs[:, 8 + 3 * bk:8 + 3 * bk + nh].unsqueeze(2).to_broadcast([128, nh, 128]),
                                    op=ALU.mult), reads=bank(5, 3) + [("rc", bk)], writes=["attn32"])
                            p.op("act", lambda e, j=j: e.activation(out=mg[:, 1024:2048], in_=attn32[:], func=AF.Square,
                                                                    accum_out=st[:, 128 + j:129 + j]),
                                 reads=["attn32"], writes=["mg1", ("st", 128 + j)])
                            p.op("dve", lambda e: e.tensor_tensor(out=mg[:, 1024:2048], in0=attn32[:], in1=gb[:, 1024:2048],
                                                                  op=ALU.mult), reads=["attn32", "gb1"], writes=["mg1"])
                            if debug and j == 1:
                                dbg_dump("attn1", attn32[:], "attn32")
                            var = 0 if j == 0 else 1
                            psP = psf[:, 0:1024].rearrange("p (c t) -> p c t", t=128)
                            for cch in range(8):
                                g = cch // 2
                                mm(psP[:, cch, :], u[:, j, cch * 128:(cch + 1) * 128], Mm[:, var, g, :], True, False,
                                   ["u", "Mm"], bank(0, 2), False)
                                mm(psP[:, cch, 0:16], uh[:, cch * 128:(cch + 1) * 128], Hf[:, j, g, :], False, True,
                                   ["u", "Hf"], bank(0, 2), cch == 7)
                            p.op("act", lambda e: e.activation(out=pooledT[:].rearrange("p c t -> p (c t)"), in_=psf[:, 0:1024],
                                                               func=AF.Copy), reads=bank(0, 2), writes=["pooledT"])
                            for g in range(4):
                                for cc in range(2):
                                    mm(psf[:, 1024 + g * 256:1024 + (g + 1) * 256], pooledT[:, 2 * g + cc, :], wps[:, g, cc, :],
                                       cc == 0, cc == 1, ["pooledT", "wps"], bank(2, 2), (g == 3 and cc == 1))
                            p.op("act", lambda e, j=j: e.activation(out=mg[:, 0:1024], in_=psf[:, 1024:2048], func=AF.Square,
                                                                    accum_out=st[:, 136 + j:137 + j]),
                                 reads=bank(2, 2), writes=["mg0", ("st", 136 + j)])
                            p.op("dve", lambda e: e.tensor_tensor(out=mg[:, 0:1024], in0=psf[:, 1024:2048], in1=gb[:, 0:1024],
                                                                  op=ALU.mult), reads=bank(2, 2) + ["gb0"], writes=["mg0"])
                            pv = psb[:, 0:2048].rearrange("p (c t) -> p c t", t=128)
                            for c in range(16):
                                p.op("pe", lambda e, c=c: e.transpose(out=pv[:, c, :], in_=mg[:, c * 128:(c + 1) * 128],
                                                                      identity=identb[:]),
                                     reads=["mg0", "mg1", "identb"], writes=bank(0, 2), signal=(c == 15))
                            p.op("act", lambda e, j=j: e.activation(out=big16[:, :, j * 128:(j + 1) * 128], in_=pv,
                                                                    func=AF.Copy),
                                 reads=bank(0, 2), writes=[("b16", j)])
                        if debug:
                            dbg_dump("mergedT", big16[:], ("b16", 7))
                            dbg_dump("st", st[:], ("st", 143))
                        p.barrier()
                        if stage == "p3":
                            return finish()
            with scope() as P4:
                mix = T(P4, "mix", [128, 8, D], F32)
                gg = T(P4, "gg", [128, D], F32)
                gt = T(P4, "gt", [128, D], F32)
                xt = [T(P4, "xt4_%d" % i, [128, D], F32) for i in range(2)]
                xnb = [T(P4, "xnb4_%d" % i, [128, D], BF16) for i in range(2)]
                p.dma("sp", gg[:], modD[0:1, 2 * D:3 * D].partition_broadcast(128), reads=["modD"], writes=["gg"])
                p.dma("sp", gt[:], g_post_mix[0:1, :].partition_broadcast(128), writes=["gt"])
                p.op("dve", lambda e: e.tensor_tensor(out=gg[:], in0=gg[:], in1=gt[:], op=ALU.mult),
                     reads=["gg", "gt"], writes=["gg"])
                rstd_batch(st[:, 128:144], st[:, 144:160], 1024, [("st", 128 + i) for i in range(16)], "rs_pa", st[:, 160:176])
                for cg in range(4):
                    W, wkey = ring_get("wout")
                    for j in range(8):
                        bA = (2 * j) % 8
                        bB = (2 * j + 1) % 8
                        for k in range(8):
                            mm(ps[:, bA, :], big16[:, k, j * 128:(j + 1) * 128], W[:, k, :], k == 0, k == 7,
                               [wkey, ("b16", j)], bank(bA), k == 7)
                        for k in range(8, 16):
                            mm(ps[:, bB, :], big16[:, k, j * 128:(j + 1) * 128], W[:, k, :], k == 8, k == 15,
                               [wkey, ("b16", j)], bank(bB), k == 15)
                        p.op("act", lambda e, j=j, cg=cg, bA=bA: e.activation(
                            out=mix[:, j, cg * 512:(cg + 1) * 512], in_=ps[:, bA, :], func=AF.Identity,
                            scale=st[:, 152 + j:153 + j]), reads=bank(bA) + ["rs_pa"], writes=[("mix", j)])
                        p.op("dve", lambda e, j=j, cg=cg, bB=bB: e.scalar_tensor_tensor(
                            out=mix[:, j, cg * 512:(cg + 1) * 512], in0=ps[:, bB, :], scalar=st[:, 144 + j:145 + j],
                            in1=mix[:, j, cg * 512:(cg + 1) * 512], op0=ALU.mult, op1=ALU.add),
                            reads=bank(bB) + ["rs_pa", ("mix", j)], writes=[("mix", j)])
                    ring_done()
                p.barrier()
                for j in range(8):
                    p.op("act", lambda e, j=j: e.activation(out=xnb[j % 2][:], in_=mix[:, j, :], func=AF.Square,
                                                            accum_out=st[:, 176 + j:177 + j]),
                         reads=[("mix", j)], writes=[("xnb", j % 2), ("st", 176 + j)])
                rstd_batch(st[:, 176:184], st[:, 184:192], D, [("st", 176 + i) for i in range(8)], "rs_m", st[:, 192:200])
                for j in range(8):
                    p.dma("sp", xt[j % 2][:], x_own[j * 128:(j + 1) * 128, :], writes=[("xt", j % 2)])
                    p.op("dve", lambda e, j=j: e.scalar_tensor_tensor(
                        out=mix[:, j, :], in0=mix[:, j, :], scalar=st[:, 184 + j:185 + j], in1=gg[:],
                        op0=ALU.mult, op1=ALU.mult), reads=[("mix", j), "rs_m", "gg"], writes=[("mix", j)])
                    p.op("dve", lambda e, j=j: e.tensor_tensor(out=mix[:, j, :], in0=mix[:, j, :], in1=xt[j % 2][:], op=ALU.add),
                         reads=[("mix", j), ("xt", j % 2)], writes=[("mix", j)])
                    p.dma("sp", x1D[j * 128:(j + 1) * 128, :], mix[:, j, :], reads=[("mix", j)], writes=["x1D"])
                    p.op("act", lambda e, j=j: e.activation(out=xnb[j % 2][:], in_=mix[:, j, :], func=AF.Square,
                                                            accum_out=st[:, 200 + j:201 + j]),
                         reads=[("mix", j)], writes=[("xnb", j % 2), ("st", 200 + j)])
                rstd_batch(st[:, 200:208], st[:, 208:216], D, [("st", 200 + i) for i in range(8)], "rs_2", st[:, 216:224])
                if debug:
                    dbg_dump("x1", mix[:], ("mix", 7))
                for j0 in range(0, 8, 2):
                    srcs = [("sbuf", (mix[:, j0 + i, :], ("mix", j0 + i)), 0) for i in range(2)]
                    dview = big16[:, :, j0 * 128:(j0 + 2) * 128]
                    dkeys[id(dview)] = "h2T"
                    norm_transpose_pair(srcs, None, xnb, dview, 64, 80, "b", 4 * ((j0 // 2) % 2),
                                        rstd_known=[(st[:, 208 + j0 + i:209 + j0 + i], "rs_2") for i in range(2)])
                if debug:
                    dbg_dump("h2T", big16[:], "h2T")
                p.barrier()
                if stage == "p4":
                    return finish()
            with scope() as P5:
                fT = T(P5, "fT", [128, 64, 1024], BF16)
                r32 = [T(P5, "r32_%d" % i, [128, 512], F32) for i in range(2)]
                ei = 0
                for fg in range(16):
                    W, wkey = ring_get("ff1")
                    for fc in range(4):
                        jc = fg * 4 + fc
                        for tg in range(2):
                            b = ei % 4
                            for k in range(16):
                                mm(ps[:, b, :], W[:, k, fc * 128:(fc + 1) * 128], big16[:, k, tg * 512:(tg + 1) * 512],
                                   k == 0, k == 15, [wkey, "h2T"], bank(b), k == 15)
                            r = r32[ei % 2]
                            rkey = ("r32", ei % 2)
                            ei += 1
                            p.op("act", lambda e, r=r, b=b: e.activation(out=r[:], in_=ps[:, b, :], func=AF.Relu),
                                 reads=bank(b), writes=[rkey])
                            p.op("dve", lambda e, r=r, jc=jc, tg=tg: e.tensor_tensor(
                                out=fT[:, jc, tg * 512:(tg + 1) * 512], in0=r[:], in1=r[:], op=ALU.mult),
                                reads=[rkey], writes=["fT"])
                    ring_done()
                if debug:
                    dbg_dump("fT", fT[:, 0:4, :], "fT")
                p.barrier()
                if stage == "p5":
                    return finish()
                fo = big16[:].rearrange("p a b -> p (a b)").rearrange("p (j d) -> p j d", d=D)
                jk = T(P5, "jk", [128, 512], BF16)
                for cg in range(4):
                    for jg in range(4):
                        W, wkey = ring_get("ff2")
                        for jj in range(16):
                            jc = jg * 16 + jj
                            for tt in range(8):
                                mm(ps[:, tt, :], fT[:, jc, tt * 128:(tt + 1) * 128], W[:, jj, :], jc == 0, jc == 63,
                                   [wkey, "fT"], bank(tt), jc == 63 or (jj == 15 and tt == 7))
                        ring_done()
                    for tt in range(8):
                        if tt % 2 == 0:
                            p.op("act", lambda e, tt=tt, cg=cg: e.activation(
                                out=fo[:, tt, cg * 512:(cg + 1) * 512], in_=ps[:, tt, :], func=AF.Copy),
                                reads=bank(tt), writes=[("fo", tt)])
                        else:
                            p.op("dve", lambda e, tt=tt, cg=cg: e.tensor_copy(
                                out=fo[:, tt, cg * 512:(cg + 1) * 512], in_=ps[:, tt, :]),
                                reads=bank(tt), writes=[("fo", tt)])
                if debug:
                    dbg_dump("fo", big16[:], ("fo", 7))
                p.barrier()
                if stage == "p6":
                    return finish()
            with scope() as P7:
                gg7 = T(P7, "gg7", [128, D], F32)
                gt7 = T(P7, "gt7", [128, D], F32)
                xt7 = [T(P7, "xt7_%d" % i, [128, D], F32) for i in range(2)]
                ot7 = [T(P7, "ot7_%d" % i, [128, D], F32) for i in range(2)]
                fo = big16[:].rearrange("p a b -> p (a b)").rearrange("p (j d) -> p j d", d=D)
                p.dma("sp", gg7[:], modD[0:1, 5 * D:6 * D].partition_broadcast(128), reads=["modD"], writes=["gg7"])
                p.dma("sp", gt7[:], g_post_ffn[0:1, :].partition_broadcast(128), writes=["gt7"])
                p.op("dve", lambda e: e.tensor_tensor(out=gg7[:], in0=gg7[:], in1=gt7[:], op=ALU.mult),
                     reads=["gg7", "gt7"], writes=["gg7"])
                for tt in range(8):
                    p.op("act", lambda e, tt=tt: e.activation(out=ot7[tt % 2][:], in_=fo[:, tt, :], func=AF.Square,
                                                              accum_out=st[:, 224 + tt:225 + tt]),
                         reads=[("fo", tt)], writes=[("ot", tt % 2), ("st", 224 + tt)])
                rstd_batch(st[:, 224:232], st[:, 72:80], D, [("st", 224 + i) for i in range(8)], "rs_f", st[:, 80:88])
                for tt in range(8):
                    p.dma("sp", xt7[tt % 2][:], x1D[tt * 128:(tt + 1) * 128, :], reads=["x1D"], writes=[("xt7", tt % 2)])
                    o = ot7[tt % 2]
                    p.op("dve", lambda e, tt=tt, o=o: e.scalar_tensor_tensor(
                        out=o[:], in0=fo[:, tt, :], scalar=st[:, 72 + tt:73 + tt], in1=gg7[:],
                        op0=ALU.mult, op1=ALU.mult), reads=[("fo", tt), "rs_f", "gg7"], writes=[("ot", tt % 2)])
                    p.op("dve", lambda e, tt=tt, o=o: e.tensor_tensor(out=o[:], in0=o[:], in1=xt7[tt % 2][:], op=ALU.add),
                         reads=[("ot", tt % 2), ("xt7", tt % 2)], writes=[("ot", tt % 2)])
                    final.append(p.dma("sp", y[tt * 128:(tt + 1) * 128, :], o[:], reads=[("ot", tt % 2)]))
                return finish()


def own_blocks(ty):
    return [2 * j + ty if j < 4 else 2 * j + 1 - ty for j in range(8)]


def _pool_mats(first_is_block0):
    wins = (2, 4, 8, 16)
    Mm = np.zeros((128, 2, 4, 128), np.float32)
    for var in range(2):
        blk0 = (var == 0 and first_is_block0)
        for g, w in enumerate(wins):
            for t in range(128):
                cnt = min(t + 1, w) if blk0 else w
                lo = max(t + 1 - w, 0)
                Mm[lo:t + 1, var, g, t] = 1.0 / cnt
                Mm[t, var, g, t] -= 1.0
    Hf = np.zeros((128, 8, 4, 16), np.float32)
    for j in range(8):
        if j == 0 and first_is_block0:
            continue
        for g, w in enumerate(wins):
            for t in range(16):
                for r in range(16):
                    off = r - 16
                    if t - w + 1 <= off:
                        Hf[16 * j + r, j, g, t] = 1.0 / w
    return Mm, Hf


def host_inputs(inputs):
    x = np.asarray(inputs["x"], np.float32)
    c = np.asarray(inputs["c"], np.float32)
    f = lambda k: np.ascontiguousarray(np.asarray(inputs[k], np.float32)[0])
    shared = {
        "w_ada": f("w_ada"), "b_ada": f("b_ada")[None, :],
        "g_post_mix": f("g_post_mix")[None, :], "g_post_ffn": f("g_post_ffn")[None, :],
        "w_in": f("w_in"), "w_pool": f("w_pool"), "pool_scale": f("pool_scale")[None, :],
        "g_pool_out": f("g_pool_out")[None, :], "g_attn_out": f("g_attn_out")[None, :],
        "w_out": f("w_out"), "w_ff1": f("w_ff1"), "w_ff2": f("w_ff2"),
        "I4": np.ascontiguousarray(np.tile(np.eye(128, dtype=np.float32), (1, 4))),
        "pw2": np.ascontiguousarray(np.tile((0.5 ** np.arange(1, 33, dtype=np.float64)).astype(np.float32)[None, :], (128, 1))),
    }
    gpre1 = f("g_pre_mix").reshape(16, 128).T
    gpre2 = f("g_pre_ffn").reshape(16, 128).T
    shared["vcols"] = np.ascontiguousarray(np.concatenate([gpre1, gpre2], axis=1))
    in_maps = []
    for core in range(NCORES):
        b, ty = core // 2, core % 2
        own = own_blocks(ty)
        oth = own_blocks(1 - ty)
        xb = x[b].reshape(16, 128, D)
        m = dict(shared)
        m["x_own"] = np.ascontiguousarray(xb[own].reshape(1024, D))
        m["x_oth"] = np.ascontiguousarray(xb[oth].reshape(1024, D))
        halo = np.zeros((128, D), np.float32)
        for j, blk in enumerate(own):
            if blk > 0:
                halo[16 * j:16 * j + 16] = x[b, blk * 128 - 16:blk * 128]
        m["x_halo"] = halo
        m["c_col"] = np.ascontiguousarray(c[b].reshape(16, 128).T)
        kpos = np.zeros(S, np.float64)
        for j in range(8):
            kpos[(2 * j) * 128:(2 * j + 1) * 128] = own[j] * 128 + np.arange(128)
            kpos[(2 * j + 1) * 128:(2 * j + 2) * 128] = oth[j] * 128 + np.arange(128)
        m["nkB"] = np.ascontiguousarray(np.tile((-kpos * BIG).astype(np.float32)[None, :], (128, 1)))
        qpos = np.array(own, np.float64)[None, :] * 128 + np.arange(128)[:, None]
        m["qB"] = np.ascontiguousarray(((qpos + 0.5) * BIG).astype(np.float32))
        Mm, Hf = _pool_mats(own[0] == 0)
        m["Mm"] = np.ascontiguousarray(Mm.reshape(128, -1))
        m["Hf"] = np.ascontiguousarray(Hf.reshape(128, -1))
        in_maps.append(m)
    return in_maps


_NC_CACHE = {}


def kernel(**inputs):
    in_maps = host_inputs(inputs)
    if "nc" not in _NC_CACHE:
        _NC_CACHE["nc"] = build_program()
    nc = _NC_CACHE["nc"]
    res = run_bass_kernel_spmd(nc, in_maps, core_ids=list(range(NCORES)))
    out = np.zeros((4, 16, 128, D), np.float32)
    for core in range(NCORES):
        b, ty = core // 2, core % 2
        yc = np.asarray(res.results[core]["y"], np.float32).reshape(8, 128, D)
        for j, blk in enumerate(own_blocks(ty)):
            out[b, blk] = yc[j]
    return out.reshape(4, S, D)
```

```python
import bisect
from contextlib import ExitStack

import numpy as np
import concourse.bass as bass
import concourse.mybir as mybir
from concourse.bass_utils import run_bass_kernel_spmd

F32 = mybir.dt.float32
BF16 = mybir.dt.bfloat16
ALU = mybir.AluOpType
AF = mybir.ActivationFunctionType
AX = mybir.AxisListType

D = 2048
S = 2048
DFF = 8192
IN_W = 4752
NCORES = 8
NIT = 24
TOPK = 256.0
EPS = 1e-6
MASKV = -30000.0
BIG = 1e30
IDX_SCALE = (16 ** -0.5) * (128 ** -0.5)
ATT_SCALE = 128 ** -0.5
NDSEM = 24
NRING = 2


class Prog:
    ENG = ("pe", "act", "dve", "pool", "sp")

    def __init__(self, nc):
        self.nc = nc
        self.ops = {e: [] for e in self.ENG}
        self.sig_seq = {e: [] for e in self.ENG}
        self.nseq = {e: 0 for e in self.ENG}
        self.wr = {}
        self.rd = {}
        self.dslot_next = 0
        self.dslot_val = [0] * NDSEM
        self.floor = []

    def _deps(self, eng, reads, writes):
        deps = list(self.floor)
        for k in reads:
            deps.extend(self.wr.get(k, {}).values())
        for k in writes:
            deps.extend(self.wr.get(k, {}).values())
            deps.extend(self.rd.get(k, {}).values())
        return [t for t in deps if not (t[0] == "c" and t[1] == eng and eng == "pe")]

    def _record(self, tok, who, reads, writes):
        for k in reads:
            self.rd.setdefault(k, {})[who] = tok
        for k in writes:
            self.wr.setdefault(k, {})[who] = tok

    def op(self, eng, fn, reads=(), writes=(), signal=True):
        deps = self._deps(eng, reads, writes)
        seq = self.nseq[eng]
        self.nseq[eng] += 1
        if signal:
            self.sig_seq[eng].append(seq)
        tok = ("c", eng, seq)
        self.ops[eng].append(dict(kind="c", fn=fn, deps=deps, seq=seq, signal=signal))
        self._record(tok, eng, reads, writes)
        return tok

    def dma(self, eng, out, in_, reads=(), writes=(), **kw):
        deps = self._deps(eng, reads, writes)
        slot = self.dslot_next
        self.dslot_next = (slot + 1) % NDSEM
        prev = self.dslot_val[slot]
        self.dslot_val[slot] = prev + 16
        tok = ("d", slot, prev + 16)
        if prev:
            deps.append(("d", slot, prev))
        self.ops[eng].append(dict(kind="d", out=out, in_=in_, deps=deps, slot=slot, kw=kw,
                                  seq=self.nseq[eng]))
        self._record(tok, "dma%d" % slot, reads, writes)
        return tok

    def barrier(self):
        fl = []
        for e in ("pe", "act", "dve", "pool"):
            if self.nseq[e]:
                assert self.sig_seq[e] and self.sig_seq[e][-1] == self.nseq[e] - 1, e
                fl.append(("c", e, self.nseq[e] - 1))
        for s in range(NDSEM):
            if self.dslot_val[s]:
                fl.append(("d", s, self.dslot_val[s]))
        self.floor = fl

    def emit(self, final_tokens=()):
        nc = self.nc
        with ExitStack() as es:
            csem = {e: es.enter_context(nc.semaphore("s_" + e)) for e in self.ENG if e != "sp"}
            dsem = [es.enter_context(nc.semaphore("d%d" % i)) for i in range(NDSEM)]
            block = es.enter_context(nc.Block())
            sig_seq = self.sig_seq

            def resolve(tok):
                if tok[0] == "d":
                    return dsem[tok[1]], tok[2]
                _, e, seq = tok
                i = bisect.bisect_left(sig_seq[e], seq)
                assert i < len(sig_seq[e]), ("no signalling op after", tok)
                return csem[e], i + 1

            def run(ename):
                def body(engine):
                    waited = {}
                    for o in self.ops[ename]:
                        for t in o["deps"]:
                            if t[0] == "c" and t[1] == ename:
                                i = bisect.bisect_left(sig_seq[ename], t[2])
                                assert i < len(sig_seq[ename]) and sig_seq[ename][i] < o["seq"], \
                                    ("same-engine dep on unsignalled op", ename, t)
                            sem, val = resolve(t)
                            if waited.get(id(sem), 0) >= val:
                                continue
                            waited[id(sem)] = val
                            engine.wait_ge(sem, val)
                        if o["kind"] == "c":
                            ins = o["fn"](engine)
                            if o["signal"]:
                                ins.then_inc(csem[ename], 1)
                        else:
                            engine.dma_start(out=o["out"], in_=o["in_"], **o["kw"]).then_inc(
                                dsem[o["slot"]], 16)
                    if ename == "sp":
                        for t in final_tokens:
                            sem, val = resolve(t)
                            engine.wait_ge(sem, val)
                return body

            block.tensor(run("pe"))
            block.scalar(run("act"))
            block.vector(run("dve"))
            block.gpsimd(run("pool"))
            block.sync(run("sp"))


def build_program(debug=False, stage=None):
    nc = bass.Bass("TRN2", target_bir_lowering=False)
    dt_in = lambda name, shape: nc.dram_tensor(name, shape, F32, kind="ExternalInput").ap()
    x_own = dt_in("x_own", [1024, D])
    x_oth = dt_in("x_oth", [1024, D])
    x_halo = dt_in("x_halo", [128, D])
    c_col = dt_in("c_col", [128, 16])
    vcols = dt_in("vcols", [128, 32])
    nkB_d = dt_in("nkB", [128, S])
    qB_d = dt_in("qB", [128, 8])
    pw2_d = dt_in("pw2", [128, 32])
    Mm_d = dt_in("Mm", [128, 2 * 4 * 128])
    Hf_d = dt_in("Hf", [128, 8 * 4 * 16])
    I4_d = dt_in("I4", [128, 512])
    w_ada = dt_in("w_ada", [D, 6 * D])
    b_ada = dt_in("b_ada", [1, 6 * D])
    g_post_mix = dt_in("g_post_mix", [1, D])
    g_post_ffn = dt_in("g_post_ffn", [1, D])
    w_in = dt_in("w_in", [D, IN_W])
    w_pool = dt_in("w_pool", [4, 256, 256])
    pool_scale = dt_in("pool_scale", [1, 1024])
    g_pool_out = dt_in("g_pool_out", [1, 1024])
    g_attn_out = dt_in("g_attn_out", [1, 1024])
    w_out = dt_in("w_out", [D, D])
    w_ff1 = dt_in("w_ff1", [D, DFF])
    w_ff2 = dt_in("w_ff2", [DFF, D])
    y = nc.dram_tensor("y", [1024, D], F32, kind="ExternalOutput").ap()
    modD = nc.dram_tensor("modD", [1, 6 * D], F32).ap()
    x1D = nc.dram_tensor("x1D", [1024, D], F32).ap()
    dbg = {}
    if debug:
        for nm, shp in debug.items():
            dbg[nm] = nc.dram_tensor("dbg_" + nm, shp, F32, kind="ExternalOutput").ap()

    p = Prog(nc)
    final = []

    def finish():
        p.emit(final_tokens=final)
        return nc

    ARENA_BYTES = 210944
    arena_cm = nc.sbuf_tensor("arena", [128, ARENA_BYTES // 2], BF16)
    arena_t = arena_cm.__enter__()
    astate = dict(off=0, peak=0)

    def T(es, name, shape, dt):
        n = 1
        for s_ in shape[1:]:
            n *= s_
        nb = n * (4 if dt == F32 else 2)
        off = (astate["off"] + 63) // 64 * 64
        assert off + nb <= ARENA_BYTES, ("SBUF arena overflow", name, off + nb)
        astate["off"] = off + nb
        astate["peak"] = max(astate["peak"], off + nb)
        ap = arena_t[:, off // 2:(off + nb) // 2]
        if dt == F32:
            ap = ap.bitcast(F32)
        if len(shape) == 3:
            ap = ap.rearrange("p (a b) -> p a b", a=shape[1])
        elif len(shape) == 4:
            ap = ap.rearrange("p (a b c) -> p a b c", a=shape[1], b=shape[2])
        return ap

    def scope():
        es = ExitStack()
        m = astate["off"]

        def rel():
            astate["off"] = m
        es.callback(rel)
        return es

    def bank(b, n=1):
        return ["pb%d" % i for i in range(b, b + n)]

    with ExitStack() as G:
        ps = G.enter_context(nc.psum_tensor("ps", [128, 8, 512], F32))
        psf = ps[:].rearrange("p a b -> p (a b)")
        psb = ps[:].bitcast(BF16).rearrange("p a b -> p (a b)")
        ring = [T(G, "ring%d" % i, [128, 16, 512], BF16) for i in range(NRING)]
        st = T(G, "st", [128, 256], F32)
        cols = T(G, "cols", [128, 96], F32)
        identb = T(G, "identb", [128, 128], BF16)
        p.op("dve", lambda e: e.memset(st[:], 0.0), writes=["st"])
        p.dma("sp", cols[:, 0:32], vcols[:, :], writes=["cols_g"])
        p.dma("pool", identb[:], I4_d[:, 0:128], writes=["identb"])

        sched = []
        for cg in range(24):
            sched.append(("ada", w_ada[:, cg * 512:(cg + 1) * 512], 512))
        sched.append(("inA", w_in[:, 2048:2560], 512))
        sched.append(("inB", w_in[:, 4608:4752], 144))
        for i in range(2):
            sched.append(("q", w_in[:, 1024 + 512 * i:1536 + 512 * i], 512))
        for i in range(4):
            sched.append(("iq", w_in[:, 2560 + 512 * i:3072 + 512 * i], 512))
        for i in range(2):
            sched.append(("pool", w_in[:, 512 * i:512 * (i + 1)], 512))
        for cg in range(4):
            sched.append(("wout", w_out[:, cg * 512:(cg + 1) * 512], 512))
        for fg in range(16):
            sched.append(("ff1", w_ff1[:, fg * 512:(fg + 1) * 512], 512))
        for cg in range(4):
            for jg in range(4):
                sched.append(("ff2", w_ff2[jg * 2048:(jg + 1) * 2048, cg * 512:(cg + 1) * 512], 512))
        rstate = dict(loaded=0, used=0)

        def ring_load():
            i = rstate["loaded"]
            if i >= len(sched):
                return
            name, src, ncol = sched[i]
            buf = ring[i % NRING]
            p.dma("pool", buf[:, :, 0:ncol], src.rearrange("(k p) c -> p k c", p=128),
                  writes=[("ring", i % NRING)])
            rstate["loaded"] += 1

        def ring_get(name):
            i = rstate["used"]
            assert sched[i][0] == name, (sched[i][0], name)
            while rstate["loaded"] <= i:
                ring_load()
            return ring[i % NRING], ("ring", i % NRING)

        def ring_done():
            rstate["used"] += 1
            ring_load()

        for _ in range(NRING):
            ring_load()

        def mm(out, lhsT, rhs, start, stop, reads, writes, signal):
            p.op("pe", lambda e: e.matmul(out, lhsT=lhsT, rhs=rhs, start=start, stop=stop),
                 reads=reads, writes=writes, signal=signal)

        def rstd_batch(ss_ap, out_ap, n, key_in, key_out, tmp_ap):
            p.op("dve", lambda e: e.tensor_scalar(out=tmp_ap, in0=ss_ap, scalar1=1.0 / n, scalar2=EPS,
                                                   op0=ALU.mult, op1=ALU.add),
                 reads=(key_in if isinstance(key_in, list) else [key_in]), writes=[key_out + "_t"])
            p.op("act", lambda e: e.activation(out=tmp_ap, in_=tmp_ap, func=AF.Sqrt),
                 reads=[key_out + "_t"], writes=[key_out + "_t"])
            p.op("dve", lambda e: e.reciprocal(out=out_ap, in_=tmp_ap),
                 reads=[key_out + "_t"], writes=[key_out])

        def norm_transpose_pair(srcs, xt, xnb, dst, gcol, scol, tagbase, pbase, rstd_known=None):
            n = len(srcs)
            pv = psb[:, pbase * 1024:(pbase + 4) * 1024].rearrange("p (k t) -> p k t", k=16)
            for i, (kind, src, sc) in enumerate(srcs):
                if kind == "dram":
                    xa = xt[i]
                    xkey = ("xt", i)
                    p.dma("sp", xa[:], src, writes=[xkey])
                    xin = xa[:]
                else:
                    xin, xkey = src
                nb = xnb[i]
                nkey = ("xnb", i)
                if rstd_known is None:
                    p.op("act", lambda e, xin=xin, nb=nb, sc=sc: e.activation(
                        out=nb[:], in_=xin, func=AF.Square, accum_out=st[:, sc:sc + 1]),
                        reads=[xkey], writes=[nkey, ("st", sc)])
                    rstd_batch(st[:, sc:sc + 1], st[:, sc + 32:sc + 33], D, ("st", sc), "rs%s%d" % (tagbase, sc),
                               st[:, sc + 64:sc + 65])
                    rkey = "rs%s%d" % (tagbase, sc)
                    rap = st[:, sc + 32:sc + 33]
                else:
                    rap, rkey = rstd_known[i]
                p.op("dve", lambda e, xin=xin, nb=nb, rap=rap: e.tensor_scalar(
                    out=nb[:], in0=xin, scalar1=rap, scalar2=None, op0=ALU.mult),
                    reads=[xkey, rkey], writes=[nkey])
                for k in range(16):
                    p.op("pe", lambda e, k=k, i=i, nb=nb: e.transpose(
                        out=pv[:, k, i * 128:(i + 1) * 128], in_=nb[:, k * 128:(k + 1) * 128], identity=identb[:]),
                        reads=[nkey, "identb"], writes=bank(pbase, 4), signal=(k == 15))
            w = n * 128
            for k in range(16):
                if (k // 4) % 2 == 0:
                    p.op("act", lambda e, k=k: e.activation(
                        out=dst[:, k, 0:w], in_=pv[:, k, 0:w], func=AF.Identity,
                        scale=cols[:, gcol + k:gcol + k + 1], bias=cols[:, scol + k:scol + k + 1]),
                        reads=bank(pbase, 4) + ["cols_m"], writes=[dst_key(dst)])
                else:
                    p.op("dve", lambda e, k=k: e.tensor_scalar(
                        out=dst[:, k, 0:w], in0=pv[:, k, 0:w], scalar1=cols[:, gcol + k:gcol + k + 1],
                        scalar2=cols[:, scol + k:scol + k + 1], op0=ALU.mult, op1=ALU.add),
                        reads=bank(pbase, 4) + ["cols_m"], writes=[dst_key(dst)])

        dkeys = {}

        def dst_key(ap):
            return dkeys[id(ap)]

        def dbg_dump(name, ap_sb, key):
            if debug and name in dbg:
                final.append(p.dma("pool", dbg[name], ap_sb, reads=[key]))

        with scope() as P0:
            modrow = T(P0, "modrow", [1, 6 * D], F32)[0:1, :]
            brow = T(P0, "brow", [1, 6 * D], F32)[0:1, :]
            ctmp = T(P0, "ctmp", [128, 32], F32)
            caT = T(P0, "caT", [128, 16], BF16)
            p.dma("sp", ctmp[:, 0:16], c_col[:, :], writes=["ccol"])
            p.dma("sp", brow[:], b_ada[:, :], writes=["brow"])
            p.op("act", lambda e: e.activation(out=ctmp[:, 16:32], in_=ctmp[:, 0:16], func=AF.Exp, scale=-1.0),
                 reads=["ccol"], writes=["cexp"])
            p.op("dve", lambda e: e.tensor_scalar(out=ctmp[:, 16:32], in0=ctmp[:, 16:32], scalar1=1.0, scalar2=None,
                                                   op0=ALU.add), reads=["cexp"], writes=["cexp"])
            p.op("dve", lambda e: e.reciprocal(out=ctmp[:, 16:32], in_=ctmp[:, 16:32]), reads=["cexp"], writes=["cexp"])
            p.op("dve", lambda e: e.tensor_tensor(out=caT[:], in0=ctmp[:, 0:16], in1=ctmp[:, 16:32], op=ALU.mult),
                 reads=["cexp", "ccol"], writes=["caT"])
            for cg in range(24):
                W, wkey = ring_get("ada")
                b = cg % 2
                for k in range(16):
                    mm(ps[0:1, b, :], caT[:, k:k + 1], W[:, k, :], k == 0, k == 15,
                       [wkey, "caT"], bank(b), k == 15)
                ring_done()
                p.op("dve", lambda e, b=b, cg=cg: e.tensor_tensor(
                    out=modrow[0:1, cg * 512:(cg + 1) * 512], in0=ps[0:1, b, :],
                    in1=brow[0:1, cg * 512:(cg + 1) * 512], op=ALU.add),
                    reads=bank(b) + ["brow"], writes=["modrow"])
            p.dma("sp", modD[:, :], modrow[:], reads=["modrow"], writes=["modD"])
            def colload(dstc, off, key):
                p.dma("sp", cols[:, dstc:dstc + 16],
                      modD[0, off:off + D].rearrange("(k p) -> p k", p=128),
                      reads=["modD"], writes=[key], allow_slow_non_contiguous=True)
            colload(48, 0, "c_sh1")
            colload(32, D, "c_sc1")
            colload(80, 3 * D, "c_sh2")
            colload(64, 4 * D, "c_sc2")
            p.op("dve", lambda e: e.scalar_tensor_tensor(out=cols[:, 32:48], in0=cols[:, 32:48], scalar=1.0,
                                                          in1=cols[:, 0:16], op0=ALU.add, op1=ALU.mult),
                 reads=["c_sc1", "cols_g"], writes=["c_sc1"])
            p.op("dve", lambda e: e.scalar_tensor_tensor(out=cols[:, 64:80], in0=cols[:, 64:80], scalar=1.0,
                                                          in1=cols[:, 16:32], op0=ALU.add, op1=ALU.mult),
                 reads=["c_sc2", "cols_g", "c_sh1", "c_sh2"], writes=["c_sc2", "cols_m"])
            if debug:
                dbg_dump("cols", cols[:], "cols_m")
            p.barrier()
            if stage == "p0":
                return finish()

        with scope() as S1:
            big16 = T(S1, "big16", [128, 16, 1024], BF16)
            dkeys[id(big16)] = "big16"
            with scope() as S2:
                KT = T(S2, "KT", [128, 2, S], BF16)
                Vaug = T(S2, "Vaug", [128, 16, 2, 129], BF16)
                kiT = T(S2, "kiT", [128, S], BF16)
                wq = T(S2, "wq", [128, 8, 16], F32)
                with scope() as S4:
                    qT = T(S4, "qT", [128, 8, 8, 128], BF16)
                    u = T(S4, "u", [128, 8, 1024], BF16)
                    uh = T(S4, "uh", [128, 1024], BF16)
                    with scope() as S3:
                        hT_own = T(S3, "hT_own", [128, 16, 1024], BF16)
                        hT_halo = T(S3, "hT_halo", [128, 16, 128], BF16)
                        dkeys[id(hT_own)] = "hT_own"
                        dkeys[id(hT_halo)] = "hT_halo"
                        xt = [T(S3, "xt%d" % i, [128, D], F32) for i in range(2)]
                        xnb = [T(S3, "xnb%d" % i, [128, D], BF16) for i in range(2)]
                        grp = 0
                        for which, src, dstT in (("own", x_own, hT_own), ("oth", x_oth, big16)):
                            for j0 in range(0, 8, 2):
                                srcs = [("dram", src[(j0 + i) * 128:(j0 + i + 1) * 128, :],
                                         (0 if which == "own" else 8) + j0 + i) for i in range(2)]
                                dview = dstT[:, :, j0 * 128:(j0 + 2) * 128]
                                dkeys[id(dview)] = dkeys[id(dstT)]
                                norm_transpose_pair(srcs, xt, xnb, dview, 32, 48, "a", 4 * (grp % 2))
                                grp += 1
                        dview = hT_halo[:, :, :]
                        dkeys[id(dview)] = "hT_halo"
                        norm_transpose_pair([("dram", x_halo[:, :], 16)], xt, xnb, dview, 32, 48, "a", 4 * (grp % 2))
                        if debug:
                            dbg_dump("hT_own", hT_own[:], "hT_own")
                            dbg_dump("hT_oth", big16[:], "big16")
                            dbg_dump("hT_halo", hT_halo[:], "hT_halo")
                        if stage == "p1":
                            return finish()
                        WA, keyA = ring_get("inA")
                        ring_done_A = False
                        pb = [0]

                        def nextbank():
                            b = pb[0]
                            pb[0] = (b + 1) % 8
                            return b

                        p.op("dve", lambda e: e.memset(Vaug[:, :, :, 128:129], 1.0), writes=["Vaug"])
                        KTv = KT[:].rearrange("p g (j two t) -> p g j two t", two=2, t=128)
                        kiTv = kiT[:].rearrange("p (j two t) -> p j two t", two=2, t=128)
                        cpy = [0]

                        def evac_copy(out, in_, reads, writes):
                            cpy[0] += 1
                            if cpy[0] % 2:
                                p.op("act", lambda e: e.activation(out=out, in_=in_, func=AF.Copy),
                                     reads=reads, writes=writes)
                            else:
                                p.op("dve", lambda e: e.tensor_copy(out=out, in_=in_), reads=reads, writes=writes)

                        for par, hsrc, hkey in ((0, hT_own, "hT_own"), (1, big16, "big16")):
                            for tg in range(2):
                                for c in range(2):
                                    b = nextbank()
                                    for k in range(16):
                                        mm(ps[:, b, :], WA[:, k, c * 128:(c + 1) * 128], hsrc[:, k, tg * 512:(tg + 1) * 512],
                                           k == 0, k == 15, [keyA, hkey], bank(b), k == 15)
                                    evac_copy(KTv[:, c, tg * 4:(tg + 1) * 4, par, :],
                                              ps[:, b, :].rearrange("p (j t) -> p j t", t=128), bank(b), ["KT"])
                                for tt in range(4):
                                    jt = tg * 4 + tt
                                    b = nextbank()
                                    for k in range(16):
                                        mm(ps[:, b, 0:256], hsrc[:, k, jt * 128:(jt + 1) * 128], WA[:, k, 256:512],
                                           k == 0, k == 15, [keyA, hkey], bank(b), k == 15)
                                    evac_copy(Vaug[:, 2 * jt + par, :, 0:128],
                                              ps[:, b, 0:256].rearrange("p (g d) -> p g d", d=128), bank(b), ["Vaug"])
                        ring_done()
                        WB, keyB = ring_get("inB")
                        for par, hsrc, hkey in ((0, hT_own, "hT_own"), (1, big16, "big16")):
                            for tg in range(2):
                                b = nextbank()
                                for k in range(16):
                                    mm(ps[:, b, :], WB[:, k, 0:128], hsrc[:, k, tg * 512:(tg + 1) * 512],
                                       k == 0, k == 15, [keyB, hkey], bank(b), k == 15)
                                evac_copy(kiTv[:, tg * 4:(tg + 1) * 4, par, :],
                                          ps[:, b, :].rearrange("p (j t) -> p j t", t=128), bank(b), ["kiT"])
                        for j in range(8):
                            b = nextbank()
                            for k in range(16):
                                mm(ps[:, b, 0:16], hT_own[:, k, j * 128:(j + 1) * 128], WB[:, k, 128:144],
                                   k == 0, k == 15, [keyB, "hT_own"], bank(b), k == 15)
                            p.op("dve", lambda e, b=b, j=j: e.tensor_scalar(
                                out=wq[:, j, :], in0=ps[:, b, 0:16], scalar1=IDX_SCALE, scalar2=None, op0=ALU.mult),
                                reads=bank(b), writes=["wq"])
                        ring_done()
                        if debug:
                            dbg_dump("KT", KT[:], "KT")
                            dbg_dump("kiT", kiT[:], "kiT")
                            dbg_dump("Vaug", Vaug[:], "Vaug")
                            dbg_dump("wq", wq[:], "wq")
                        p.barrier()
                        if stage == "p2a":
                            return finish()
                        for i in range(2):
                            W, wkey = ring_get("q")
                            for hh in range(4):
                                h = 4 * i + hh
                                for tg in range(2):
                                    b = nextbank()
                                    for k in range(16):
                                        mm(ps[:, b, :], W[:, k, hh * 128:(hh + 1) * 128], hT_own[:, k, tg * 512:(tg + 1) * 512],
                                           k == 0, k == 15, [wkey, "hT_own"], bank(b), k == 15)
                                    evac_copy(qT[:, tg * 4:(tg + 1) * 4, h, :],
                                              ps[:, b, :].rearrange("p (j t) -> p j t", t=128), bank(b), ["qT"])
                            ring_done()
                        for i in range(4):
                            W, wkey = ring_get("iq")
                            for hh in range(4):
                                h = 4 * i + hh
                                for tg in range(2):
                                    b = nextbank()
                                    for k in range(16):
                                        mm(ps[:, b, :], W[:, k, hh * 128:(hh + 1) * 128], hT_own[:, k, tg * 512:(tg + 1) * 512],
                                           k == 0, k == 15, [wkey, "hT_own"], bank(b), k == 15)
                                    evac_copy(big16[:, h, tg * 512:(tg + 1) * 512], ps[:, b, :], bank(b),
                                              [("b16", tg * 4 + jj) for jj in range(4)])
                            ring_done()
                        for i in range(2):
                            W, wkey = ring_get("pool")
                            for j in range(9):
                                b = nextbank()
                                for k in range(16):
                                    lhsT = hT_own[:, k, j * 128:(j + 1) * 128] if j < 8 else hT_halo[:, k, :]
                                    mm(ps[:, b, :], lhsT, W[:, k, :], k == 0, k == 15,
                                       [wkey, "hT_own", "hT_halo"], bank(b), k == 15)
                                dst = u[:, j, i * 512:(i + 1) * 512] if j < 8 else uh[:, i * 512:(i + 1) * 512]
                                evac_copy(dst, ps[:, b, :], bank(b), ["u"])
                            ring_done()
                        if debug:
                            dbg_dump("qT", qT[:], "qT")
                            dbg_dump("qiT", big16[:], ("b16", 7))
                            dbg_dump("u", u[:], "u")
                            dbg_dump("uh", uh[:], "u")
                        p.barrier()
                        if stage == "p2b":
                            return finish()
                    with scope() as P3:
                        nkB = T(P3, "nkB", [128, S], F32)
                        qB = T(P3, "qB", [128, 8], F32)
                        pw2 = T(P3, "pw2", [128, 32], F32)
                        Mm = T(P3, "Mm", [128, 2, 4, 128], BF16)
                        Hf = T(P3, "Hf", [128, 8, 4, 16], BF16)
                        I4 = T(P3, "I4", [128, 512], BF16)
                        wps = T(P3, "wps", [128, 4, 2, 256], BF16)
                        gb = T(P3, "gb", [128, 2048], F32)
                        p.dma("sp", nkB[:], nkB_d[:, :], writes=["nkB"])
                        p.dma("sp", qB[:], qB_d[:, :], writes=["qB"])
                        p.dma("sp", pw2[:], pw2_d[:, :], writes=["pw2"])
                        p.dma("pool", Mm[:].rearrange("p a g t -> p (a g t)"), Mm_d[:, :], writes=["Mm"])
                        p.dma("pool", Hf[:].rearrange("p a g t -> p (a g t)"), Hf_d[:, :], writes=["Hf"])
                        p.dma("pool", I4[:], I4_d[:, :], writes=["I4"])
                        p.dma("sp", gb[:, 0:1024], g_pool_out[0:1, :].partition_broadcast(128), writes=["gb0"])
                        p.dma("sp", gb[:, 1024:2048], g_attn_out[0:1, :].partition_broadcast(128), writes=["gb1"])
                        with scope() as P3s:
                            wpf = T(P3s, "wpf", [128, 4, 2, 256], F32)
                            psb_t = T(P3s, "psb_t", [128, 1024], F32)
                            p.dma("sp", wpf[:], w_pool.rearrange("g (cc p) d -> p g cc d", p=128), writes=["wpf"])
                            p.dma("sp", psb_t[:], pool_scale[0:1, :].partition_broadcast(128), writes=["psb_t"])
                            for g in range(4):
                                p.op("dve", lambda e, g=g, wpf=wpf, psb_t=psb_t: e.tensor_tensor(
                                    out=wps[:, g, :, :], in0=wpf[:, g, :, :],
                                    in1=psb_t[:, g * 256:(g + 1) * 256].unsqueeze(1).to_broadcast([128, 2, 256]),
                                    op=ALU.mult), reads=["wpf", "psb_t"], writes=["wps"])
                            p.barrier()
                        scoreS = [[T(P3, "scs%d" % s, [128, 1024], F32), T(P3, "scb%d" % s, [128, S], F32)] for s in range(2)]
                        maskS = [[T(P3, "mks%d" % s, [128, 1024], BF16), T(P3, "mkb%d" % s, [128, S], BF16)] for s in range(2)]
                        bsS = [[T(P3, "bs%d_%d" % (s, i), [128, 64], F32) for i in range(2)] for s in range(2)]
                        Rb = [T(P3, "Rb%d" % i, [128, 1024], F32) for i in range(2)]
                        PT = [T(P3, "PT%d" % i, [128, 512], BF16) for i in range(2)]
                        attn32 = T(P3, "attn32", [128, 1024], F32)
                        mg = T(P3, "mg", [128, 2048], BF16)
                        pooledT = T(P3, "pooledT", [128, 8, 128], BF16)
                        rcT = T(P3, "rcT", [128, 8], F32)
                        cnt_ci = [0]
                        cnt_ui = [0]

                        def stage_A(j, score, bs, tag):
                            L = 2 * j + 2
                            N = 128 * L
                            skey = ("score", tag)
                            chunks = [(c0, min(1024, N - c0)) for c0 in range(0, N, 1024)]
                            for h in range(16):
                                for (c0, cw) in chunks:
                                    ci = cnt_ci[0]
                                    cnt_ci[0] += 1
                                    bb = 2 * (ci % 2)
                                    rb = Rb[ci % 2]
                                    rkey = ("Rb", ci % 2)
                                    nmm = (cw + 511) // 512
                                    for m in range(nmm):
                                        w = min(512, cw - m * 512)
                                        mm(ps[:, bb + m, 0:w], big16[:, h, j * 128:(j + 1) * 128],
                                           kiT[:, c0 + m * 512:c0 + m * 512 + w], True, True,
                                           [("b16", j), "kiT"], bank(bb + m), m == nmm - 1)
                                    pin = psf[:, bb * 512:bb * 512 + cw]
                                    p.op("act", lambda e, rb=rb, pin=pin, cw=cw: e.activation(
                                        out=rb[:, 0:cw], in_=pin, func=AF.Relu),
                                        reads=bank(bb, nmm), writes=[rkey])
                                    if h == 0:
                                        p.op("dve", lambda e, rb=rb, c0=c0, cw=cw: e.tensor_scalar(
                                            out=score[:, c0:c0 + cw], in0=rb[:, 0:cw], scalar1=wq[:, j, 0:1],
                                            scalar2=None, op0=ALU.mult), reads=[rkey, "wq"], writes=[skey])
                                    else:
                                        p.op("dve", lambda e, rb=rb, c0=c0, cw=cw, h=h: e.scalar_tensor_tensor(
                                            out=score[:, c0:c0 + cw], in0=rb[:, 0:cw], scalar=wq[:, j, h:h + 1],
                                            in1=score[:, c0:c0 + cw], op0=ALU.mult, op1=ALU.add),
                                            reads=[rkey, "wq", skey], writes=[skey])
                                    yield
                            if debug and j == 1:
                                dbg_dump("score1", score[:, 0:512], skey)
                            p.op("dve", lambda e: e.tensor_reduce(out=bs[:, 0:1], in_=score[:, 0:N], axis=AX.X, op=ALU.max),
                                 reads=[skey], writes=[("bsM", tag)])
                            yield
                            p.op("dve", lambda e: e.tensor_reduce(out=bs[:, 1:2], in_=score[:, 0:N], axis=AX.X, op=ALU.min),
                                 reads=[skey], writes=[("bsm", tag)])
                            yield
                            p.op("dve", lambda e: e.scalar_tensor_tensor(
                                out=score[:, 0:N], in0=nkB[:, 0:N], scalar=qB[:, j:j + 1], in1=score[:, 0:N],
                                op0=ALU.add, op1=ALU.min), reads=["nkB", "qB", skey], writes=[skey])
                            yield
                            p.op("dve", lambda e: e.tensor_scalar(out=bs[:, 2:3], in0=bs[:, 1:2], scalar1=-1.0, scalar2=None,
                                                                   op0=ALU.add), reads=[("bsm", tag)], writes=[("bslo", tag)])
                            yield
                            p.op("dve", lambda e: e.tensor_tensor(out=bs[:, 3:4], in0=bs[:, 0:1], in1=bs[:, 2:3], op=ALU.subtract),
                                 reads=[("bsM", tag), ("bslo", tag)], writes=[("bsW", tag)])
                            yield
                            p.op("dve", lambda e: e.tensor_scalar(out=bs[:, 32:64], in0=pw2[:, :], scalar1=bs[:, 3:4], scalar2=None,
                                                                   op0=ALU.mult), reads=[("bsW", tag), "pw2"], writes=[("bswd", tag)])
                            yield
                            p.op("dve", lambda e: e.tensor_tensor(out=bs[:, 4:5], in0=bs[:, 2:3], in1=bs[:, 32:33], op=ALU.add),
                                 reads=[("bslo", tag), ("bswd", tag)], writes=[("bsmid", tag)])
                            yield

                        def stage_B(j, score, maskb, bs, tag):
                            N = 128 * (2 * j + 2)
                            skey = ("score", tag)
                            mkey = ("maskb", tag)
                            for it in range(NIT):
                                p.op("dve", lambda e: e.tensor_scalar(
                                    out=maskb[:, 0:N], in0=score[:, 0:N], scalar1=bs[:, 4:5], scalar2=None,
                                    op0=ALU.is_ge, op1=ALU.add, accum_out=bs[:, 5:6]),
                                    reads=[skey, ("bsmid", tag), mkey], writes=[mkey, ("bscnt", tag)])
                                yield
                                last = it == NIT - 1
                                p.op("dve", lambda e, last=last: e.tensor_scalar(
                                    out=bs[:, 6:7], in0=bs[:, 5:6], scalar1=TOPK, scalar2=(-1.0 if last else -0.5),
                                    op0=ALU.is_ge, op1=ALU.add), reads=[("bscnt", tag)], writes=[("bsge", tag)])
                                yield
                                p.op("dve", lambda e, it=it: e.scalar_tensor_tensor(
                                    out=bs[:, 4:5], in0=bs[:, 6:7], scalar=bs[:, 32 + it:33 + it], in1=bs[:, 4:5],
                                    op0=ALU.mult, op1=ALU.add),
                                    reads=[("bsge", tag), ("bswd", tag), ("bsmid", tag)], writes=[("bsmid", tag)])
                                yield
                            if debug and j == 1:
                                dbg_dump("tau1", bs[:, 0:8], ("bsmid", tag))
                            p.op("dve", lambda e: e.tensor_scalar(
                                out=maskb[:, 0:N], in0=score[:, 0:N], scalar1=bs[:, 4:5], scalar2=MASKV,
                                op0=ALU.is_lt, op1=ALU.mult), reads=[skey, ("bsmid", tag), mkey], writes=[mkey])
                            yield

                        def oacc(h, n=129):
                            return ps[:, 5 + h // 3, (h % 3) * 129:(h % 3) * 129 + n]

                        def stage_C(j, maskb, tag):
                            L = 2 * j + 2
                            mkey = ("maskb", tag)
                            for kt in range(L):
                                for g in range(2):
                                    mm(ps[:, 4, :], KT[:, g, kt * 128:(kt + 1) * 128],
                                       qT[:, j, 4 * g:4 * g + 4, :], True, False,
                                       ["KT", "qT"], bank(4), False)
                                    mm(ps[:, 4, :], maskb[:, kt * 128:(kt + 1) * 128], I4[:, :], False, True,
                                       [mkey, "I4"], bank(4), True)
                                    ui = cnt_ui[0]
                                    cnt_ui[0] += 1
                                    pt = PT[ui % 2]
                                    pkey = ("PT", ui % 2)
                                    p.op("act", lambda e, pt=pt: e.activation(out=pt[:], in_=ps[:, 4, :], func=AF.Exp,
                                                                              scale=ATT_SCALE),
                                         reads=bank(4), writes=[pkey])
                                    for hh in range(4):
                                        h = 4 * g + hh
                                        lastmm = (kt == L - 1) and (g == 1) and (hh == 3)
                                        mm(oacc(h), pt[:, hh * 128:(hh + 1) * 128], Vaug[:, kt, g, :],
                                           kt == 0 and h % 3 == 0, kt == L - 1, [pkey, "Vaug"], bank(5, 3),
                                           lastmm or hh == 3)
                                    yield
                            for bk in range(3):
                                nh = 3 if bk < 2 else 2
                                den = ps[:, 5 + bk, 0:nh * 129].rearrange("p (h c) -> p h c", c=129)
                                p.op("dve", lambda e, den=den, bk=bk, nh=nh: e.reciprocal(
                                    out=rcT[:, 3 * bk:3 * bk + nh], in_=den[:, :, 128]),
                                    reads=bank(5, 3), writes=[("rc", bk)])
                                yield
                                p.op("dve", lambda e, den=den, bk=bk, nh=nh: e.tensor_tensor(
                                    out=attn32[:, 384 * bk:384 * bk + 128 * nh].rearrange("p (h d) -> p h d", d=128),
                                    in0=den[:, :, 0:128],
                                    in1=rcT[:, 3 * bk:3 * bk + nh].unsqueeze(2).to_broadcast([128, nh, 128]),
                                    op=ALU.mult), reads=bank(5, 3) + [("rc", bk)], writes=["attn32"])
                                yield
                            p.op("act", lambda e: e.activation(out=mg[:, 1024:2048], in_=attn32[:], func=AF.Square,
                                                               accum_out=st[:, 128 + j:129 + j]),
                                 reads=["attn32"], writes=["mg1", ("st", 128 + j)])
                            p.op("dve", lambda e: e.tensor_tensor(out=mg[:, 1024:2048], in0=attn32[:], in1=gb[:, 1024:2048],
                                                                  op=ALU.mult), reads=["attn32", "gb1"], writes=["mg1"])
                            yield
                            if debug and j == 1:
                                dbg_dump("attn1", attn32[:], "attn32")
                            var = 0 if j == 0 else 1
                            psP = psf[:, 2048:3072].rearrange("p (c t) -> p c t", t=128)
                            for cch in range(8):
                                g = cch // 2
                                mm(psP[:, cch, :], u[:, j, cch * 128:(cch + 1) * 128], Mm[:, var, g, :], True, False,
                                   ["u", "Mm"], bank(4, 2), False)
                                mm(psP[:, cch, 0:16], uh[:, cch * 128:(cch + 1) * 128], Hf[:, j, g, :], False, True,
                                   ["u", "Hf"], bank(4, 2), cch == 7)
                            p.op("act", lambda e: e.activation(out=pooledT[:].rearrange("p c t -> p (c t)"), in_=psf[:, 2048:3072],
                                                               func=AF.Copy), reads=bank(4, 2), writes=["pooledT"])
                            for g in range(4):
                                for cc in range(2):
                                    mm(psf[:, 3072 + g * 256:3072 + (g + 1) * 256], pooledT[:, 2 * g + cc, :], wps[:, g, cc, :],
                                       cc == 0, cc == 1, ["pooledT", "wps"], bank(6, 2), (g == 3 and cc == 1))
                            p.op("act", lambda e: e.activation(out=mg[:, 0:1024], in_=psf[:, 3072:4096], func=AF.Square,
                                                               accum_out=st[:, 136 + j:137 + j]),
                                 reads=bank(6, 2), writes=["mg0", ("st", 136 + j)])
                            p.op("dve", lambda e: e.tensor_tensor(out=mg[:, 0:1024], in0=psf[:, 3072:4096], in1=gb[:, 0:1024],
                                                                  op=ALU.mult), reads=bank(6, 2) + ["gb0"], writes=["mg0"])
                            yield
                            pv = psb[:, 4096:6144].rearrange("p (c t) -> p c t", t=128)
                            for c in range(16):
                                p.op("pe", lambda e, c=c: e.transpose(out=pv[:, c, :], in_=mg[:, c * 128:(c + 1) * 128],
                                                                      identity=identb[:]),
                                     reads=["mg0", "mg1", "identb"], writes=bank(4, 2), signal=(c == 15))
                            p.op("act", lambda e: e.activation(out=big16[:, :, j * 128:(j + 1) * 128], in_=pv,
                                                               func=AF.Copy),
                                 reads=bank(4, 2), writes=[("b16", j)])
                            yield

                        def run_rr(gens):
                            gens = [g for g in gens if g is not None]
                            while gens:
                                for g in list(gens):
                                    try:
                                        next(g)
                                    except StopIteration:
                                        gens.remove(g)

                        def chain(*gs):
                            for g in gs:
                                yield from g

                        pairs = [(0, 7), (1, 6), (2, 5), (3, 4)]

                        def bufs(q, i):
                            s = q % 2
                            return scoreS[s][i], maskS[s][i], bsS[s][i], (s, i)

                        def A_pair(q):
                            return chain(*[stage_A(pairs[q][i], bufs(q, i)[0], bufs(q, i)[2], bufs(q, i)[3]) for i in range(2)])

                        def C_pair(q):
                            return chain(*[stage_C(pairs[q][i], bufs(q, i)[1], bufs(q, i)[3]) for i in range(2)])

                        def B_one(q, i):
                            sc, mk, bs_, tag = bufs(q, i)
                            return stage_B(pairs[q][i], sc, mk, bs_, tag)

                        run_rr([A_pair(0)])
                        for q in range(4):
                            run_rr([B_one(q, 0), B_one(q, 1),
                                    A_pair(q + 1) if q + 1 < 4 else None,
                                    C_pair(q - 1) if q >= 1 else None])
                        run_rr([C_pair(3)])
                        if debug:
                            dbg_dump("mergedT", big16[:], ("b16", 7))
                            dbg_dump("st", st[:], ("st", 143))
                        p.barrier()
                        if stage == "p3":
                            return finish()
            with scope() as P4:
                mix = T(P4, "mix", [128, 8, D], F32)
                gg = T(P4, "gg", [128, D], F32)
                gt = T(P4, "gt", [128, D], F32)
                xt = [T(P4, "xt4_%d" % i, [128, D], F32) for i in range(2)]
                xnb = [T(P4, "xnb4_%d" % i, [128, D], BF16) for i in range(2)]
                p.dma("sp", gg[:], modD[0:1, 2 * D:3 * D].partition_broadcast(128), reads=["modD"], writes=["gg"])
                p.dma("sp", gt[:], g_post_mix[0:1, :].partition_broadcast(128), writes=["gt"])
                p.op("dve", lambda e: e.tensor_tensor(out=gg[:], in0=gg[:], in1=gt[:], op=ALU.mult),
                     reads=["gg", "gt"], writes=["gg"])
                rstd_batch(st[:, 128:144], st[:, 144:160], 1024, [("st", 128 + i) for i in range(16)], "rs_pa", st[:, 160:176])
                for cg in range(4):
                    W, wkey = ring_get("wout")
                    for j in range(8):
                        bA = (2 * j) % 8
                        bB = (2 * j + 1) % 8
                        for k in range(8):
                            mm(ps[:, bA, :], big16[:, k, j * 128:(j + 1) * 128], W[:, k, :], k == 0, k == 7,
                               [wkey, ("b16", j)], bank(bA), k == 7)
                        for k in range(8, 16):
                            mm(ps[:, bB, :], big16[:, k, j * 128:(j + 1) * 128], W[:, k, :], k == 8, k == 15,
                               [wkey, ("b16", j)], bank(bB), k == 15)
                        p.op("act", lambda e, j=j, cg=cg, bA=bA: e.activation(
                            out=mix[:, j, cg * 512:(cg + 1) * 512], in_=ps[:, bA, :], func=AF.Identity,
                            scale=st[:, 152 + j:153 + j]), reads=bank(bA) + ["rs_pa"], writes=[("mix", j)])
                        p.op("dve", lambda e, j=j, cg=cg, bB=bB: e.scalar_tensor_tensor(
                            out=mix[:, j, cg * 512:(cg + 1) * 512], in0=ps[:, bB, :], scalar=st[:, 144 + j:145 + j],
                            in1=mix[:, j, cg * 512:(cg + 1) * 512], op0=ALU.mult, op1=ALU.add),
                            reads=bank(bB) + ["rs_pa", ("mix", j)], writes=[("mix", j)])
                    ring_done()
                p.barrier()
                for j in range(8):
                    p.op("act", lambda e, j=j: e.activation(out=xnb[j % 2][:], in_=mix[:, j, :], func=AF.Square,
                                                            accum_out=st[:, 176 + j:177 + j]),
                         reads=[("mix", j)], writes=[("xnb", j % 2), ("st", 176 + j)])
                rstd_batch(st[:, 176:184], st[:, 184:192], D, [("st", 176 + i) for i in range(8)], "rs_m", st[:, 192:200])
                for j in range(8):
                    p.dma("sp", xt[j % 2][:], x_own[j * 128:(j + 1) * 128, :], writes=[("xt", j % 2)])
                    p.op("dve", lambda e, j=j: e.scalar_tensor_tensor(
                        out=mix[:, j, :], in0=mix[:, j, :], scalar=st[:, 184 + j:185 + j], in1=gg[:],
                        op0=ALU.mult, op1=ALU.mult), reads=[("mix", j), "rs_m", "gg"], writes=[("mix", j)])
                    p.op("dve", lambda e, j=j: e.tensor_tensor(out=mix[:, j, :], in0=mix[:, j, :], in1=xt[j % 2][:], op=ALU.add),
                         reads=[("mix", j), ("xt", j % 2)], writes=[("mix", j)])
                    p.dma("sp", x1D[j * 128:(j + 1) * 128, :], mix[:, j, :], reads=[("mix", j)], writes=["x1D"])
                    p.op("act", lambda e, j=j: e.activation(out=xnb[j % 2][:], in_=mix[:, j, :], func=AF.Square,
                                                            accum_out=st[:, 200 + j:201 + j]),
                         reads=[("mix", j)], writes=[("xnb", j % 2), ("st", 200 + j)])
                rstd_batch(st[:, 200:208], st[:, 208:216], D, [("st", 200 + i) for i in range(8)], "rs_2", st[:, 216:224])
                if debug:
                    dbg_dump("x1", mix[:], ("mix", 7))
                for j0 in range(0, 8, 2):
                    srcs = [("sbuf", (mix[:, j0 + i, :], ("mix", j0 + i)), 0) for i in range(2)]
                    dview = big16[:, :, j0 * 128:(j0 + 2) * 128]
                    dkeys[id(dview)] = "h2T"
                    norm_transpose_pair(srcs, None, xnb, dview, 64, 80, "b", 4 * ((j0 // 2) % 2),
                                        rstd_known=[(st[:, 208 + j0 + i:209 + j0 + i], "rs_2") for i in range(2)])
                if debug:
                    dbg_dump("h2T", big16[:], "h2T")
                p.barrier()
                if stage == "p4":
                    return finish()
            with scope() as P5:
                fT = T(P5, "fT", [128, 64, 1024], BF16)
                r32 = [T(P5, "r32_%d" % i, [128, 512], F32) for i in range(2)]
                ei = 0
                for fg in range(16):
                    W, wkey = ring_get("ff1")
                    for fc in range(4):
                        jc = fg * 4 + fc
                        for tg in range(2):
                            b = ei % 4
                            for k in range(16):
                                mm(ps[:, b, :], W[:, k, fc * 128:(fc + 1) * 128], big16[:, k, tg * 512:(tg + 1) * 512],
                                   k == 0, k == 15, [wkey, "h2T"], bank(b), k == 15)
                            r = r32[ei % 2]
                            rkey = ("r32", ei % 2)
                            ei += 1
                            p.op("act", lambda e, r=r, b=b: e.activation(out=r[:], in_=ps[:, b, :], func=AF.Relu),
                                 reads=bank(b), writes=[rkey])
                            p.op("dve", lambda e, r=r, jc=jc, tg=tg: e.tensor_tensor(
                                out=fT[:, jc, tg * 512:(tg + 1) * 512], in0=r[:], in1=r[:], op=ALU.mult),
                                reads=[rkey], writes=["fT"])
                    ring_done()
                if debug:
                    dbg_dump("fT", fT[:, 0:4, :], "fT")
                p.barrier()
                if stage == "p5":
                    return finish()
                fo = big16[:].rearrange("p a b -> p (a b)").rearrange("p (j d) -> p j d", d=D)
                jk = T(P5, "jk", [128, 512], BF16)
                for cg in range(4):
                    for jg in range(4):
                        W, wkey = ring_get("ff2")
                        for jj in range(16):
                            jc = jg * 16 + jj
                            for tt in range(8):
                                mm(ps[:, tt, :], fT[:, jc, tt * 128:(tt + 1) * 128], W[:, jj, :], jc == 0, jc == 63,
                                   [wkey, "fT"], bank(tt), jc == 63 or (jj == 15 and tt == 7))
                        ring_done()
                    for tt in range(8):
                        if tt % 2 == 0:
                            p.op("act", lambda e, tt=tt, cg=cg: e.activation(
                                out=fo[:, tt, cg * 512:(cg + 1) * 512], in_=ps[:, tt, :], func=AF.Copy),
                                reads=bank(tt), writes=[("fo", tt)])
                        else:
                            p.op("dve", lambda e, tt=tt, cg=cg: e.tensor_copy(
                                out=fo[:, tt, cg * 512:(cg + 1) * 512], in_=ps[:, tt, :]),
                                reads=bank(tt), writes=[("fo", tt)])
                if debug:
                    dbg_dump("fo", big16[:], ("fo", 7))
                p.barrier()
                if stage == "p6":
                    return finish()
            with scope() as P7:
                gg7 = T(P7, "gg7", [128, D], F32)
                gt7 = T(P7, "gt7", [128, D], F32)
                xt7 = [T(P7, "xt7_%d" % i, [128, D], F32) for i in range(2)]
                ot7 = [T(P7, "ot7_%d" % i, [128, D], F32) for i in range(2)]
                fo = big16[:].rearrange("p a b -> p (a b)").rearrange("p (j d) -> p j d", d=D)
                p.dma("sp", gg7[:], modD[0:1, 5 * D:6 * D].partition_broadcast(128), reads=["modD"], writes=["gg7"])
                p.dma("sp", gt7[:], g_post_ffn[0:1, :].partition_broadcast(128), writes=["gt7"])
                p.op("dve", lambda e: e.tensor_tensor(out=gg7[:], in0=gg7[:], in1=gt7[:], op=ALU.mult),
                     reads=["gg7", "gt7"], writes=["gg7"])
                for tt in range(8):
                    p.op("act", lambda e, tt=tt: e.activation(out=ot7[tt % 2][:], in_=fo[:, tt, :], func=AF.Square,
                                                              accum_out=st[:, 224 + tt:225 + tt]),
                         reads=[("fo", tt)], writes=[("ot", tt % 2), ("st", 224 + tt)])
                rstd_batch(st[:, 224:232], st[:, 72:80], D, [("st", 224 + i) for i in range(8)], "rs_f", st[:, 80:88])
                for tt in range(8):
                    p.dma("sp", xt7[tt % 2][:], x1D[tt * 128:(tt + 1) * 128, :], reads=["x1D"], writes=[("xt7", tt % 2)])
                    o = ot7[tt % 2]
                    p.op("dve", lambda e, tt=tt, o=o: e.scalar_tensor_tensor(
                        out=o[:], in0=fo[:, tt, :], scalar=st[:, 72 + tt:73 + tt], in1=gg7[:],
                        op0=ALU.mult, op1=ALU.mult), reads=[("fo", tt), "rs_f", "gg7"], writes=[("ot", tt % 2)])
                    p.op("dve", lambda e, tt=tt, o=o: e.tensor_tensor(out=o[:], in0=o[:], in1=xt7[tt % 2][:], op=ALU.add),
                         reads=[("ot", tt % 2), ("xt7", tt % 2)], writes=[("ot", tt % 2)])
                    final.append(p.dma("sp", y[tt * 128:(tt + 1) * 128, :], o[:], reads=[("ot", tt % 2)]))
                return finish()


def own_blocks(ty):
    return [2 * j + ty if j < 4 else 2 * j + 1 - ty for j in range(8)]


def _pool_mats(first_is_block0):
    wins = (2, 4, 8, 16)
    Mm = np.zeros((128, 2, 4, 128), np.float32)
    for var in range(2):
        blk0 = (var == 0 and first_is_block0)
        for g, w in enumerate(wins):
            for t in range(128):
                cnt = min(t + 1, w) if blk0 else w
                lo = max(t + 1 - w, 0)
                Mm[lo:t + 1, var, g, t] = 1.0 / cnt
                Mm[t, var, g, t] -= 1.0
    Hf = np.zeros((128, 8, 4, 16), np.float32)
    for j in range(8):
        if j == 0 and first_is_block0:
            continue
        for g, w in enumerate(wins):
            for t in range(16):
                for r in range(16):
                    off = r - 16
                    if t - w + 1 <= off:
                        Hf[16 * j + r, j, g, t] = 1.0 / w
    return Mm, Hf


def host_inputs(inputs):
    x = np.asarray(inputs["x"], np.float32)
    c = np.asarray(inputs["c"], np.float32)
    f = lambda k: np.ascontiguousarray(np.asarray(inputs[k], np.float32)[0])
    shared = {
        "w_ada": f("w_ada"), "b_ada": f("b_ada")[None, :],
        "g_post_mix": f("g_post_mix")[None, :], "g_post_ffn": f("g_post_ffn")[None, :],
        "w_in": f("w_in"), "w_pool": f("w_pool"), "pool_scale": f("pool_scale")[None, :],
        "g_pool_out": f("g_pool_out")[None, :], "g_attn_out": f("g_attn_out")[None, :],
        "w_out": f("w_out"), "w_ff1": f("w_ff1"), "w_ff2": f("w_ff2"),
        "I4": np.ascontiguousarray(np.tile(np.eye(128, dtype=np.float32), (1, 4))),
        "pw2": np.ascontiguousarray(np.tile((0.5 ** np.arange(1, 33, dtype=np.float64)).astype(np.float32)[None, :], (128, 1))),
    }
    gpre1 = f("g_pre_mix").reshape(16, 128).T
    gpre2 = f("g_pre_ffn").reshape(16, 128).T
    shared["vcols"] = np.ascontiguousarray(np.concatenate([gpre1, gpre2], axis=1))
    in_maps = []
    for core in range(NCORES):
        b, ty = core // 2, core % 2
        own = own_blocks(ty)
        oth = own_blocks(1 - ty)
        xb = x[b].reshape(16, 128, D)
        m = dict(shared)
        m["x_own"] = np.ascontiguousarray(xb[own].reshape(1024, D))
        m["x_oth"] = np.ascontiguousarray(xb[oth].reshape(1024, D))
        halo = np.zeros((128, D), np.float32)
        for j, blk in enumerate(own):
            if blk > 0:
                halo[16 * j:16 * j + 16] = x[b, blk * 128 - 16:blk * 128]
        m["x_halo"] = halo
        m["c_col"] = np.ascontiguousarray(c[b].reshape(16, 128).T)
        kpos = np.zeros(S, np.float64)
        for j in range(8):
            kpos[(2 * j) * 128:(2 * j + 1) * 128] = own[j] * 128 + np.arange(128)
            kpos[(2 * j + 1) * 128:(2 * j + 2) * 128] = oth[j] * 128 + np.arange(128)
        m["nkB"] = np.ascontiguousarray(np.tile((-kpos * BIG).astype(np.float32)[None, :], (128, 1)))
        qpos = np.array(own, np.float64)[None, :] * 128 + np.arange(128)[:, None]
        m["qB"] = np.ascontiguousarray(((qpos + 0.5) * BIG).astype(np.float32))
        Mm, Hf = _pool_mats(own[0] == 0)
        m["Mm"] = np.ascontiguousarray(Mm.reshape(128, -1))
        m["Hf"] = np.ascontiguousarray(Hf.reshape(128, -1))
        in_maps.append(m)
    return in_maps


_NC_CACHE = {}


def kernel(**inputs):
    in_maps = host_inputs(inputs)
    if "nc" not in _NC_CACHE:
        _NC_CACHE["nc"] = build_program()
    nc = _NC_CACHE["nc"]
    res = run_bass_kernel_spmd(nc, in_maps, core_ids=list(range(NCORES)))
    out = np.zeros((4, 16, 128, D), np.float32)
    for core in range(NCORES):
        b, ty = core // 2, core % 2
        yc = np.asarray(res.results[core]["y"], np.float32).reshape(8, 128, D)
        for j, blk in enumerate(own_blocks(ty)):
            out[b, blk] = yc[j]
    return out.reshape(4, S, D)
```

```python
import bisect
from contextlib import ExitStack

import numpy as np
import concourse.bass as bass
import concourse.mybir as mybir
from concourse.bass_utils import run_bass_kernel_spmd

F32 = mybir.dt.float32
BF16 = mybir.dt.bfloat16
ALU = mybir.AluOpType
AF = mybir.ActivationFunctionType
AX = mybir.AxisListType

D = 2048
S = 2048
DFF = 8192
IN_W = 4752
NCORES = 8
NIT = 24
TOPK = 256.0
EPS = 1e-6
MASKV = -30000.0
BIG = 1e30
IDX_SCALE = (16 ** -0.5) * (128 ** -0.5)
ATT_SCALE = 128 ** -0.5
NDSEM = 24
NRING = 2


class Prog:
    ENG = ("pe", "act", "dve", "pool", "sp")

    def __init__(self, nc):
        self.nc = nc
        self.ops = {e: [] for e in self.ENG}
        self.sig_seq = {e: [] for e in self.ENG}
        self.nseq = {e: 0 for e in self.ENG}
        self.wr = {}
        self.rd = {}
        self.dslot_next = 0
        self.dslot_val = [0] * NDSEM
        self.floor = []

    def _deps(self, eng, reads, writes):
        deps = list(self.floor)
        for k in reads:
            deps.extend(self.wr.get(k, {}).values())
        for k in writes:
            deps.extend(self.wr.get(k, {}).values())
            deps.extend(self.rd.get(k, {}).values())
        return [t for t in deps if not (t[0] == "c" and t[1] == eng and eng == "pe")]

    def _record(self, tok, who, reads, writes):
        for k in reads:
            self.rd.setdefault(k, {})[who] = tok
        for k in writes:
            self.wr.setdefault(k, {})[who] = tok

    def op(self, eng, fn, reads=(), writes=(), signal=True):
        deps = self._deps(eng, reads, writes)
        seq = self.nseq[eng]
        self.nseq[eng] += 1
        if signal:
            self.sig_seq[eng].append(seq)
        tok = ("c", eng, seq)
        self.ops[eng].append(dict(kind="c", fn=fn, deps=deps, seq=seq, signal=signal))
        self._record(tok, eng, reads, writes)
        return tok

    def dma(self, eng, out, in_, reads=(), writes=(), **kw):
        deps = self._deps(eng, reads, writes)
        slot = self.dslot_next
        self.dslot_next = (slot + 1) % NDSEM
        prev = self.dslot_val[slot]
        self.dslot_val[slot] = prev + 16
        tok = ("d", slot, prev + 16)
        if prev:
            deps.append(("d", slot, prev))
        self.ops[eng].append(dict(kind="d", out=out, in_=in_, deps=deps, slot=slot, kw=kw,
                                  seq=self.nseq[eng]))
        self._record(tok, "dma%d" % slot, reads, writes)
        return tok

    def barrier(self):
        fl = []
        for e in ("pe", "act", "dve", "pool"):
            if self.nseq[e]:
                assert self.sig_seq[e] and self.sig_seq[e][-1] == self.nseq[e] - 1, e
                fl.append(("c", e, self.nseq[e] - 1))
        for s in range(NDSEM):
            if self.dslot_val[s]:
                fl.append(("d", s, self.dslot_val[s]))
        self.floor = fl

    def emit(self, final_tokens=()):
        nc = self.nc
        with ExitStack() as es:
            csem = {e: es.enter_context(nc.semaphore("s_" + e)) for e in self.ENG if e != "sp"}
            dsem = [es.enter_context(nc.semaphore("d%d" % i)) for i in range(NDSEM)]
            block = es.enter_context(nc.Block())
            sig_seq = self.sig_seq

            def resolve(tok):
                if tok[0] == "d":
                    return dsem[tok[1]], tok[2]
                _, e, seq = tok
                i = bisect.bisect_left(sig_seq[e], seq)
                assert i < len(sig_seq[e]), ("no signalling op after", tok)
                return csem[e], i + 1

            def run(ename):
                def body(engine):
                    waited = {}
                    for o in self.ops[ename]:
                        for t in o["deps"]:
                            if t[0] == "c" and t[1] == ename:
                                i = bisect.bisect_left(sig_seq[ename], t[2])
                                assert i < len(sig_seq[ename]) and sig_seq[ename][i] < o["seq"], \
                                    ("same-engine dep on unsignalled op", ename, t)
                            sem, val = resolve(t)
                            if waited.get(id(sem), 0) >= val:
                                continue
                            waited[id(sem)] = val
                            engine.wait_ge(sem, val)
                        if o["kind"] == "c":
                            ins = o["fn"](engine)
                            if o["signal"]:
                                ins.then_inc(csem[ename], 1)
                        else:
                            engine.dma_start(out=o["out"], in_=o["in_"], **o["kw"]).then_inc(
                                dsem[o["slot"]], 16)
                    if ename == "sp":
                        for t in final_tokens:
                            sem, val = resolve(t)
                            engine.wait_ge(sem, val)
                return body

            block.tensor(run("pe"))
            block.scalar(run("act"))
            block.vector(run("dve"))
            block.gpsimd(run("pool"))
            block.sync(run("sp"))


def build_program(debug=False, stage=None):
    nc = bass.Bass("TRN2", target_bir_lowering=False)
    dt_in = lambda name, shape: nc.dram_tensor(name, shape, F32, kind="ExternalInput").ap()
    x_own = dt_in("x_own", [1024, D])
    x_oth = dt_in("x_oth", [1024, D])
    x_halo = dt_in("x_halo", [128, D])
    c_col = dt_in("c_col", [128, 16])
    vcols = dt_in("vcols", [128, 32])
    nkB_d = dt_in("nkB", [128, S])
    qB_d = dt_in("qB", [128, 8])
    pw2_d = dt_in("pw2", [128, 32])
    Mm_d = dt_in("Mm", [128, 2 * 4 * 128])
    Hf_d = dt_in("Hf", [128, 8 * 4 * 16])
    I4_d = dt_in("I4", [128, 512])
    w_ada = dt_in("w_ada", [D, 6 * D])
    b_ada = dt_in("b_ada", [1, 6 * D])
    g_post_mix = dt_in("g_post_mix", [1, D])
    g_post_ffn = dt_in("g_post_ffn", [1, D])
    w_in = dt_in("w_in", [D, IN_W])
    w_pool = dt_in("w_pool", [4, 256, 256])
    pool_scale = dt_in("pool_scale", [1, 1024])
    g_pool_out = dt_in("g_pool_out", [1, 1024])
    g_attn_out = dt_in("g_attn_out", [1, 1024])
    w_out = dt_in("w_out", [D, D])
    w_ff1 = dt_in("w_ff1", [D, DFF])
    w_ff2 = dt_in("w_ff2", [DFF, D])
    y = nc.dram_tensor("y", [1024, D], F32, kind="ExternalOutput").ap()
    modD = nc.dram_tensor("modD", [1, 6 * D], F32).ap()
    x1D = nc.dram_tensor("x1D", [1024, D], F32).ap()
    dbg = {}
    if debug:
        for nm, shp in debug.items():
            dbg[nm] = nc.dram_tensor("dbg_" + nm, shp, F32, kind="ExternalOutput").ap()

    p = Prog(nc)
    final = []

    def finish():
        p.emit(final_tokens=final)
        return nc

    ARENA_BYTES = 210944
    arena_cm = nc.sbuf_tensor("arena", [128, ARENA_BYTES // 2], BF16)
    arena_t = arena_cm.__enter__()
    astate = dict(off=0, peak=0)

    def T(es, name, shape, dt):
        n = 1
        for s_ in shape[1:]:
            n *= s_
        nb = n * (4 if dt == F32 else 2)
        off = (astate["off"] + 63) // 64 * 64
        assert off + nb <= ARENA_BYTES, ("SBUF arena overflow", name, off + nb)
        astate["off"] = off + nb
        astate["peak"] = max(astate["peak"], off + nb)
        ap = arena_t[:, off // 2:(off + nb) // 2]
        if dt == F32:
            ap = ap.bitcast(F32)
        if len(shape) == 3:
            ap = ap.rearrange("p (a b) -> p a b", a=shape[1])
        elif len(shape) == 4:
            ap = ap.rearrange("p (a b c) -> p a b c", a=shape[1], b=shape[2])
        return ap

    def scope():
        es = ExitStack()
        m = astate["off"]

        def rel():
            astate["off"] = m
        es.callback(rel)
        return es

    def bank(b, n=1):
        return ["pb%d" % i for i in range(b, b + n)]

    with ExitStack() as G:
        ps = G.enter_context(nc.psum_tensor("ps", [128, 8, 512], F32))
        psf = ps[:].rearrange("p a b -> p (a b)")
        psb = ps[:].bitcast(BF16).rearrange("p a b -> p (a b)")
        ring = [T(G, "ring%d" % i, [128, 16, 512], BF16) for i in range(NRING)]
        st = T(G, "st", [128, 256], F32)
        cols = T(G, "cols", [128, 96], F32)
        identb = T(G, "identb", [128, 128], BF16)
        p.op("dve", lambda e: e.memset(st[:], 0.0), writes=["st"])
        p.dma("sp", cols[:, 0:32], vcols[:, :], writes=["cols_g"])
        p.dma("pool", identb[:], I4_d[:, 0:128], writes=["identb"])

        sched = []
        for cg in range(8):
            sched.append(("ada", w_ada[:, cg * 512:(cg + 1) * 512], 512))
        sched.append(("inA", w_in[:, 2048:2560], 512))
        sched.append(("inB", w_in[:, 4608:4752], 144))
        for i in range(2):
            sched.append(("q", w_in[:, 1024 + 512 * i:1536 + 512 * i], 512))
        for i in range(4):
            sched.append(("iq", w_in[:, 2560 + 512 * i:3072 + 512 * i], 512))
        for i in range(2):
            sched.append(("pool", w_in[:, 512 * i:512 * (i + 1)], 512))
        for cg in range(8, 24):
            sched.append(("ada", w_ada[:, cg * 512:(cg + 1) * 512], 512))
        for cg in range(4):
            sched.append(("wout", w_out[:, cg * 512:(cg + 1) * 512], 512))
        for fg in range(16):
            sched.append(("ff1", w_ff1[:, fg * 512:(fg + 1) * 512], 512))
        for cg in range(4):
            for jg in range(4):
                sched.append(("ff2", w_ff2[jg * 2048:(jg + 1) * 2048, cg * 512:(cg + 1) * 512], 512))
        rstate = dict(loaded=0, used=0)

        def ring_load():
            i = rstate["loaded"]
            if i >= len(sched):
                return
            name, src, ncol = sched[i]
            buf = ring[i % NRING]
            p.dma("pool", buf[:, :, 0:ncol], src.rearrange("(k p) c -> p k c", p=128),
                  writes=[("ring", i % NRING)])
            rstate["loaded"] += 1

        def ring_get(name):
            i = rstate["used"]
            assert sched[i][0] == name, (sched[i][0], name)
            while rstate["loaded"] <= i:
                ring_load()
            return ring[i % NRING], ("ring", i % NRING)

        def ring_done():
            rstate["used"] += 1
            ring_load()

        for _ in range(NRING):
            ring_load()

        def mm(out, lhsT, rhs, start, stop, reads, writes, signal):
            p.op("pe", lambda e: e.matmul(out, lhsT=lhsT, rhs=rhs, start=start, stop=stop),
                 reads=reads, writes=writes, signal=signal)

        def rstd_batch(ss_ap, out_ap, n, key_in, key_out, tmp_ap):
            p.op("dve", lambda e: e.tensor_scalar(out=tmp_ap, in0=ss_ap, scalar1=1.0 / n, scalar2=EPS,
                                                   op0=ALU.mult, op1=ALU.add),
                 reads=(key_in if isinstance(key_in, list) else [key_in]), writes=[key_out + "_t"])
            p.op("act", lambda e: e.activation(out=tmp_ap, in_=tmp_ap, func=AF.Sqrt),
                 reads=[key_out + "_t"], writes=[key_out + "_t"])
            p.op("dve", lambda e: e.reciprocal(out=out_ap, in_=tmp_ap),
                 reads=[key_out + "_t"], writes=[key_out])

        def norm_transpose_pair(srcs, xt, xnb, dst, gcol, scol, tagbase, pbase, rstd_known=None):
            n = len(srcs)
            pv = psb[:, pbase * 1024:(pbase + 4) * 1024].rearrange("p (k t) -> p k t", k=16)
            for i, (kind, src, sc) in enumerate(srcs):
                if kind == "dram":
                    xa = xt[i]
                    xkey = ("xt", i)
                    p.dma("sp", xa[:], src, writes=[xkey])
                    xin = xa[:]
                else:
                    xin, xkey = src
                nb = xnb[i]
                nkey = ("xnb", i)
                if rstd_known is None:
                    p.op("act", lambda e, xin=xin, nb=nb, sc=sc: e.activation(
                        out=nb[:], in_=xin, func=AF.Square, accum_out=st[:, sc:sc + 1]),
                        reads=[xkey], writes=[nkey, ("st", sc)])
                    rstd_batch(st[:, sc:sc + 1], st[:, sc + 32:sc + 33], D, ("st", sc), "rs%s%d" % (tagbase, sc),
                               st[:, sc + 64:sc + 65])
                    rkey = "rs%s%d" % (tagbase, sc)
                    rap = st[:, sc + 32:sc + 33]
                else:
                    rap, rkey = rstd_known[i]
                p.op("dve", lambda e, xin=xin, nb=nb, rap=rap: e.tensor_scalar(
                    out=nb[:], in0=xin, scalar1=rap, scalar2=None, op0=ALU.mult),
                    reads=[xkey, rkey], writes=[nkey])
                for k in range(16):
                    p.op("pe", lambda e, k=k, i=i, nb=nb: e.transpose(
                        out=pv[:, k, i * 128:(i + 1) * 128], in_=nb[:, k * 128:(k + 1) * 128], identity=identb[:]),
                        reads=[nkey, "identb"], writes=bank(pbase, 4), signal=(k == 15))
            w = n * 128
            if gcol is None:
                for kb in range(4):
                    o_ = dst[:, 4 * kb:4 * kb + 4, 0:w]
                    i_ = pv[:, 4 * kb:4 * kb + 4, 0:w]
                    if kb % 2 == 0:
                        p.op("act", lambda e, o_=o_, i_=i_: e.activation(out=o_, in_=i_, func=AF.Copy),
                             reads=bank(pbase + kb), writes=[dst_key(dst)])
                    else:
                        p.op("dve", lambda e, o_=o_, i_=i_: e.tensor_copy(out=o_, in_=i_),
                             reads=bank(pbase + kb), writes=[dst_key(dst)])
                return
            for k in range(16):
                if (k // 4) % 2 == 0:
                    p.op("act", lambda e, k=k: e.activation(
                        out=dst[:, k, 0:w], in_=pv[:, k, 0:w], func=AF.Identity,
                        scale=cols[:, gcol + k:gcol + k + 1], bias=cols[:, scol + k:scol + k + 1]),
                        reads=bank(pbase, 4) + ["cols_m"], writes=[dst_key(dst)])
                else:
                    p.op("dve", lambda e, k=k: e.tensor_scalar(
                        out=dst[:, k, 0:w], in0=pv[:, k, 0:w], scalar1=cols[:, gcol + k:gcol + k + 1],
                        scalar2=cols[:, scol + k:scol + k + 1], op0=ALU.mult, op1=ALU.add),
                        reads=bank(pbase, 4) + ["cols_m"], writes=[dst_key(dst)])

        dkeys = {}

        def dst_key(ap):
            return dkeys[id(ap)]

        def dbg_dump(name, ap_sb, key):
            if debug and name in dbg:
                final.append(p.dma("pool", dbg[name], ap_sb, reads=[key]))

        brow = [T(G, "brow%d" % i, [1, 512], F32)[0:1, :] for i in range(1)]
        mrow = [T(G, "mrow%d" % i, [1, 512], F32)[0:1, :] for i in range(1)]
        ctmp = T(G, "ctmp", [128, 32], F32)
        caT = T(G, "caT", [128, 16], BF16)
        p.dma("sp", ctmp[:, 0:16], c_col[:, :], writes=["ccol"])
        p.op("act", lambda e: e.activation(out=ctmp[:, 16:32], in_=ctmp[:, 0:16], func=AF.Exp, scale=-1.0),
             reads=["ccol"], writes=["cexp"])
        p.op("dve", lambda e: e.tensor_scalar(out=ctmp[:, 16:32], in0=ctmp[:, 16:32], scalar1=1.0, scalar2=None,
                                               op0=ALU.add), reads=["cexp"], writes=["cexp"])
        p.op("dve", lambda e: e.reciprocal(out=ctmp[:, 16:32], in_=ctmp[:, 16:32]), reads=["cexp"], writes=["cexp"])
        p.op("dve", lambda e: e.tensor_tensor(out=caT[:], in0=ctmp[:, 0:16], in1=ctmp[:, 16:32], op=ALU.mult),
             reads=["cexp", "ccol"], writes=["caT"])
        ada_n = [0]

        def ada_group(b):
            cg = ada_n[0]
            ada_n[0] += 1
            W, wkey = ring_get("ada")
            i = 0
            p.dma("sp", brow[i], b_ada[0:1, cg * 512:(cg + 1) * 512], writes=[("brow", i)])
            for k in range(16):
                mm(ps[0:1, b, :], caT[:, k:k + 1], W[:, k, :], k == 0, k == 15,
                   [wkey, "caT"], bank(b), k == 15)
            ring_done()
            p.op("dve", lambda e: e.tensor_tensor(out=mrow[i], in0=ps[0:1, b, :], in1=brow[i], op=ALU.add),
                 reads=bank(b) + [("brow", i)], writes=[("mrow", i)])
            p.dma("sp", modD[0:1, cg * 512:(cg + 1) * 512], mrow[i], reads=[("mrow", i)], writes=["modD"])

        def colload(dstc, off, key):
            p.dma("sp", cols[:, dstc:dstc + 16],
                  modD[0, off:off + D].rearrange("(k p) -> p k", p=128),
                  reads=["modD"], writes=[key], allow_slow_non_contiguous=True)

        with scope() as S1:
            big16 = T(S1, "big16", [128, 16, 1024], BF16)
            dkeys[id(big16)] = "big16"
            with scope() as S2:
                KT = T(S2, "KT", [128, 2, S], BF16)
                Vaug = T(S2, "Vaug", [128, 16, 2, 129], BF16)
                kiT = T(S2, "kiT", [128, S], BF16)
                wq = T(S2, "wq", [128, 8, 16], F32)
                with scope() as S4:
                    qT = T(S4, "qT", [128, 8, 8, 128], BF16)
                    u = T(S4, "u", [128, 8, 1024], BF16)
                    uh = T(S4, "uh", [128, 1024], BF16)
                    with scope() as S3:
                        hT_own = T(S3, "hT_own", [128, 16, 1024], BF16)
                        hT_halo = T(S3, "hT_halo", [128, 16, 128], BF16)
                        dkeys[id(hT_own)] = "hT_own"
                        dkeys[id(hT_halo)] = "hT_halo"
                        xt = [T(S3, "xt%d" % i, [128, D], F32) for i in range(2)]
                        xnb = [T(S3, "xnb%d" % i, [128, D], BF16) for i in range(2)]
                        grp = 0
                        for which, src, dstT in (("own", x_own, hT_own), ("oth", x_oth, big16)):
                            for j0 in range(0, 8, 2):
                                srcs = [("dram", src[(j0 + i) * 128:(j0 + i + 1) * 128, :],
                                         (0 if which == "own" else 8) + j0 + i) for i in range(2)]
                                dview = dstT[:, :, j0 * 128:(j0 + 2) * 128]
                                dkeys[id(dview)] = dkeys[id(dstT)]
                                ada_group(4 * ((grp + 1) % 2))
                                norm_transpose_pair(srcs, xt, xnb, dview, None, None, "a", 4 * (grp % 2))
                                grp += 1
                        dview = hT_halo[:, :, :]
                        dkeys[id(dview)] = "hT_halo"
                        norm_transpose_pair([("dram", x_halo[:, :], 16)], xt, xnb, dview, None, None, "a", 4 * (grp % 2))
                        colload(48, 0, "c_sh1")
                        colload(32, D, "c_sc1")
                        p.op("dve", lambda e: e.scalar_tensor_tensor(out=cols[:, 32:48], in0=cols[:, 32:48], scalar=1.0,
                                                                      in1=cols[:, 0:16], op0=ALU.add, op1=ALU.mult),
                             reads=["c_sc1", "cols_g", "c_sh1"], writes=["c_sc1", "cols_m1"])
                        for (tile_, tkey) in ((hT_own, "hT_own"), (big16, "big16"), (hT_halo, "hT_halo")):
                            for k in range(16):
                                p.op("dve", lambda e, tile_=tile_, k=k: e.tensor_scalar(
                                    out=tile_[:, k, :], in0=tile_[:, k, :], scalar1=cols[:, 32 + k:33 + k],
                                    scalar2=cols[:, 48 + k:49 + k], op0=ALU.mult, op1=ALU.add),
                                    reads=[tkey, "cols_m1"], writes=[tkey])
                        if debug:
                            dbg_dump("hT_own", hT_own[:], "hT_own")
                            dbg_dump("hT_oth", big16[:], "big16")
                            dbg_dump("hT_halo", hT_halo[:], "hT_halo")
                        if stage == "p1":
                            return finish()
                        WA, keyA = ring_get("inA")
                        ring_done_A = False
                        pb = [0]

                        def nextbank():
                            b = pb[0]
                            pb[0] = (b + 1) % 8
                            return b

                        p.op("dve", lambda e: e.memset(Vaug[:, :, :, 128:129], 1.0), writes=["Vaug"])
                        KTv = KT[:].rearrange("p g (j two t) -> p g j two t", two=2, t=128)
                        kiTv = kiT[:].rearrange("p (j two t) -> p j two t", two=2, t=128)
                        cpy = [0]

                        def evac_copy(out, in_, reads, writes):
                            cpy[0] += 1
                            if cpy[0] % 2:
                                p.op("act", lambda e: e.activation(out=out, in_=in_, func=AF.Copy),
                                     reads=reads, writes=writes)
                            else:
                                p.op("dve", lambda e: e.tensor_copy(out=out, in_=in_), reads=reads, writes=writes)

                        for par, hsrc, hkey in ((0, hT_own, "hT_own"), (1, big16, "big16")):
                            for tg in range(2):
                                for c in range(2):
                                    b = nextbank()
                                    for k in range(16):
                                        mm(ps[:, b, :], WA[:, k, c * 128:(c + 1) * 128], hsrc[:, k, tg * 512:(tg + 1) * 512],
                                           k == 0, k == 15, [keyA, hkey], bank(b), k == 15)
                                    evac_copy(KTv[:, c, tg * 4:(tg + 1) * 4, par, :],
                                              ps[:, b, :].rearrange("p (j t) -> p j t", t=128), bank(b), ["KT"])
                                for tt in range(4):
                                    jt = tg * 4 + tt
                                    b = nextbank()
                                    for k in range(16):
                                        mm(ps[:, b, 0:256], hsrc[:, k, jt * 128:(jt + 1) * 128], WA[:, k, 256:512],
                                           k == 0, k == 15, [keyA, hkey], bank(b), k == 15)
                                    evac_copy(Vaug[:, 2 * jt + par, :, 0:128],
                                              ps[:, b, 0:256].rearrange("p (g d) -> p g d", d=128), bank(b), ["Vaug"])
                        ring_done()
                        WB, keyB = ring_get("inB")
                        for par, hsrc, hkey in ((0, hT_own, "hT_own"), (1, big16, "big16")):
                            for tg in range(2):
                                b = nextbank()
                                for k in range(16):
                                    mm(ps[:, b, :], WB[:, k, 0:128], hsrc[:, k, tg * 512:(tg + 1) * 512],
                                       k == 0, k == 15, [keyB, hkey], bank(b), k == 15)
                                evac_copy(kiTv[:, tg * 4:(tg + 1) * 4, par, :],
                                          ps[:, b, :].rearrange("p (j t) -> p j t", t=128), bank(b), ["kiT"])
                        for j in range(8):
                            b = nextbank()
                            for k in range(16):
                                mm(ps[:, b, 0:16], hT_own[:, k, j * 128:(j + 1) * 128], WB[:, k, 128:144],
                                   k == 0, k == 15, [keyB, "hT_own"], bank(b), k == 15)
                            p.op("dve", lambda e, b=b, j=j: e.tensor_scalar(
                                out=wq[:, j, :], in0=ps[:, b, 0:16], scalar1=IDX_SCALE, scalar2=None, op0=ALU.mult),
                                reads=bank(b), writes=["wq"])
                        ring_done()
                        if debug:
                            dbg_dump("KT", KT[:], "KT")
                            dbg_dump("kiT", kiT[:], "kiT")
                            dbg_dump("Vaug", Vaug[:], "Vaug")
                            dbg_dump("wq", wq[:], "wq")
                        p.barrier()
                        if stage == "p2a":
                            return finish()
                        for i in range(2):
                            W, wkey = ring_get("q")
                            for hh in range(4):
                                h = 4 * i + hh
                                for tg in range(2):
                                    b = nextbank()
                                    for k in range(16):
                                        mm(ps[:, b, :], W[:, k, hh * 128:(hh + 1) * 128], hT_own[:, k, tg * 512:(tg + 1) * 512],
                                           k == 0, k == 15, [wkey, "hT_own"], bank(b), k == 15)
                                    evac_copy(qT[:, tg * 4:(tg + 1) * 4, h, :],
                                              ps[:, b, :].rearrange("p (j t) -> p j t", t=128), bank(b), ["qT"])
                            ring_done()
                        for i in range(4):
                            W, wkey = ring_get("iq")
                            for hh in range(4):
                                h = 4 * i + hh
                                for tg in range(2):
                                    b = nextbank()
                                    for k in range(16):
                                        mm(ps[:, b, :], W[:, k, hh * 128:(hh + 1) * 128], hT_own[:, k, tg * 512:(tg + 1) * 512],
                                           k == 0, k == 15, [wkey, "hT_own"], bank(b), k == 15)
                                    evac_copy(big16[:, h, tg * 512:(tg + 1) * 512], ps[:, b, :], bank(b),
                                              [("b16", tg * 4 + jj) for jj in range(4)])
                            ring_done()
                        for i in range(2):
                            W, wkey = ring_get("pool")
                            for j in range(9):
                                b = nextbank()
                                for k in range(16):
                                    lhsT = hT_own[:, k, j * 128:(j + 1) * 128] if j < 8 else hT_halo[:, k, :]
                                    mm(ps[:, b, :], lhsT, W[:, k, :], k == 0, k == 15,
                                       [wkey, "hT_own", "hT_halo"], bank(b), k == 15)
                                dst = u[:, j, i * 512:(i + 1) * 512] if j < 8 else uh[:, i * 512:(i + 1) * 512]
                                evac_copy(dst, ps[:, b, :], bank(b), ["u"])
                            ring_done()
                        if debug:
                            dbg_dump("qT", qT[:], "qT")
                            dbg_dump("qiT", big16[:], ("b16", 7))
                            dbg_dump("u", u[:], "u")
                            dbg_dump("uh", uh[:], "u")
                        p.barrier()
                        if stage == "p2b":
                            return finish()
                    with scope() as P3:
                        nkB = T(P3, "nkB", [128, S], F32)
                        qB = T(P3, "qB", [128, 8], F32)
                        pw2 = T(P3, "pw2", [128, 32], F32)
                        Mm = T(P3, "Mm", [128, 2, 4, 128], BF16)
                        Hf = T(P3, "Hf", [128, 8, 4, 16], BF16)
                        I4 = T(P3, "I4", [128, 512], BF16)
                        wps = T(P3, "wps", [128, 4, 2, 256], BF16)
                        gb = T(P3, "gb", [128, 2048], F32)
                        p.dma("sp", nkB[:], nkB_d[:, :], writes=["nkB"])
                        p.dma("sp", qB[:], qB_d[:, :], writes=["qB"])
                        p.dma("sp", pw2[:], pw2_d[:, :], writes=["pw2"])
                        p.dma("pool", Mm[:].rearrange("p a g t -> p (a g t)"), Mm_d[:, :], writes=["Mm"])
                        p.dma("pool", Hf[:].rearrange("p a g t -> p (a g t)"), Hf_d[:, :], writes=["Hf"])
                        p.dma("pool", I4[:], I4_d[:, :], writes=["I4"])
                        p.dma("sp", gb[:, 0:1024], g_pool_out[0:1, :].partition_broadcast(128), writes=["gb0"])
                        p.dma("sp", gb[:, 1024:2048], g_attn_out[0:1, :].partition_broadcast(128), writes=["gb1"])
                        with scope() as P3s:
                            wpf = T(P3s, "wpf", [128, 4, 2, 256], F32)
                            psb_t = T(P3s, "psb_t", [128, 1024], F32)
                            p.dma("sp", wpf[:], w_pool.rearrange("g (cc p) d -> p g cc d", p=128), writes=["wpf"])
                            p.dma("sp", psb_t[:], pool_scale[0:1, :].partition_broadcast(128), writes=["psb_t"])
                            for g in range(4):
                                p.op("dve", lambda e, g=g, wpf=wpf, psb_t=psb_t: e.tensor_tensor(
                                    out=wps[:, g, :, :], in0=wpf[:, g, :, :],
                                    in1=psb_t[:, g * 256:(g + 1) * 256].unsqueeze(1).to_broadcast([128, 2, 256]),
                                    op=ALU.mult), reads=["wpf", "psb_t"], writes=["wps"])
                            p.barrier()
                        scoreS = [[T(P3, "scs%d" % s, [128, 1024], F32), T(P3, "scb%d" % s, [128, S], F32)] for s in range(2)]
                        maskS = [[T(P3, "mks%d" % s, [128, 1024], BF16), T(P3, "mkb%d" % s, [128, S], BF16)] for s in range(2)]
                        bsS = [[T(P3, "bs%d_%d" % (s, i), [128, 64], F32) for i in range(2)] for s in range(2)]
                        Rb = [T(P3, "Rb%d" % i, [128, 1024], F32) for i in range(2)]
                        PT = [T(P3, "PT%d" % i, [128, 512], BF16) for i in range(2)]
                        attn32 = T(P3, "attn32", [128, 1024], F32)
                        mg = T(P3, "mg", [128, 2048], BF16)
                        pooledT = T(P3, "pooledT", [128, 8, 128], BF16)
                        rcT = T(P3, "rcT", [128, 8], F32)
                        cnt_ci = [0]
                        cnt_ui = [0]

                        def stage_A(j, score, bs, tag):
                            L = 2 * j + 2
                            N = 128 * L
                            skey = ("score", tag)
                            chunks = [(c0, min(1024, N - c0)) for c0 in range(0, N, 1024)]
                            for h in range(16):
                                for (c0, cw) in chunks:
                                    ci = cnt_ci[0]
                                    cnt_ci[0] += 1
                                    bb = 2 * (ci % 2)
                                    rb = Rb[ci % 2]
                                    rkey = ("Rb", ci % 2)
                                    nmm = (cw + 511) // 512
                                    for m in range(nmm):
                                        w = min(512, cw - m * 512)
                                        mm(ps[:, bb + m, 0:w], big16[:, h, j * 128:(j + 1) * 128],
                                           kiT[:, c0 + m * 512:c0 + m * 512 + w], True, True,
                                           [("b16", j), "kiT"], bank(bb + m), m == nmm - 1)
                                    pin = psf[:, bb * 512:bb * 512 + cw]
                                    p.op("act", lambda e, rb=rb, pin=pin, cw=cw: e.activation(
                                        out=rb[:, 0:cw], in_=pin, func=AF.Relu),
                                        reads=bank(bb, nmm), writes=[rkey])
                                    if h == 0:
                                        p.op("dve", lambda e, rb=rb, c0=c0, cw=cw: e.tensor_scalar(
                                            out=score[:, c0:c0 + cw], in0=rb[:, 0:cw], scalar1=wq[:, j, 0:1],
                                            scalar2=None, op0=ALU.mult), reads=[rkey, "wq"], writes=[skey])
                                    else:
                                        p.op("dve", lambda e, rb=rb, c0=c0, cw=cw, h=h: e.scalar_tensor_tensor(
                                            out=score[:, c0:c0 + cw], in0=rb[:, 0:cw], scalar=wq[:, j, h:h + 1],
                                            in1=score[:, c0:c0 + cw], op0=ALU.mult, op1=ALU.add),
                                            reads=[rkey, "wq", skey], writes=[skey])
                                    yield
                            if debug and j == 1:
                                dbg_dump("score1", score[:, 0:512], skey)
                            p.op("dve", lambda e: e.tensor_reduce(out=bs[:, 0:1], in_=score[:, 0:N], axis=AX.X, op=ALU.max),
                                 reads=[skey], writes=[("bsM", tag)])
                            yield
                            p.op("dve", lambda e: e.tensor_reduce(out=bs[:, 1:2], in_=score[:, 0:N], axis=AX.X, op=ALU.min),
                                 reads=[skey], writes=[("bsm", tag)])
                            yield
                            p.op("dve", lambda e: e.scalar_tensor_tensor(
                                out=score[:, 0:N], in0=nkB[:, 0:N], scalar=qB[:, j:j + 1], in1=score[:, 0:N],
                                op0=ALU.add, op1=ALU.min), reads=["nkB", "qB", skey], writes=[skey])
                            yield
                            p.op("dve", lambda e: e.tensor_scalar(out=bs[:, 2:3], in0=bs[:, 1:2], scalar1=-1.0, scalar2=None,
                                                                   op0=ALU.add), reads=[("bsm", tag)], writes=[("bslo", tag)])
                            yield
                            p.op("dve", lambda e: e.tensor_tensor(out=bs[:, 3:4], in0=bs[:, 0:1], in1=bs[:, 2:3], op=ALU.subtract),
                                 reads=[("bsM", tag), ("bslo", tag)], writes=[("bsW", tag)])
                            yield
                            p.op("dve", lambda e: e.tensor_scalar(out=bs[:, 32:64], in0=pw2[:, :], scalar1=bs[:, 3:4], scalar2=None,
                                                                   op0=ALU.mult), reads=[("bsW", tag), "pw2"], writes=[("bswd", tag)])
                            yield
                            p.op("dve", lambda e: e.tensor_tensor(out=bs[:, 4:5], in0=bs[:, 2:3], in1=bs[:, 32:33], op=ALU.add),
                                 reads=[("bslo", tag), ("bswd", tag)], writes=[("bsmid", tag)])
                            yield

                        def stage_B(j, score, maskb, bs, tag):
                            N = 128 * (2 * j + 2)
                            skey = ("score", tag)
                            mkey = ("maskb", tag)
                            for it in range(NIT):
                                p.op("dve", lambda e: e.tensor_scalar(
                                    out=maskb[:, 0:N], in0=score[:, 0:N], scalar1=bs[:, 4:5], scalar2=None,
                                    op0=ALU.is_ge, op1=ALU.add, accum_out=bs[:, 5:6]),
                                    reads=[skey, ("bsmid", tag), mkey], writes=[mkey, ("bscnt", tag)])
                                yield
                                last = it == NIT - 1
                                p.op("dve", lambda e, last=last: e.tensor_scalar(
                                    out=bs[:, 6:7], in0=bs[:, 5:6], scalar1=TOPK, scalar2=(-1.0 if last else -0.5),
                                    op0=ALU.is_ge, op1=ALU.add), reads=[("bscnt", tag)], writes=[("bsge", tag)])
                                yield
                                p.op("dve", lambda e, it=it: e.scalar_tensor_tensor(
                                    out=bs[:, 4:5], in0=bs[:, 6:7], scalar=bs[:, 32 + it:33 + it], in1=bs[:, 4:5],
                                    op0=ALU.mult, op1=ALU.add),
                                    reads=[("bsge", tag), ("bswd", tag), ("bsmid", tag)], writes=[("bsmid", tag)])
                                yield
                            if debug and j == 1:
                                dbg_dump("tau1", bs[:, 0:8], ("bsmid", tag))
                            p.op("dve", lambda e: e.tensor_scalar(
                                out=maskb[:, 0:N], in0=score[:, 0:N], scalar1=bs[:, 4:5], scalar2=MASKV,
                                op0=ALU.is_lt, op1=ALU.mult), reads=[skey, ("bsmid", tag), mkey], writes=[mkey])
                            yield

                        def oacc(h, n=129):
                            return ps[:, 5 + h // 3, (h % 3) * 129:(h % 3) * 129 + n]

                        def stage_C(j, maskb, tag):
                            L = 2 * j + 2
                            mkey = ("maskb", tag)
                            for kt in range(L):
                                for g in range(2):
                                    mm(ps[:, 4, :], KT[:, g, kt * 128:(kt + 1) * 128],
                                       qT[:, j, 4 * g:4 * g + 4, :], True, False,
                                       ["KT", "qT"], bank(4), False)
                                    mm(ps[:, 4, :], maskb[:, kt * 128:(kt + 1) * 128], I4[:, :], False, True,
                                       [mkey, "I4"], bank(4), True)
                                    ui = cnt_ui[0]
                                    cnt_ui[0] += 1
                                    pt = PT[ui % 2]
                                    pkey = ("PT", ui % 2)
                                    p.op("act", lambda e, pt=pt: e.activation(out=pt[:], in_=ps[:, 4, :], func=AF.Exp,
                                                                              scale=ATT_SCALE),
                                         reads=bank(4), writes=[pkey])
                                    for hh in range(4):
                                        h = 4 * g + hh
                                        lastmm = (kt == L - 1) and (g == 1) and (hh == 3)
                                        mm(oacc(h), pt[:, hh * 128:(hh + 1) * 128], Vaug[:, kt, g, :],
                                           kt == 0 and h % 3 == 0, kt == L - 1, [pkey, "Vaug"], bank(5, 3),
                                           lastmm or hh == 3)
                                    yield
                            for bk in range(3):
                                nh = 3 if bk < 2 else 2
                                den = ps[:, 5 + bk, 0:nh * 129].rearrange("p (h c) -> p h c", c=129)
                                p.op("dve", lambda e, den=den, bk=bk, nh=nh: e.reciprocal(
                                    out=rcT[:, 3 * bk:3 * bk + nh], in_=den[:, :, 128]),
                                    reads=bank(5, 3), writes=[("rc", bk)])
                                yield
                                p.op("dve", lambda e, den=den, bk=bk, nh=nh: e.tensor_tensor(
                                    out=attn32[:, 384 * bk:384 * bk + 128 * nh].rearrange("p (h d) -> p h d", d=128),
                                    in0=den[:, :, 0:128],
                                    in1=rcT[:, 3 * bk:3 * bk + nh].unsqueeze(2).to_broadcast([128, nh, 128]),
                                    op=ALU.mult), reads=bank(5, 3) + [("rc", bk)], writes=["attn32"])
                                yield
                            p.op("act", lambda e: e.activation(out=mg[:, 1024:2048], in_=attn32[:], func=AF.Square,
                                                               accum_out=st[:, 128 + j:129 + j]),
                                 reads=["attn32"], writes=["mg1", ("st", 128 + j)])
                            p.op("dve", lambda e: e.tensor_tensor(out=mg[:, 1024:2048], in0=attn32[:], in1=gb[:, 1024:2048],
                                                                  op=ALU.mult), reads=["attn32", "gb1"], writes=["mg1"])
                            yield
                            if debug and j == 1:
                                dbg_dump("attn1", attn32[:], "attn32")
                            var = 0 if j == 0 else 1
                            psP = psf[:, 2048:3072].rearrange("p (c t) -> p c t", t=128)
                            for cch in range(8):
                                g = cch // 2
                                mm(psP[:, cch, :], u[:, j, cch * 128:(cch + 1) * 128], Mm[:, var, g, :], True, False,
                                   ["u", "Mm"], bank(4, 2), False)
                                mm(psP[:, cch, 0:16], uh[:, cch * 128:(cch + 1) * 128], Hf[:, j, g, :], False, True,
                                   ["u", "Hf"], bank(4, 2), cch == 7)
                            p.op("act", lambda e: e.activation(out=pooledT[:].rearrange("p c t -> p (c t)"), in_=psf[:, 2048:3072],
                                                               func=AF.Copy), reads=bank(4, 2), writes=["pooledT"])
                            for g in range(4):
                                for cc in range(2):
                                    mm(psf[:, 3072 + g * 256:3072 + (g + 1) * 256], pooledT[:, 2 * g + cc, :], wps[:, g, cc, :],
                                       cc == 0, cc == 1, ["pooledT", "wps"], bank(6, 2), (g == 3 and cc == 1))
                            p.op("act", lambda e: e.activation(out=mg[:, 0:1024], in_=psf[:, 3072:4096], func=AF.Square,
                                                               accum_out=st[:, 136 + j:137 + j]),
                                 reads=bank(6, 2), writes=["mg0", ("st", 136 + j)])
                            p.op("dve", lambda e: e.tensor_tensor(out=mg[:, 0:1024], in0=psf[:, 3072:4096], in1=gb[:, 0:1024],
                                                                  op=ALU.mult), reads=bank(6, 2) + ["gb0"], writes=["mg0"])
                            yield
                            pv = psb[:, 4096:6144].rearrange("p (c t) -> p c t", t=128)
                            for c in range(16):
                                p.op("pe", lambda e, c=c: e.transpose(out=pv[:, c, :], in_=mg[:, c * 128:(c + 1) * 128],
                                                                      identity=identb[:]),
                                     reads=["mg0", "mg1", "identb"], writes=bank(4, 2), signal=(c == 15))
                            p.op("act", lambda e: e.activation(out=big16[:, :, j * 128:(j + 1) * 128], in_=pv,
                                                               func=AF.Copy),
                                 reads=bank(4, 2), writes=[("b16", j)])
                            yield

                        def run_rr(gens):
                            gens = [g for g in gens if g is not None]
                            while gens:
                                for g in list(gens):
                                    try:
                                        next(g)
                                    except StopIteration:
                                        gens.remove(g)

                        def chain(*gs):
                            for g in gs:
                                yield from g

                        pairs = [(0, 7), (1, 6), (2, 5), (3, 4)]

                        def bufs(q, i):
                            s = q % 2
                            return scoreS[s][i], maskS[s][i], bsS[s][i], (s, i)

                        def A_pair(q):
                            return chain(*[stage_A(pairs[q][i], bufs(q, i)[0], bufs(q, i)[2], bufs(q, i)[3]) for i in range(2)])

                        def C_pair(q):
                            return chain(*[stage_C(pairs[q][i], bufs(q, i)[1], bufs(q, i)[3]) for i in range(2)])

                        def B_one(q, i):
                            sc, mk, bs_, tag = bufs(q, i)
                            return stage_B(pairs[q][i], sc, mk, bs_, tag)

                        def ada_rest(n):
                            for _ in range(n):
                                ci = cnt_ci[0]
                                cnt_ci[0] += 1
                                ada_group(2 * (ci % 2))
                                for _ in range(3):
                                    yield

                        run_rr([A_pair(0), ada_rest(2)])
                        for q in range(4):
                            run_rr([B_one(q, 0), B_one(q, 1),
                                    A_pair(q + 1) if q + 1 < 4 else None,
                                    C_pair(q - 1) if q >= 1 else None,
                                    ada_rest(4 if q < 3 else 2)])
                        run_rr([C_pair(3)])
                        assert ada_n[0] == 24
                        if debug:
                            dbg_dump("mergedT", big16[:], ("b16", 7))
                            dbg_dump("st", st[:], ("st", 143))
                        p.barrier()
                        if stage == "p3":
                            return finish()
            with scope() as P4:
                mix = T(P4, "mix", [128, 8, D], F32)
                gg = T(P4, "gg", [128, D], F32)
                gt = T(P4, "gt", [128, D], F32)
                xt = [T(P4, "xt4_%d" % i, [128, D], F32) for i in range(2)]
                xnb = [T(P4, "xnb4_%d" % i, [128, D], BF16) for i in range(2)]
                colload(80, 3 * D, "c_sh2")
                colload(64, 4 * D, "c_sc2")
                p.op("dve", lambda e: e.scalar_tensor_tensor(out=cols[:, 64:80], in0=cols[:, 64:80], scalar=1.0,
                                                              in1=cols[:, 16:32], op0=ALU.add, op1=ALU.mult),
                     reads=["c_sc2", "cols_g", "c_sh2"], writes=["c_sc2", "cols_m"])
                p.dma("sp", gg[:], modD[0:1, 2 * D:3 * D].partition_broadcast(128), reads=["modD"], writes=["gg"])
                p.dma("sp", gt[:], g_post_mix[0:1, :].partition_broadcast(128), writes=["gt"])
                p.op("dve", lambda e: e.tensor_tensor(out=gg[:], in0=gg[:], in1=gt[:], op=ALU.mult),
                     reads=["gg", "gt"], writes=["gg"])
                rstd_batch(st[:, 128:144], st[:, 144:160], 1024, [("st", 128 + i) for i in range(16)], "rs_pa", st[:, 160:176])
                for cg in range(4):
                    W, wkey = ring_get("wout")
                    for j in range(8):
                        bA = (2 * j) % 8
                        bB = (2 * j + 1) % 8
                        for k in range(8):
                            mm(ps[:, bA, :], big16[:, k, j * 128:(j + 1) * 128], W[:, k, :], k == 0, k == 7,
                               [wkey, ("b16", j)], bank(bA), k == 7)
                        for k in range(8, 16):
                            mm(ps[:, bB, :], big16[:, k, j * 128:(j + 1) * 128], W[:, k, :], k == 8, k == 15,
                               [wkey, ("b16", j)], bank(bB), k == 15)
                        p.op("act", lambda e, j=j, cg=cg, bA=bA: e.activation(
                            out=mix[:, j, cg * 512:(cg + 1) * 512], in_=ps[:, bA, :], func=AF.Identity,
                            scale=st[:, 152 + j:153 + j]), reads=bank(bA) + ["rs_pa"], writes=[("mix", j)])
                        p.op("dve", lambda e, j=j, cg=cg, bB=bB: e.scalar_tensor_tensor(
                            out=mix[:, j, cg * 512:(cg + 1) * 512], in0=ps[:, bB, :], scalar=st[:, 144 + j:145 + j],
                            in1=mix[:, j, cg * 512:(cg + 1) * 512], op0=ALU.mult, op1=ALU.add),
                            reads=bank(bB) + ["rs_pa", ("mix", j)], writes=[("mix", j)])
                    ring_done()
                p.barrier()
                for j in range(8):
                    p.op("act", lambda e, j=j: e.activation(out=xnb[j % 2][:], in_=mix[:, j, :], func=AF.Square,
                                                            accum_out=st[:, 176 + j:177 + j]),
                         reads=[("mix", j)], writes=[("xnb", j % 2), ("st", 176 + j)])
                rstd_batch(st[:, 176:184], st[:, 184:192], D, [("st", 176 + i) for i in range(8)], "rs_m", st[:, 192:200])
                for j in range(8):
                    p.dma("sp", xt[j % 2][:], x_own[j * 128:(j + 1) * 128, :], writes=[("xt", j % 2)])
                    p.op("dve", lambda e, j=j: e.scalar_tensor_tensor(
                        out=mix[:, j, :], in0=mix[:, j, :], scalar=st[:, 184 + j:185 + j], in1=gg[:],
                        op0=ALU.mult, op1=ALU.mult), reads=[("mix", j), "rs_m", "gg"], writes=[("mix", j)])
                    p.op("dve", lambda e, j=j: e.tensor_tensor(out=mix[:, j, :], in0=mix[:, j, :], in1=xt[j % 2][:], op=ALU.add),
                         reads=[("mix", j), ("xt", j % 2)], writes=[("mix", j)])
                    p.dma("sp", x1D[j * 128:(j + 1) * 128, :], mix[:, j, :], reads=[("mix", j)], writes=["x1D"])
                    p.op("act", lambda e, j=j: e.activation(out=xnb[j % 2][:], in_=mix[:, j, :], func=AF.Square,
                                                            accum_out=st[:, 200 + j:201 + j]),
                         reads=[("mix", j)], writes=[("xnb", j % 2), ("st", 200 + j)])
                rstd_batch(st[:, 200:208], st[:, 208:216], D, [("st", 200 + i) for i in range(8)], "rs_2", st[:, 216:224])
                if debug:
                    dbg_dump("x1", mix[:], ("mix", 7))
                for j0 in range(0, 8, 2):
                    srcs = [("sbuf", (mix[:, j0 + i, :], ("mix", j0 + i)), 0) for i in range(2)]
                    dview = big16[:, :, j0 * 128:(j0 + 2) * 128]
                    dkeys[id(dview)] = "h2T"
                    norm_transpose_pair(srcs, None, xnb, dview, 64, 80, "b", 4 * ((j0 // 2) % 2),
                                        rstd_known=[(st[:, 208 + j0 + i:209 + j0 + i], "rs_2") for i in range(2)])
                if debug:
                    dbg_dump("h2T", big16[:], "h2T")
                p.barrier()
                if stage == "p4":
                    return finish()
            with scope() as P5:
                fT = T(P5, "fT", [128, 64, 1024], BF16)
                r32 = [T(P5, "r32_%d" % i, [128, 512], F32) for i in range(2)]
                ei = 0
                for fg in range(16):
                    W, wkey = ring_get("ff1")
                    for fc in range(4):
                        jc = fg * 4 + fc
                        for tg in range(2):
                            b = ei % 4
                            for k in range(16):
                                mm(ps[:, b, :], W[:, k, fc * 128:(fc + 1) * 128], big16[:, k, tg * 512:(tg + 1) * 512],
                                   k == 0, k == 15, [wkey, "h2T"], bank(b), k == 15)
                            r = r32[ei % 2]
                            rkey = ("r32", ei % 2)
                            ei += 1
                            p.op("act", lambda e, r=r, b=b: e.activation(out=r[:], in_=ps[:, b, :], func=AF.Relu),
                                 reads=bank(b), writes=[rkey])
                            p.op("dve", lambda e, r=r, jc=jc, tg=tg: e.tensor_tensor(
                                out=fT[:, jc, tg * 512:(tg + 1) * 512], in0=r[:], in1=r[:], op=ALU.mult),
                                reads=[rkey], writes=["fT"])
                    ring_done()
                if debug:
                    dbg_dump("fT", fT[:, 0:4, :], "fT")
                p.barrier()
                if stage == "p5":
                    return finish()
                fo = big16[:].rearrange("p a b -> p (a b)").rearrange("p (j d) -> p j d", d=D)
                jk = T(P5, "jk", [128, 512], BF16)
                for cg in range(4):
                    for jg in range(4):
                        W, wkey = ring_get("ff2")
                        for jj in range(16):
                            jc = jg * 16 + jj
                            for tt in range(8):
                                mm(ps[:, tt, :], fT[:, jc, tt * 128:(tt + 1) * 128], W[:, jj, :], jc == 0, jc == 63,
                                   [wkey, "fT"], bank(tt), jc == 63 or (jj == 15 and tt == 7))
                        ring_done()
                    for tt in range(8):
                        if tt % 2 == 0:
                            p.op("act", lambda e, tt=tt, cg=cg: e.activation(
                                out=fo[:, tt, cg * 512:(cg + 1) * 512], in_=ps[:, tt, :], func=AF.Copy),
                                reads=bank(tt), writes=[("fo", tt)])
                        else:
                            p.op("dve", lambda e, tt=tt, cg=cg: e.tensor_copy(
                                out=fo[:, tt, cg * 512:(cg + 1) * 512], in_=ps[:, tt, :]),
                                reads=bank(tt), writes=[("fo", tt)])
                if debug:
                    dbg_dump("fo", big16[:], ("fo", 7))
                p.barrier()
                if stage == "p6":
                    return finish()
            with scope() as P7:
                gg7 = T(P7, "gg7", [128, D], F32)
                gt7 = T(P7, "gt7", [128, D], F32)
                xt7 = [T(P7, "xt7_%d" % i, [128, D], F32) for i in range(2)]
                ot7 = [T(P7, "ot7_%d" % i, [128, D], F32) for i in range(2)]
                fo = big16[:].rearrange("p a b -> p (a b)").rearrange("p (j d) -> p j d", d=D)
                p.dma("sp", gg7[:], modD[0:1, 5 * D:6 * D].partition_broadcast(128), reads=["modD"], writes=["gg7"])
                p.dma("sp", gt7[:], g_post_ffn[0:1, :].partition_broadcast(128), writes=["gt7"])
                p.op("dve", lambda e: e.tensor_tensor(out=gg7[:], in0=gg7[:], in1=gt7[:], op=ALU.mult),
                     reads=["gg7", "gt7"], writes=["gg7"])
                for tt in range(8):
                    p.op("act", lambda e, tt=tt: e.activation(out=ot7[tt % 2][:], in_=fo[:, tt, :], func=AF.Square,
                                                              accum_out=st[:, 224 + tt:225 + tt]),
                         reads=[("fo", tt)], writes=[("ot", tt % 2), ("st", 224 + tt)])
                rstd_batch(st[:, 224:232], st[:, 72:80], D, [("st", 224 + i) for i in range(8)], "rs_f", st[:, 80:88])
                for tt in range(8):
                    p.dma("sp", xt7[tt % 2][:], x1D[tt * 128:(tt + 1) * 128, :], reads=["x1D"], writes=[("xt7", tt % 2)])
                    o = ot7[tt % 2]
                    p.op("dve", lambda e, tt=tt, o=o: e.scalar_tensor_tensor(
                        out=o[:], in0=fo[:, tt, :], scalar=st[:, 72 + tt:73 + tt], in1=gg7[:],
                        op0=ALU.mult, op1=ALU.mult), reads=[("fo", tt), "rs_f", "gg7"], writes=[("ot", tt % 2)])
                    p.op("dve", lambda e, tt=tt, o=o: e.tensor_tensor(out=o[:], in0=o[:], in1=xt7[tt % 2][:], op=ALU.add),
                         reads=[("ot", tt % 2), ("xt7", tt % 2)], writes=[("ot", tt % 2)])
                    final.append(p.dma("sp", y[tt * 128:(tt + 1) * 128, :], o[:], reads=[("ot", tt % 2)]))
                return finish()


def own_blocks(ty):
    return [2 * j + ty if j < 4 else 2 * j + 1 - ty for j in range(8)]


def _pool_mats(first_is_block0):
    wins = (2, 4, 8, 16)
    Mm = np.zeros((128, 2, 4, 128), np.float32)
    for var in range(2):
        blk0 = (var == 0 and first_is_block0)
        for g, w in enumerate(wins):
            for t in range(128):
                cnt = min(t + 1, w) if blk0 else w
                lo = max(t + 1 - w, 0)
                Mm[lo:t + 1, var, g, t] = 1.0 / cnt
                Mm[t, var, g, t] -= 1.0
    Hf = np.zeros((128, 8, 4, 16), np.float32)
    for j in range(8):
        if j == 0 and first_is_block0:
            continue
        for g, w in enumerate(wins):
            for t in range(16):
                for r in range(16):
                    off = r - 16
                    if t - w + 1 <= off:
                        Hf[16 * j + r, j, g, t] = 1.0 / w
    return Mm, Hf


def host_inputs(inputs):
    x = np.asarray(inputs["x"], np.float32)
    c = np.asarray(inputs["c"], np.float32)
    f = lambda k: np.ascontiguousarray(np.asarray(inputs[k], np.float32)[0])
    shared = {
        "w_ada": f("w_ada"), "b_ada": f("b_ada")[None, :],
        "g_post_mix": f("g_post_mix")[None, :], "g_post_ffn": f("g_post_ffn")[None, :],
        "w_in": f("w_in"), "w_pool": f("w_pool"), "pool_scale": f("pool_scale")[None, :],
        "g_pool_out": f("g_pool_out")[None, :], "g_attn_out": f("g_attn_out")[None, :],
        "w_out": f("w_out"), "w_ff1": f("w_ff1"), "w_ff2": f("w_ff2"),
        "I4": np.ascontiguousarray(np.tile(np.eye(128, dtype=np.float32), (1, 4))),
        "pw2": np.ascontiguousarray(np.tile((0.5 ** np.arange(1, 33, dtype=np.float64)).astype(np.float32)[None, :], (128, 1))),
    }
    gpre1 = f("g_pre_mix").reshape(16, 128).T
    gpre2 = f("g_pre_ffn").reshape(16, 128).T
    shared["vcols"] = np.ascontiguousarray(np.concatenate([gpre1, gpre2], axis=1))
    in_maps = []
    for core in range(NCORES):
        b, ty = core // 2, core % 2
        own = own_blocks(ty)
        oth = own_blocks(1 - ty)
        xb = x[b].reshape(16, 128, D)
        m = dict(shared)
        m["x_own"] = np.ascontiguousarray(xb[own].reshape(1024, D))
        m["x_oth"] = np.ascontiguousarray(xb[oth].reshape(1024, D))
        halo = np.zeros((128, D), np.float32)
        for j, blk in enumerate(own):
            if blk > 0:
                halo[16 * j:16 * j + 16] = x[b, blk * 128 - 16:blk * 128]
        m["x_halo"] = halo
        m["c_col"] = np.ascontiguousarray(c[b].reshape(16, 128).T)
        kpos = np.zeros(S, np.float64)
        for j in range(8):
            kpos[(2 * j) * 128:(2 * j + 1) * 128] = own[j] * 128 + np.arange(128)
            kpos[(2 * j + 1) * 128:(2 * j + 2) * 128] = oth[j] * 128 + np.arange(128)
        m["nkB"] = np.ascontiguousarray(np.tile((-kpos * BIG).astype(np.float32)[None, :], (128, 1)))
        qpos = np.array(own, np.float64)[None, :] * 128 + np.arange(128)[:, None]
        m["qB"] = np.ascontiguousarray(((qpos + 0.5) * BIG).astype(np.float32))
        Mm, Hf = _pool_mats(own[0] == 0)
        m["Mm"] = np.ascontiguousarray(Mm.reshape(128, -1))
        m["Hf"] = np.ascontiguousarray(Hf.reshape(128, -1))
        in_maps.append(m)
    return in_maps


_NC_CACHE = {}


def kernel(**inputs):
    in_maps = host_inputs(inputs)
    if "nc" not in _NC_CACHE:
        _NC_CACHE["nc"] = build_program()
    nc = _NC_CACHE["nc"]
    res = run_bass_kernel_spmd(nc, in_maps, core_ids=list(range(NCORES)))
    out = np.zeros((4, 16, 128, D), np.float32)
    for core in range(NCORES):
        b, ty = core // 2, core % 2
        yc = np.asarray(res.results[core]["y"], np.float32).reshape(8, 128, D)
        for j, blk in enumerate(own_blocks(ty)):
            out[b, blk] = yc[j]
    return out.reshape(4, S, D)
```

```python
import bisect
from contextlib import ExitStack

import numpy as np
import concourse.bass as bass
import concourse.mybir as mybir
from concourse.bass_utils import run_bass_kernel_spmd

F32 = mybir.dt.float32
BF16 = mybir.dt.bfloat16
ALU = mybir.AluOpType
AF = mybir.ActivationFunctionType
AX = mybir.AxisListType

D = 2048
S = 2048
DFF = 8192
IN_W = 4752
NCORES = 8
NIT = 24
TOPK = 256.0
EPS = 1e-6
MASKV = -30000.0
BIG = 1e30
IDX_SCALE = (16 ** -0.5) * (128 ** -0.5)
ATT_SCALE = 128 ** -0.5
NDSEM = 24
NRING = 2


class Prog:
    ENG = ("pe", "act", "dve", "pool", "sp")

    def __init__(self, nc):
        self.nc = nc
        self.ops = {e: [] for e in self.ENG}
        self.sig_seq = {e: [] for e in self.ENG}
        self.nseq = {e: 0 for e in self.ENG}
        self.wr = {}
        self.rd = {}
        self.dslot_next = {"sp": 0, "pool": 0}
        self.dslot_val = [0] * NDSEM
        self.floor = []

    def _deps(self, eng, reads, writes):
        deps = list(self.floor)
        for k in reads:
            deps.extend(self.wr.get(k, {}).values())
        for k in writes:
            deps.extend(self.wr.get(k, {}).values())
            deps.extend(self.rd.get(k, {}).values())
        return [t for t in deps if not (t[0] == "c" and t[1] == eng and eng == "pe")]

    def _record(self, tok, who, reads, writes):
        for k in reads:
            self.rd.setdefault(k, {})[who] = tok
        for k in writes:
            self.wr.setdefault(k, {})[who] = tok

    def op(self, eng, fn, reads=(), writes=(), signal=True):
        deps = self._deps(eng, reads, writes)
        seq = self.nseq[eng]
        self.nseq[eng] += 1
        if signal:
            self.sig_seq[eng].append(seq)
        tok = ("c", eng, seq)
        self.ops[eng].append(dict(kind="c", fn=fn, deps=deps, seq=seq, signal=signal))
        self._record(tok, eng, reads, writes)
        return tok

    def dma(self, eng, out, in_, reads=(), writes=(), **kw):
        deps = self._deps(eng, reads, writes)
        half = NDSEM // 2
        base = 0 if eng == "sp" else half
        slot = base + self.dslot_next[eng]
        self.dslot_next[eng] = (self.dslot_next[eng] + 1) % half
        prev = self.dslot_val[slot]
        self.dslot_val[slot] = prev + 16
        tok = ("d", slot, prev + 16)
        if prev:
            deps.append(("d", slot, prev))
        self.ops[eng].append(dict(kind="d", out=out, in_=in_, deps=deps, slot=slot, kw=kw,
                                  seq=self.nseq[eng]))
        self._record(tok, "dma%d" % slot, reads, writes)
        return tok

    def barrier(self):
        fl = []
        for e in ("pe", "act", "dve", "pool"):
            if self.nseq[e]:
                assert self.sig_seq[e] and self.sig_seq[e][-1] == self.nseq[e] - 1, e
                fl.append(("c", e, self.nseq[e] - 1))
        for s in range(NDSEM):
            if self.dslot_val[s]:
                fl.append(("d", s, self.dslot_val[s]))
        self.floor = fl

    def emit(self, final_tokens=()):
        nc = self.nc
        with ExitStack() as es:
            csem = {e: es.enter_context(nc.semaphore("s_" + e)) for e in self.ENG if e != "sp"}
            dsem = [es.enter_context(nc.semaphore("d%d" % i)) for i in range(NDSEM)]
            block = es.enter_context(nc.Block())
            sig_seq = self.sig_seq

            def resolve(tok):
                if tok[0] == "d":
                    return dsem[tok[1]], tok[2]
                _, e, seq = tok
                i = bisect.bisect_left(sig_seq[e], seq)
                assert i < len(sig_seq[e]), ("no signalling op after", tok)
                return csem[e], i + 1

            def run(ename):
                def body(engine):
                    waited = {}
                    for o in self.ops[ename]:
                        for t in o["deps"]:
                            if t[0] == "c" and t[1] == ename:
                                i = bisect.bisect_left(sig_seq[ename], t[2])
                                assert i < len(sig_seq[ename]) and sig_seq[ename][i] < o["seq"], \
                                    ("same-engine dep on unsignalled op", ename, t)
                            sem, val = resolve(t)
                            if waited.get(id(sem), 0) >= val:
                                continue
                            waited[id(sem)] = val
                            engine.wait_ge(sem, val)
                        if o["kind"] == "c":
                            ins = o["fn"](engine)
                            if o["signal"]:
                                ins.then_inc(csem[ename], 1)
                        else:
                            engine.dma_start(out=o["out"], in_=o["in_"], **o["kw"]).then_inc(
                                dsem[o["slot"]], 16)
                    if ename == "sp":
                        for t in final_tokens:
                            sem, val = resolve(t)
                            engine.wait_ge(sem, val)
                return body

            block.tensor(run("pe"))
            block.scalar(run("act"))
            block.vector(run("dve"))
            block.gpsimd(run("pool"))
            block.sync(run("sp"))


def build_program(debug=False, stage=None):
    nc = bass.Bass("TRN2", target_bir_lowering=False)
    dt_in = lambda name, shape: nc.dram_tensor(name, shape, F32, kind="ExternalInput").ap()
    x_own = dt_in("x_own", [1024, D])
    x_oth = dt_in("x_oth", [1024, D])
    x_halo = dt_in("x_halo", [128, D])
    c_col = dt_in("c_col", [128, 16])
    vcols = dt_in("vcols", [128, 32])
    nkB_d = dt_in("nkB", [128, S])
    qB_d = dt_in("qB", [128, 8])
    pw2_d = dt_in("pw2", [128, 32])
    Mm_d = dt_in("Mm", [128, 2 * 4 * 128])
    Hf_d = dt_in("Hf", [128, 8 * 4 * 16])
    I4_d = dt_in("I4", [128, 512])
    w_ada = dt_in("w_ada", [D, 6 * D])
    b_ada = dt_in("b_ada", [1, 6 * D])
    g_post_mix = dt_in("g_post_mix", [1, D])
    g_post_ffn = dt_in("g_post_ffn", [1, D])
    w_in = dt_in("w_in", [D, IN_W])
    w_pool = dt_in("w_pool", [4, 256, 256])
    pool_scale = dt_in("pool_scale", [1, 1024])
    g_pool_out = dt_in("g_pool_out", [1, 1024])
    g_attn_out = dt_in("g_attn_out", [1, 1024])
    w_out = dt_in("w_out", [D, D])
    w_ff1 = dt_in("w_ff1", [D, DFF])
    w_ff2 = dt_in("w_ff2", [DFF, D])
    y = nc.dram_tensor("y", [1024, D], F32, kind="ExternalOutput").ap()
    modD = nc.dram_tensor("modD", [1, 6 * D], F32).ap()
    x1D = nc.dram_tensor("x1D", [1024, D], F32).ap()
    dbg = {}
    if debug:
        for nm, shp in debug.items():
            dbg[nm] = nc.dram_tensor("dbg_" + nm, shp, F32, kind="ExternalOutput").ap()

    p = Prog(nc)
    final = []

    def finish():
        p.emit(final_tokens=final)
        return nc

    ARENA_BYTES = 210944
    arena_cm = nc.sbuf_tensor("arena", [128, ARENA_BYTES // 2], BF16)
    arena_t = arena_cm.__enter__()
    astate = dict(off=0, peak=0)

    def T(es, name, shape, dt):
        n = 1
        for s_ in shape[1:]:
            n *= s_
        nb = n * (4 if dt == F32 else 2)
        off = (astate["off"] + 63) // 64 * 64
        assert off + nb <= ARENA_BYTES, ("SBUF arena overflow", name, off + nb)
        astate["off"] = off + nb
        astate["peak"] = max(astate["peak"], off + nb)
        ap = arena_t[:, off // 2:(off + nb) // 2]
        if dt == F32:
            ap = ap.bitcast(F32)
        if len(shape) == 3:
            ap = ap.rearrange("p (a b) -> p a b", a=shape[1])
        elif len(shape) == 4:
            ap = ap.rearrange("p (a b c) -> p a b c", a=shape[1], b=shape[2])
        return ap

    def scope():
        es = ExitStack()
        m = astate["off"]

        def rel():
            astate["off"] = m
        es.callback(rel)
        return es

    def bank(b, n=1):
        return ["pb%d" % i for i in range(b, b + n)]

    with ExitStack() as G:
        ps = G.enter_context(nc.psum_tensor("ps", [128, 8, 512], F32))
        psf = ps[:].rearrange("p a b -> p (a b)")
        psb = ps[:].bitcast(BF16).rearrange("p a b -> p (a b)")
        ring = [T(G, "ring%d" % i, [128, 16, 512], BF16) for i in range(NRING)]
        st = T(G, "st", [128, 256], F32)
        cols = T(G, "cols", [128, 96], F32)
        identb = T(G, "identb", [128, 128], BF16)
        p.op("dve", lambda e: e.memset(st[:], 0.0), writes=["st"])
        p.dma("sp", cols[:, 0:32], vcols[:, :], writes=["cols_g"])
        p.dma("pool", identb[:], I4_d[:, 0:128], writes=["identb"])

        sched = []
        for cg in range(8):
            sched.append(("ada", w_ada[:, cg * 512:(cg + 1) * 512], 512))
        sched.append(("inA", w_in[:, 2048:2560], 512))
        sched.append(("inB", w_in[:, 4608:4752], 144))
        for i in range(2):
            sched.append(("q", w_in[:, 1024 + 512 * i:1536 + 512 * i], 512))
        for i in range(4):
            sched.append(("iq", w_in[:, 2560 + 512 * i:3072 + 512 * i], 512))
        for i in range(2):
            sched.append(("pool", w_in[:, 512 * i:512 * (i + 1)], 512))
        for cg in range(8, 24):
            sched.append(("ada", w_ada[:, cg * 512:(cg + 1) * 512], 512))
        for cg in range(4):
            sched.append(("wout", w_out[:, cg * 512:(cg + 1) * 512], 512))
        for fg in range(16):
            sched.append(("ff1", w_ff1[:, fg * 512:(fg + 1) * 512], 512))
        for cg in range(4):
            for jg in range(4):
                sched.append(("ff2", w_ff2[jg * 2048:(jg + 1) * 2048, cg * 512:(cg + 1) * 512], 512))
        rstate = dict(loaded=0, used=0)

        def ring_load():
            i = rstate["loaded"]
            if i >= len(sched):
                return
            name, src, ncol = sched[i]
            buf = ring[i % NRING]
            p.dma("pool", buf[:, :, 0:ncol], src.rearrange("(k p) c -> p k c", p=128),
                  writes=[("ring", i % NRING)])
            rstate["loaded"] += 1

        def ring_get(name):
            i = rstate["used"]
            assert sched[i][0] == name, (sched[i][0], name)
            while rstate["loaded"] <= i:
                ring_load()
            return ring[i % NRING], ("ring", i % NRING)

        def ring_done():
            rstate["used"] += 1
            ring_load()

        for _ in range(NRING):
            ring_load()

        def mm(out, lhsT, rhs, start, stop, reads, writes, signal):
            p.op("pe", lambda e: e.matmul(out, lhsT=lhsT, rhs=rhs, start=start, stop=stop),
                 reads=reads, writes=writes, signal=signal)

        def rstd_batch(ss_ap, out_ap, n, key_in, key_out, tmp_ap):
            p.op("dve", lambda e: e.tensor_scalar(out=tmp_ap, in0=ss_ap, scalar1=1.0 / n, scalar2=EPS,
                                                   op0=ALU.mult, op1=ALU.add),
                 reads=(key_in if isinstance(key_in, list) else [key_in]), writes=[key_out + "_t"])
            p.op("act", lambda e: e.activation(out=tmp_ap, in_=tmp_ap, func=AF.Sqrt),
                 reads=[key_out + "_t"], writes=[key_out + "_t"])
            p.op("dve", lambda e: e.reciprocal(out=out_ap, in_=tmp_ap),
                 reads=[key_out + "_t"], writes=[key_out])

        def norm_transpose_pair(srcs, xt, xnb, dst, gcol, scol, tagbase, pbase, rstd_known=None):
            n = len(srcs)
            pv = psb[:, pbase * 1024:(pbase + 4) * 1024].rearrange("p (k t) -> p k t", k=16)
            for i, (kind, src, sc) in enumerate(srcs):
                if kind == "dram":
                    xa = xt[i]
                    xkey = ("xt", i)
                    p.dma("sp", xa[:], src, writes=[xkey])
                    xin = xa[:]
                else:
                    xin, xkey = src
                nb = xnb[i]
                nkey = ("xnb", i)
                if rstd_known is None:
                    p.op("act", lambda e, xin=xin, nb=nb, sc=sc: e.activation(
                        out=nb[:], in_=xin, func=AF.Square, accum_out=st[:, sc:sc + 1]),
                        reads=[xkey], writes=[nkey, ("st", sc)])
                    rstd_batch(st[:, sc:sc + 1], st[:, sc + 32:sc + 33], D, ("st", sc), "rs%s%d" % (tagbase, sc),
                               st[:, sc + 64:sc + 65])
                    rkey = "rs%s%d" % (tagbase, sc)
                    rap = st[:, sc + 32:sc + 33]
                else:
                    rap, rkey = rstd_known[i]
                p.op("dve", lambda e, xin=xin, nb=nb, rap=rap: e.tensor_scalar(
                    out=nb[:], in0=xin, scalar1=rap, scalar2=None, op0=ALU.mult),
                    reads=[xkey, rkey], writes=[nkey])
                for k in range(16):
                    p.op("pe", lambda e, k=k, i=i, nb=nb: e.transpose(
                        out=pv[:, k, i * 128:(i + 1) * 128], in_=nb[:, k * 128:(k + 1) * 128], identity=identb[:]),
                        reads=[nkey, "identb"], writes=bank(pbase, 4), signal=(k == 15))
            w = n * 128
            if gcol is None:
                for kb in range(4):
                    o_ = dst[:, 4 * kb:4 * kb + 4, 0:w]
                    i_ = pv[:, 4 * kb:4 * kb + 4, 0:w]
                    if kb % 2 == 0:
                        p.op("act", lambda e, o_=o_, i_=i_: e.activation(out=o_, in_=i_, func=AF.Copy),
                             reads=bank(pbase + kb), writes=[dst_key(dst)])
                    else:
                        p.op("dve", lambda e, o_=o_, i_=i_: e.tensor_copy(out=o_, in_=i_),
                             reads=bank(pbase + kb), writes=[dst_key(dst)])
                return
            for k in range(16):
                if (k // 4) % 2 == 0:
                    p.op("act", lambda e, k=k: e.activation(
                        out=dst[:, k, 0:w], in_=pv[:, k, 0:w], func=AF.Identity,
                        scale=cols[:, gcol + k:gcol + k + 1], bias=cols[:, scol + k:scol + k + 1]),
                        reads=bank(pbase, 4) + ["cols_m"], writes=[dst_key(dst)])
                else:
                    p.op("dve", lambda e, k=k: e.tensor_scalar(
                        out=dst[:, k, 0:w], in0=pv[:, k, 0:w], scalar1=cols[:, gcol + k:gcol + k + 1],
                        scalar2=cols[:, scol + k:scol + k + 1], op0=ALU.mult, op1=ALU.add),
                        reads=bank(pbase, 4) + ["cols_m"], writes=[dst_key(dst)])

        dkeys = {}

        def dst_key(ap):
            return dkeys[id(ap)]

        def dbg_dump(name, ap_sb, key):
            if debug and name in dbg:
                final.append(p.dma("pool", dbg[name], ap_sb, reads=[key]))

        brow = [T(G, "brow%d" % i, [1, 512], F32)[0:1, :] for i in range(1)]
        mrow = [T(G, "mrow%d" % i, [1, 512], F32)[0:1, :] for i in range(1)]
        ctmp = T(G, "ctmp", [128, 32], F32)
        caT = T(G, "caT", [128, 16], BF16)
        p.dma("sp", ctmp[:, 0:16], c_col[:, :], writes=["ccol"])
        p.op("act", lambda e: e.activation(out=ctmp[:, 16:32], in_=ctmp[:, 0:16], func=AF.Exp, scale=-1.0),
             reads=["ccol"], writes=["cexp"])
        p.op("dve", lambda e: e.tensor_scalar(out=ctmp[:, 16:32], in0=ctmp[:, 16:32], scalar1=1.0, scalar2=None,
                                               op0=ALU.add), reads=["cexp"], writes=["cexp"])
        p.op("dve", lambda e: e.reciprocal(out=ctmp[:, 16:32], in_=ctmp[:, 16:32]), reads=["cexp"], writes=["cexp"])
        p.op("dve", lambda e: e.tensor_tensor(out=caT[:], in0=ctmp[:, 0:16], in1=ctmp[:, 16:32], op=ALU.mult),
             reads=["cexp", "ccol"], writes=["caT"])
        ada_n = [0]

        def ada_group(b):
            cg = ada_n[0]
            ada_n[0] += 1
            W, wkey = ring_get("ada")
            i = 0
            p.dma("sp", brow[i], b_ada[0:1, cg * 512:(cg + 1) * 512], writes=[("brow", i)])
            for k in range(16):
                mm(ps[0:1, b, :], caT[:, k:k + 1], W[:, k, :], k == 0, k == 15,
                   [wkey, "caT"], bank(b), k == 15)
            ring_done()
            p.op("dve", lambda e: e.tensor_tensor(out=mrow[i], in0=ps[0:1, b, :], in1=brow[i], op=ALU.add),
                 reads=bank(b) + [("brow", i)], writes=[("mrow", i)])
            p.dma("sp", modD[0:1, cg * 512:(cg + 1) * 512], mrow[i], reads=[("mrow", i)], writes=["modD"])

        def colload(dstc, off, key):
            p.dma("sp", cols[:, dstc:dstc + 16],
                  modD[0, off:off + D].rearrange("(k p) -> p k", p=128),
                  reads=["modD"], writes=[key], allow_slow_non_contiguous=True)

        with scope() as S1:
            big16 = T(S1, "big16", [128, 16, 1024], BF16)
            dkeys[id(big16)] = "big16"
            with scope() as S2:
                KT = T(S2, "KT", [128, 2, S], BF16)
                Vaug = T(S2, "Vaug", [128, 16, 2, 129], BF16)
                kiT = T(S2, "kiT", [128, S], BF16)
                wq = T(S2, "wq", [128, 8, 16], F32)
                with scope() as S4:
                    qT = T(S4, "qT", [128, 8, 8, 128], BF16)
                    u = T(S4, "u", [128, 8, 1024], BF16)
                    uh = T(S4, "uh", [128, 1024], BF16)
                    with scope() as S3:
                        hT_own = T(S3, "hT_own", [128, 16, 1024], BF16)
                        hT_halo = T(S3, "hT_halo", [128, 16, 128], BF16)
                        dkeys[id(hT_own)] = "hT_own"
                        dkeys[id(hT_halo)] = "hT_halo"
                        xt = [T(S3, "xt%d" % i, [128, D], F32) for i in range(2)]
                        xnb = [T(S3, "xnb%d" % i, [128, D], BF16) for i in range(2)]
                        grp = 0
                        for which, src, dstT in (("own", x_own, hT_own), ("oth", x_oth, big16)):
                            for j0 in range(0, 8, 2):
                                srcs = [("dram", src[(j0 + i) * 128:(j0 + i + 1) * 128, :],
                                         (0 if which == "own" else 8) + j0 + i) for i in range(2)]
                                dview = dstT[:, :, j0 * 128:(j0 + 2) * 128]
                                dkeys[id(dview)] = dkeys[id(dstT)]
                                ada_group(4 * ((grp + 1) % 2))
                                norm_transpose_pair(srcs, xt, xnb, dview, None, None, "a", 4 * (grp % 2))
                                grp += 1
                        dview = hT_halo[:, :, :]
                        dkeys[id(dview)] = "hT_halo"
                        norm_transpose_pair([("dram", x_halo[:, :], 16)], xt, xnb, dview, None, None, "a", 4 * (grp % 2))
                        colload(48, 0, "c_sh1")
                        colload(32, D, "c_sc1")
                        p.op("dve", lambda e: e.scalar_tensor_tensor(out=cols[:, 32:48], in0=cols[:, 32:48], scalar=1.0,
                                                                      in1=cols[:, 0:16], op0=ALU.add, op1=ALU.mult),
                             reads=["c_sc1", "cols_g", "c_sh1"], writes=["c_sc1", "cols_m1"])
                        for (tile_, tkey) in ((hT_own, "hT_own"), (big16, "big16"), (hT_halo, "hT_halo")):
                            for k in range(16):
                                p.op("dve", lambda e, tile_=tile_, k=k: e.tensor_scalar(
                                    out=tile_[:, k, :], in0=tile_[:, k, :], scalar1=cols[:, 32 + k:33 + k],
                                    scalar2=cols[:, 48 + k:49 + k], op0=ALU.mult, op1=ALU.add),
                                    reads=[tkey, "cols_m1"], writes=[tkey])
                        if debug:
                            dbg_dump("hT_own", hT_own[:], "hT_own")
                            dbg_dump("hT_oth", big16[:], "big16")
                            dbg_dump("hT_halo", hT_halo[:], "hT_halo")
                        if stage == "p1":
                            return finish()
                        WA, keyA = ring_get("inA")
                        ring_done_A = False
                        pb = [0]

                        def nextbank():
                            b = pb[0]
                            pb[0] = (b + 1) % 8
                            return b

                        p.op("dve", lambda e: e.memset(Vaug[:, :, :, 128:129], 1.0), writes=["Vaug"])
                        KTv = KT[:].rearrange("p g (j two t) -> p g j two t", two=2, t=128)
                        kiTv = kiT[:].rearrange("p (j two t) -> p j two t", two=2, t=128)
                        cpy = [0]

                        def evac_copy(out, in_, reads, writes):
                            cpy[0] += 1
                            if cpy[0] % 2:
                                p.op("act", lambda e: e.activation(out=out, in_=in_, func=AF.Copy),
                                     reads=reads, writes=writes)
                            else:
                                p.op("dve", lambda e: e.tensor_copy(out=out, in_=in_), reads=reads, writes=writes)

                        for par, hsrc, hkey in ((0, hT_own, "hT_own"), (1, big16, "big16")):
                            for tg in range(2):
                                for c in range(2):
                                    b = nextbank()
                                    for k in range(16):
                                        mm(ps[:, b, :], WA[:, k, c * 128:(c + 1) * 128], hsrc[:, k, tg * 512:(tg + 1) * 512],
                                           k == 0, k == 15, [keyA, hkey], bank(b), k == 15)
                                    evac_copy(KTv[:, c, tg * 4:(tg + 1) * 4, par, :],
                                              ps[:, b, :].rearrange("p (j t) -> p j t", t=128), bank(b), ["KT"])
                                for tt in range(4):
                                    jt = tg * 4 + tt
                                    b = nextbank()
                                    for k in range(16):
                                        mm(ps[:, b, 0:256], hsrc[:, k, jt * 128:(jt + 1) * 128], WA[:, k, 256:512],
                                           k == 0, k == 15, [keyA, hkey], bank(b), k == 15)
                                    evac_copy(Vaug[:, 2 * jt + par, :, 0:128],
                                              ps[:, b, 0:256].rearrange("p (g d) -> p g d", d=128), bank(b), ["Vaug"])
                        ring_done()
                        WB, keyB = ring_get("inB")
                        for par, hsrc, hkey in ((0, hT_own, "hT_own"), (1, big16, "big16")):
                            for tg in range(2):
                                b = nextbank()
                                for k in range(16):
                                    mm(ps[:, b, :], WB[:, k, 0:128], hsrc[:, k, tg * 512:(tg + 1) * 512],
                                       k == 0, k == 15, [keyB, hkey], bank(b), k == 15)
                                evac_copy(kiTv[:, tg * 4:(tg + 1) * 4, par, :],
                                          ps[:, b, :].rearrange("p (j t) -> p j t", t=128), bank(b), ["kiT"])
                        for j in range(8):
                            b = nextbank()
                            for k in range(16):
                                mm(ps[:, b, 0:16], hT_own[:, k, j * 128:(j + 1) * 128], WB[:, k, 128:144],
                                   k == 0, k == 15, [keyB, "hT_own"], bank(b), k == 15)
                            p.op("dve", lambda e, b=b, j=j: e.tensor_scalar(
                                out=wq[:, j, :], in0=ps[:, b, 0:16], scalar1=IDX_SCALE, scalar2=None, op0=ALU.mult),
                                reads=bank(b), writes=["wq"])
                        ring_done()
                        if debug:
                            dbg_dump("KT", KT[:], "KT")
                            dbg_dump("kiT", kiT[:], "kiT")
                            dbg_dump("Vaug", Vaug[:], "Vaug")
                            dbg_dump("wq", wq[:], "wq")
                        p.barrier()
                        if stage == "p2a":
                            return finish()
                        for i in range(2):
                            W, wkey = ring_get("q")
                            for hh in range(4):
                                h = 4 * i + hh
                                for tg in range(2):
                                    b = nextbank()
                                    for k in range(16):
                                        mm(ps[:, b, :], W[:, k, hh * 128:(hh + 1) * 128], hT_own[:, k, tg * 512:(tg + 1) * 512],
                                           k == 0, k == 15, [wkey, "hT_own"], bank(b), k == 15)
                                    evac_copy(qT[:, tg * 4:(tg + 1) * 4, h, :],
                                              ps[:, b, :].rearrange("p (j t) -> p j t", t=128), bank(b), ["qT"])
                            ring_done()
                        for i in range(4):
                            W, wkey = ring_get("iq")
                            for hh in range(4):
                                h = 4 * i + hh
                                for tg in range(2):
                                    b = nextbank()
                                    for k in range(16):
                                        mm(ps[:, b, :], W[:, k, hh * 128:(hh + 1) * 128], hT_own[:, k, tg * 512:(tg + 1) * 512],
                                           k == 0, k == 15, [wkey, "hT_own"], bank(b), k == 15)
                                    evac_copy(big16[:, h, tg * 512:(tg + 1) * 512], ps[:, b, :], bank(b),
                                              [("b16", tg * 4 + jj) for jj in range(4)])
                            ring_done()
                        for i in range(2):
                            W, wkey = ring_get("pool")
                            for j in range(9):
                                b = nextbank()
                                for k in range(16):
                                    lhsT = hT_own[:, k, j * 128:(j + 1) * 128] if j < 8 else hT_halo[:, k, :]
                                    mm(ps[:, b, :], lhsT, W[:, k, :], k == 0, k == 15,
                                       [wkey, "hT_own", "hT_halo"], bank(b), k == 15)
                                dst = u[:, j, i * 512:(i + 1) * 512] if j < 8 else uh[:, i * 512:(i + 1) * 512]
                                evac_copy(dst, ps[:, b, :], bank(b), ["u"])
                            ring_done()
                        if debug:
                            dbg_dump("qT", qT[:], "qT")
                            dbg_dump("qiT", big16[:], ("b16", 7))
                            dbg_dump("u", u[:], "u")
                            dbg_dump("uh", uh[:], "u")
                        p.barrier()
                        if stage == "p2b":
                            return finish()
                    with scope() as P3:
                        nkB = T(P3, "nkB", [128, S], F32)
                        qB = T(P3, "qB", [128, 8], F32)
                        pw2 = T(P3, "pw2", [128, 32], F32)
                        Mm = T(P3, "Mm", [128, 2, 4, 128], BF16)
                        Hf = T(P3, "Hf", [128, 8, 4, 16], BF16)
                        I4 = T(P3, "I4", [128, 512], BF16)
                        wps = T(P3, "wps", [128, 4, 2, 256], BF16)
                        gb = T(P3, "gb", [128, 2048], F32)
                        p.dma("sp", nkB[:], nkB_d[:, :], writes=["nkB"])
                        p.dma("sp", qB[:], qB_d[:, :], writes=["qB"])
                        p.dma("sp", pw2[:], pw2_d[:, :], writes=["pw2"])
                        p.dma("pool", Mm[:].rearrange("p a g t -> p (a g t)"), Mm_d[:, :], writes=["Mm"])
                        p.dma("pool", Hf[:].rearrange("p a g t -> p (a g t)"), Hf_d[:, :], writes=["Hf"])
                        p.dma("pool", I4[:], I4_d[:, :], writes=["I4"])
                        p.dma("sp", gb[:, 0:1024], g_pool_out[0:1, :].partition_broadcast(128), writes=["gb0"])
                        p.dma("sp", gb[:, 1024:2048], g_attn_out[0:1, :].partition_broadcast(128), writes=["gb1"])
                        with scope() as P3s:
                            wpf = T(P3s, "wpf", [128, 4, 2, 256], F32)
                            psb_t = T(P3s, "psb_t", [128, 1024], F32)
                            p.dma("sp", wpf[:], w_pool.rearrange("g (cc p) d -> p g cc d", p=128), writes=["wpf"])
                            p.dma("sp", psb_t[:], pool_scale[0:1, :].partition_broadcast(128), writes=["psb_t"])
                            for g in range(4):
                                p.op("dve", lambda e, g=g, wpf=wpf, psb_t=psb_t: e.tensor_tensor(
                                    out=wps[:, g, :, :], in0=wpf[:, g, :, :],
                                    in1=psb_t[:, g * 256:(g + 1) * 256].unsqueeze(1).to_broadcast([128, 2, 256]),
                                    op=ALU.mult), reads=["wpf", "psb_t"], writes=["wps"])
                            p.barrier()
                        scoreS = [[T(P3, "scs%d" % s, [128, 1024], F32), T(P3, "scb%d" % s, [128, S], F32)] for s in range(2)]
                        maskS = [[T(P3, "mks%d" % s, [128, 1024], BF16), T(P3, "mkb%d" % s, [128, S], BF16)] for s in range(2)]
                        bsS = [[T(P3, "bs%d_%d" % (s, i), [128, 64], F32) for i in range(2)] for s in range(2)]
                        cntS = [T(P3, "cnt%d" % s, [128, 32], F32) for s in range(2)]
                        Rb = [T(P3, "Rb%d" % i, [128, 1024], F32) for i in range(2)]
                        PT = [T(P3, "PT%d" % i, [128, 512], BF16) for i in range(2)]
                        attn32 = T(P3, "attn32", [128, 1024], F32)
                        mg = T(P3, "mg", [128, 2048], BF16)
                        pooledT = T(P3, "pooledT", [128, 8, 128], BF16)
                        rcT = T(P3, "rcT", [128, 8], F32)
                        cnt_ci = [0]
                        cnt_ui = [0]

                        def stage_A(j, score, bs, tag):
                            L = 2 * j + 2
                            N = 128 * L
                            skey = ("score", tag)
                            chunks = [(c0, min(1024, N - c0)) for c0 in range(0, N, 1024)]
                            for h in range(16):
                                for (c0, cw) in chunks:
                                    ci = cnt_ci[0]
                                    cnt_ci[0] += 1
                                    bb = 2 * (ci % 2)
                                    rb = Rb[ci % 2]
                                    rkey = ("Rb", ci % 2)
                                    nmm = (cw + 511) // 512
                                    for m in range(nmm):
                                        w = min(512, cw - m * 512)
                                        mm(ps[:, bb + m, 0:w], big16[:, h, j * 128:(j + 1) * 128],
                                           kiT[:, c0 + m * 512:c0 + m * 512 + w], True, True,
                                           [("b16", j), "kiT"], bank(bb + m), m == nmm - 1)
                                    pin = psf[:, bb * 512:bb * 512 + cw]
                                    p.op("act", lambda e, rb=rb, pin=pin, cw=cw: e.activation(
                                        out=rb[:, 0:cw], in_=pin, func=AF.Relu),
                                        reads=bank(bb, nmm), writes=[rkey])
                                    if h == 0:
                                        p.op("dve", lambda e, rb=rb, c0=c0, cw=cw: e.tensor_scalar(
                                            out=score[:, c0:c0 + cw], in0=rb[:, 0:cw], scalar1=wq[:, j, 0:1],
                                            scalar2=None, op0=ALU.mult), reads=[rkey, "wq"], writes=[skey])
                                    else:
                                        p.op("dve", lambda e, rb=rb, c0=c0, cw=cw, h=h: e.scalar_tensor_tensor(
                                            out=score[:, c0:c0 + cw], in0=rb[:, 0:cw], scalar=wq[:, j, h:h + 1],
                                            in1=score[:, c0:c0 + cw], op0=ALU.mult, op1=ALU.add),
                                            reads=[rkey, "wq", skey], writes=[skey])
                                    yield
                            if debug and j == 1:
                                dbg_dump("score1", score[:, 0:512], skey)
                            p.op("dve", lambda e: e.tensor_reduce(out=bs[:, 0:1], in_=score[:, 0:N], axis=AX.X, op=ALU.max),
                                 reads=[skey], writes=[("bsM", tag)])
                            yield
                            p.op("dve", lambda e: e.tensor_reduce(out=bs[:, 1:2], in_=score[:, 0:N], axis=AX.X, op=ALU.min),
                                 reads=[skey], writes=[("bsm", tag)])
                            yield
                            p.op("dve", lambda e: e.scalar_tensor_tensor(
                                out=score[:, 0:N], in0=nkB[:, 0:N], scalar=qB[:, j:j + 1], in1=score[:, 0:N],
                                op0=ALU.add, op1=ALU.min), reads=["nkB", "qB", skey], writes=[skey])
                            yield
                            p.op("dve", lambda e: e.tensor_scalar(out=bs[:, 2:3], in0=bs[:, 1:2], scalar1=-1.0, scalar2=None,
                                                                   op0=ALU.add), reads=[("bsm", tag)], writes=[("bslo", tag)])
                            yield
                            p.op("dve", lambda e: e.tensor_tensor(out=bs[:, 3:4], in0=bs[:, 0:1], in1=bs[:, 2:3], op=ALU.subtract),
                                 reads=[("bsM", tag), ("bslo", tag)], writes=[("bsW", tag)])
                            yield
                            p.op("dve", lambda e: e.tensor_scalar(out=bs[:, 32:64], in0=pw2[:, :], scalar1=bs[:, 3:4], scalar2=None,
                                                                   op0=ALU.mult), reads=[("bsW", tag), "pw2"], writes=[("bswd", tag)])
                            yield
                            p.op("dve", lambda e: e.tensor_tensor(out=bs[:, 4:5], in0=bs[:, 2:3], in1=bs[:, 32:33], op=ALU.add),
                                 reads=[("bslo", tag), ("bswd", tag)], writes=[("bsmid", tag)])
                            yield
                            if tag[1] == 1:
                                p.op("dve", lambda e: e.tensor_scalar(out=bs[:, 7:8], in0=bs[:, 4:5], scalar1=-1.0, scalar2=None,
                                                                       op0=ALU.mult), reads=[("bsmid", tag)], writes=[("bsnmid", tag)])
                                yield
                                p.op("dve", lambda e: e.tensor_scalar(out=bs[:, 32:64], in0=bs[:, 32:64], scalar1=-1.0, scalar2=None,
                                                                       op0=ALU.mult), reads=[("bswd", tag), ("bsmid", tag)],
                                     writes=[("bswd", tag)])
                                yield
                                p.op("dve", lambda e: e.memset(cntS[tag[0]][:], 0.0),
                                     reads=[("bscnt", tag)], writes=[("bscnt", tag)])
                                yield

                        def stage_B_act(j, score, maskb, bs, tag):
                            N = 128 * (2 * j + 2)
                            skey = ("score", tag)
                            mkey = ("maskb", tag)
                            for it in range(NIT):
                                p.op("act", lambda e, it=it: e.activation(
                                    out=maskb[:, 0:N], in_=score[:, 0:N], func=AF.Sign, bias=bs[:, 7:8], scale=1.0,
                                    accum_out=cntS[tag[0]][:, it:it + 1]),
                                    reads=[skey, ("bsnmid", tag), mkey], writes=[mkey, ("bscnt", tag)])
                                last = it == NIT - 1
                                p.op("dve", lambda e, last=last, it=it: e.tensor_scalar(
                                    out=bs[:, 6:7], in0=cntS[tag[0]][:, it:it + 1], scalar1=2.0 * TOPK - N, scalar2=(-1.0 if last else -0.5),
                                    op0=ALU.is_ge, op1=ALU.add), reads=[("bscnt", tag)], writes=[("bsge", tag)])
                                yield
                                p.op("dve", lambda e, it=it: e.scalar_tensor_tensor(
                                    out=bs[:, 7:8], in0=bs[:, 6:7], scalar=bs[:, 32 + it:33 + it], in1=bs[:, 7:8],
                                    op0=ALU.mult, op1=ALU.add),
                                    reads=[("bsge", tag), ("bswd", tag), ("bsnmid", tag)], writes=[("bsnmid", tag)])
                                yield
                            p.op("dve", lambda e: e.tensor_scalar(out=bs[:, 4:5], in0=bs[:, 7:8], scalar1=-1.0, scalar2=None,
                                                                   op0=ALU.mult), reads=[("bsnmid", tag)], writes=[("bsmid", tag)])
                            yield
                            p.op("dve", lambda e: e.tensor_scalar(
                                out=maskb[:, 0:N], in0=score[:, 0:N], scalar1=bs[:, 4:5], scalar2=MASKV,
                                op0=ALU.is_lt, op1=ALU.mult), reads=[skey, ("bsmid", tag), mkey], writes=[mkey])
                            yield

                        def stage_B(j, score, maskb, bs, tag):
                            N = 128 * (2 * j + 2)
                            skey = ("score", tag)
                            mkey = ("maskb", tag)
                            for it in range(NIT):
                                p.op("dve", lambda e: e.tensor_scalar(
                                    out=maskb[:, 0:N], in0=score[:, 0:N], scalar1=bs[:, 4:5], scalar2=None,
                                    op0=ALU.is_ge, op1=ALU.add, accum_out=bs[:, 5:6]),
                                    reads=[skey, ("bsmid", tag), mkey], writes=[mkey, ("bscnt", tag)])
                                yield
                                last = it == NIT - 1
                                p.op("dve", lambda e, last=last: e.tensor_scalar(
                                    out=bs[:, 6:7], in0=bs[:, 5:6], scalar1=TOPK, scalar2=(-1.0 if last else -0.5),
                                    op0=ALU.is_ge, op1=ALU.add), reads=[("bscnt", tag)], writes=[("bsge", tag)])
                                yield
                                p.op("dve", lambda e, it=it: e.scalar_tensor_tensor(
                                    out=bs[:, 4:5], in0=bs[:, 6:7], scalar=bs[:, 32 + it:33 + it], in1=bs[:, 4:5],
                                    op0=ALU.mult, op1=ALU.add),
                                    reads=[("bsge", tag), ("bswd", tag), ("bsmid", tag)], writes=[("bsmid", tag)])
                                yield
                            if debug and j == 1:
                                dbg_dump("tau1", bs[:, 0:8], ("bsmid", tag))
                            p.op("dve", lambda e: e.tensor_scalar(
                                out=maskb[:, 0:N], in0=score[:, 0:N], scalar1=bs[:, 4:5], scalar2=MASKV,
                                op0=ALU.is_lt, op1=ALU.mult), reads=[skey, ("bsmid", tag), mkey], writes=[mkey])
                            yield

                        def oacc(h, n=129):
                            return ps[:, 5 + h // 3, (h % 3) * 129:(h % 3) * 129 + n]

                        def stage_C(j, maskb, tag):
                            L = 2 * j + 2
                            mkey = ("maskb", tag)
                            for kt in range(L):
                                for g in range(2):
                                    mm(ps[:, 4, :], KT[:, g, kt * 128:(kt + 1) * 128],
                                       qT[:, j, 4 * g:4 * g + 4, :], True, False,
                                       ["KT", "qT"], bank(4), False)
                                    mm(ps[:, 4, :], maskb[:, kt * 128:(kt + 1) * 128], I4[:, :], False, True,
                                       [mkey, "I4"], bank(4), True)
                                    ui = cnt_ui[0]
                                    cnt_ui[0] += 1
                                    pt = PT[ui % 2]
                                    pkey = ("PT", ui % 2)
                                    p.op("act", lambda e, pt=pt: e.activation(out=pt[:], in_=ps[:, 4, :], func=AF.Exp,
                                                                              scale=ATT_SCALE),
                                         reads=bank(4), writes=[pkey])
                                    for hh in range(4):
                                        h = 4 * g + hh
                                        lastmm = (kt == L - 1) and (g == 1) and (hh == 3)
                                        mm(oacc(h), pt[:, hh * 128:(hh + 1) * 128], Vaug[:, kt, g, :],
                                           kt == 0 and h % 3 == 0, kt == L - 1, [pkey, "Vaug"], bank(5, 3),
                                           lastmm or hh == 3)
                                    yield
                            for bk in range(3):
                                nh = 3 if bk < 2 else 2
                                den = ps[:, 5 + bk, 0:nh * 129].rearrange("p (h c) -> p h c", c=129)
                                p.op("dve", lambda e, den=den, bk=bk, nh=nh: e.reciprocal(
                                    out=rcT[:, 3 * bk:3 * bk + nh], in_=den[:, :, 128]),
                                    reads=bank(5, 3), writes=[("rc", bk)])
                                yield
                                p.op("dve", lambda e, den=den, bk=bk, nh=nh: e.tensor_tensor(
                                    out=attn32[:, 384 * bk:384 * bk + 128 * nh].rearrange("p (h d) -> p h d", d=128),
                                    in0=den[:, :, 0:128],
                                    in1=rcT[:, 3 * bk:3 * bk + nh].unsqueeze(2).to_broadcast([128, nh, 128]),
                                    op=ALU.mult), reads=bank(5, 3) + [("rc", bk)], writes=["attn32"])
                                yield
                            p.op("act", lambda e: e.activation(out=mg[:, 1024:2048], in_=attn32[:], func=AF.Square,
                                                               accum_out=st[:, 128 + j:129 + j]),
                                 reads=["attn32"], writes=["mg1", ("st", 128 + j)])
                            p.op("dve", lambda e: e.tensor_tensor(out=mg[:, 1024:2048], in0=attn32[:], in1=gb[:, 1024:2048],
                                                                  op=ALU.mult), reads=["attn32", "gb1"], writes=["mg1"])
                            yield
                            if debug and j == 1:
                                dbg_dump("attn1", attn32[:], "attn32")
                            var = 0 if j == 0 else 1
                            psP = psf[:, 2048:3072].rearrange("p (c t) -> p c t", t=128)
                            for cch in range(8):
                                g = cch // 2
                                mm(psP[:, cch, :], u[:, j, cch * 128:(cch + 1) * 128], Mm[:, var, g, :], True, False,
                                   ["u", "Mm"], bank(4, 2), False)
                                mm(psP[:, cch, 0:16], uh[:, cch * 128:(cch + 1) * 128], Hf[:, j, g, :], False, True,
                                   ["u", "Hf"], bank(4, 2), cch == 7)
                            p.op("act", lambda e: e.activation(out=pooledT[:].rearrange("p c t -> p (c t)"), in_=psf[:, 2048:3072],
                                                               func=AF.Copy), reads=bank(4, 2), writes=["pooledT"])
                            for g in range(4):
                                for cc in range(2):
                                    mm(psf[:, 3072 + g * 256:3072 + (g + 1) * 256], pooledT[:, 2 * g + cc, :], wps[:, g, cc, :],
                                       cc == 0, cc == 1, ["pooledT", "wps"], bank(6, 2), (g == 3 and cc == 1))
                            p.op("act", lambda e: e.activation(out=mg[:, 0:1024], in_=psf[:, 3072:4096], func=AF.Square,
                                                               accum_out=st[:, 136 + j:137 + j]),
                                 reads=bank(6, 2), writes=["mg0", ("st", 136 + j)])
                            p.op("dve", lambda e: e.tensor_tensor(out=mg[:, 0:1024], in0=psf[:, 3072:4096], in1=gb[:, 0:1024],
                                                                  op=ALU.mult), reads=bank(6, 2) + ["gb0"], writes=["mg0"])
                            yield
                            pv = psb[:, 4096:6144].rearrange("p (c t) -> p c t", t=128)
                            for c in range(16):
                                p.op("pe", lambda e, c=c: e.transpose(out=pv[:, c, :], in_=mg[:, c * 128:(c + 1) * 128],
                                                                      identity=identb[:]),
                                     reads=["mg0", "mg1", "identb"], writes=bank(4, 2), signal=(c == 15))
                            p.op("act", lambda e: e.activation(out=big16[:, :, j * 128:(j + 1) * 128], in_=pv,
                                                               func=AF.Copy),
                                 reads=bank(4, 2), writes=[("b16", j)])
                            yield

                        def run_rr(gens):
                            gens = [g for g in gens if g is not None]
                            while gens:
                                for g in list(gens):
                                    try:
                                        next(g)
                                    except StopIteration:
                                        gens.remove(g)

                        def chain(*gs):
                            for g in gs:
                                yield from g

                        pairs = [(0, 7), (1, 6), (2, 5), (3, 4)]

                        def bufs(q, i):
                            s = q % 2
                            return scoreS[s][i], maskS[s][i], bsS[s][i], (s, i)

                        def A_pair(q):
                            return chain(*[stage_A(pairs[q][i], bufs(q, i)[0], bufs(q, i)[2], bufs(q, i)[3]) for i in range(2)])

                        def C_pair(q):
                            return chain(*[stage_C(pairs[q][i], bufs(q, i)[1], bufs(q, i)[3]) for i in range(2)])

                        def B_one(q, i):
                            sc, mk, bs_, tag = bufs(q, i)
                            if i == 1:
                                return stage_B_act(pairs[q][i], sc, mk, bs_, tag)
                            return stage_B(pairs[q][i], sc, mk, bs_, tag)

                        def ada_rest(n):
                            for _ in range(n):
                                ci = cnt_ci[0]
                                cnt_ci[0] += 1
                                ada_group(2 * (ci % 2))
                                for _ in range(3):
                                    yield

                        run_rr([A_pair(0), ada_rest(2)])
                        for q in range(4):
                            run_rr([B_one(q, 0), B_one(q, 1),
                                    A_pair(q + 1) if q + 1 < 4 else None,
                                    C_pair(q - 1) if q >= 1 else None,
                                    ada_rest(4 if q < 3 else 2)])
                        run_rr([C_pair(3)])
                        assert ada_n[0] == 24
                        if debug:
                            dbg_dump("mergedT", big16[:], ("b16", 7))
                            dbg_dump("st", st[:], ("st", 143))
                        p.barrier()
                        if stage == "p3":
                            return finish()
            with scope() as P4:
                mix = T(P4, "mix", [128, 8, D], F32)
                gg = T(P4, "gg", [128, D], F32)
                gt = T(P4, "gt", [128, D], F32)
                xt = [T(P4, "xt4_%d" % i, [128, D], F32) for i in range(2)]
                xnb = [T(P4, "xnb4_%d" % i, [128, D], BF16) for i in range(2)]
                colload(80, 3 * D, "c_sh2")
                colload(64, 4 * D, "c_sc2")
                p.op("dve", lambda e: e.scalar_tensor_tensor(out=cols[:, 64:80], in0=cols[:, 64:80], scalar=1.0,
                                                              in1=cols[:, 16:32], op0=ALU.add, op1=ALU.mult),
                     reads=["c_sc2", "cols_g", "c_sh2"], writes=["c_sc2", "cols_m"])
                p.dma("sp", gg[:], modD[0:1, 2 * D:3 * D].partition_broadcast(128), reads=["modD"], writes=["gg"])
                p.dma("sp", gt[:], g_post_mix[0:1, :].partition_broadcast(128), writes=["gt"])
                p.op("dve", lambda e: e.tensor_tensor(out=gg[:], in0=gg[:], in1=gt[:], op=ALU.mult),
                     reads=["gg", "gt"], writes=["gg"])
                rstd_batch(st[:, 128:144], st[:, 144:160], 1024, [("st", 128 + i) for i in range(16)], "rs_pa", st[:, 160:176])
                for cg in range(4):
                    W, wkey = ring_get("wout")
                    for j in range(8):
                        bA = (2 * j) % 8
                        bB = (2 * j + 1) % 8
                        for k in range(8):
                            mm(ps[:, bA, :], big16[:, k, j * 128:(j + 1) * 128], W[:, k, :], k == 0, k == 7,
                               [wkey, ("b16", j)], bank(bA), k == 7)
                        for k in range(8, 16):
                            mm(ps[:, bB, :], big16[:, k, j * 128:(j + 1) * 128], W[:, k, :], k == 8, k == 15,
                               [wkey, ("b16", j)], bank(bB), k == 15)
                        p.op("act", lambda e, j=j, cg=cg, bA=bA: e.activation(
                            out=mix[:, j, cg * 512:(cg + 1) * 512], in_=ps[:, bA, :], func=AF.Identity,
                            scale=st[:, 152 + j:153 + j]), reads=bank(bA) + ["rs_pa"], writes=[("mix", j)])
                        p.op("dve", lambda e, j=j, cg=cg, bB=bB: e.scalar_tensor_tensor(
                            out=mix[:, j, cg * 512:(cg + 1) * 512], in0=ps[:, bB, :], scalar=st[:, 144 + j:145 + j],
                            in1=mix[:, j, cg * 512:(cg + 1) * 512], op0=ALU.mult, op1=ALU.add),
                            reads=bank(bB) + ["rs_pa", ("mix", j)], writes=[("mix", j)])
                    ring_done()
                p.barrier()
                for j in range(8):
                    p.op("act", lambda e, j=j: e.activation(out=xnb[j % 2][:], in_=mix[:, j, :], func=AF.Square,
                                                            accum_out=st[:, 176 + j:177 + j]),
                         reads=[("mix", j)], writes=[("xnb", j % 2), ("st", 176 + j)])
                rstd_batch(st[:, 176:184], st[:, 184:192], D, [("st", 176 + i) for i in range(8)], "rs_m", st[:, 192:200])
                for j in range(8):
                    p.dma("sp", xt[j % 2][:], x_own[j * 128:(j + 1) * 128, :], writes=[("xt", j % 2)])
                    p.op("dve", lambda e, j=j: e.scalar_tensor_tensor(
                        out=mix[:, j, :], in0=mix[:, j, :], scalar=st[:, 184 + j:185 + j], in1=gg[:],
                        op0=ALU.mult, op1=ALU.mult), reads=[("mix", j), "rs_m", "gg"], writes=[("mix", j)])
                    p.op("dve", lambda e, j=j: e.tensor_tensor(out=mix[:, j, :], in0=mix[:, j, :], in1=xt[j % 2][:], op=ALU.add),
                         reads=[("mix", j), ("xt", j % 2)], writes=[("mix", j)])
                    p.dma("sp", x1D[j * 128:(j + 1) * 128, :], mix[:, j, :], reads=[("mix", j)], writes=["x1D"])
                    p.op("act", lambda e, j=j: e.activation(out=xnb[j % 2][:], in_=mix[:, j, :], func=AF.Square,
                                                            accum_out=st[:, 200 + j:201 + j]),
                         reads=[("mix", j)], writes=[("xnb", j % 2), ("st", 200 + j)])
                rstd_batch(st[:, 200:208], st[:, 208:216], D, [("st", 200 + i) for i in range(8)], "rs_2", st[:, 216:224])
                if debug:
                    dbg_dump("x1", mix[:], ("mix", 7))
                for j0 in range(0, 8, 2):
                    srcs = [("sbuf", (mix[:, j0 + i, :], ("mix", j0 + i)), 0) for i in range(2)]
                    dview = big16[:, :, j0 * 128:(j0 + 2) * 128]
                    dkeys[id(dview)] = "h2T"
                    norm_transpose_pair(srcs, None, xnb, dview, 64, 80, "b", 4 * ((j0 // 2) % 2),
                                        rstd_known=[(st[:, 208 + j0 + i:209 + j0 + i], "rs_2") for i in range(2)])
                if debug:
                    dbg_dump("h2T", big16[:], "h2T")
                p.barrier()
                if stage == "p4":
                    return finish()
            with scope() as P5:
                fT = T(P5, "fT", [128, 64, 1024], BF16)
                r32 = [T(P5, "r32_%d" % i, [128, 512], F32) for i in range(2)]
                ei = 0
                for fg in range(16):
                    W, wkey = ring_get("ff1")
                    for fc in range(4):
                        jc = fg * 4 + fc
                        for tg in range(2):
                            b = ei % 4
                            for k in range(16):
                                mm(ps[:, b, :], W[:, k, fc * 128:(fc + 1) * 128], big16[:, k, tg * 512:(tg + 1) * 512],
                                   k == 0, k == 15, [wkey, "h2T"], bank(b), k == 15)
                            r = r32[ei % 2]
                            rkey = ("r32", ei % 2)
                            ei += 1
                            p.op("act", lambda e, r=r, b=b: e.activation(out=r[:], in_=ps[:, b, :], func=AF.Relu),
                                 reads=bank(b), writes=[rkey])
                            p.op("dve", lambda e, r=r, jc=jc, tg=tg: e.tensor_tensor(
                                out=fT[:, jc, tg * 512:(tg + 1) * 512], in0=r[:], in1=r[:], op=ALU.mult),
                                reads=[rkey], writes=["fT"])
                    ring_done()
                if debug:
                    dbg_dump("fT", fT[:, 0:4, :], "fT")
                p.barrier()
                if stage == "p5":
                    return finish()
                fo = big16[:].rearrange("p a b -> p (a b)").rearrange("p (j d) -> p j d", d=D)
                jk = T(P5, "jk", [128, 512], BF16)
                for cg in range(4):
                    for jg in range(4):
                        W, wkey = ring_get("ff2")
                        for jj in range(16):
                            jc = jg * 16 + jj
                            for tt in range(8):
                                mm(ps[:, tt, :], fT[:, jc, tt * 128:(tt + 1) * 128], W[:, jj, :], jc == 0, jc == 63,
                                   [wkey, "fT"], bank(tt), jc == 63 or (jj == 15 and tt == 7))
                        ring_done()
                    for tt in range(8):
                        if tt % 2 == 0:
                            p.op("act", lambda e, tt=tt, cg=cg: e.activation(
                                out=fo[:, tt, cg * 512:(cg + 1) * 512], in_=ps[:, tt, :], func=AF.Copy),
                                reads=bank(tt), writes=[("fo", tt)])
                        else:
                            p.op("dve", lambda e, tt=tt, cg=cg: e.tensor_copy(
                                out=fo[:, tt, cg * 512:(cg + 1) * 512], in_=ps[:, tt, :]),
                                reads=bank(tt), writes=[("fo", tt)])
                if debug:
                    dbg_dump("fo", big16[:], ("fo", 7))
                p.barrier()
                if stage == "p6":
                    return finish()
            with scope() as P7:
                gg7 = T(P7, "gg7", [128, D], F32)
                gt7 = T(P7, "gt7", [128, D], F32)
                xt7 = [T(P7, "xt7_%d" % i, [128, D], F32) for i in range(2)]
                ot7 = [T(P7, "ot7_%d" % i, [128, D], F32) for i in range(2)]
                fo = big16[:].rearrange("p a b -> p (a b)").rearrange("p (j d) -> p j d", d=D)
                p.dma("sp", gg7[:], modD[0:1, 5 * D:6 * D].partition_broadcast(128), reads=["modD"], writes=["gg7"])
                p.dma("sp", gt7[:], g_post_ffn[0:1, :].partition_broadcast(128), writes=["gt7"])
                p.op("dve", lambda e: e.tensor_tensor(out=gg7[:], in0=gg7[:], in1=gt7[:], op=ALU.mult),
                     reads=["gg7", "gt7"], writes=["gg7"])
                for tt in range(8):
                    p.op("act", lambda e, tt=tt: e.activation(out=ot7[tt % 2][:], in_=fo[:, tt, :], func=AF.Square,
                                                              accum_out=st[:, 224 + tt:225 + tt]),
                         reads=[("fo", tt)], writes=[("ot", tt % 2), ("st", 224 + tt)])
                rstd_batch(st[:, 224:232], st[:, 72:80], D, [("st", 224 + i) for i in range(8)], "rs_f", st[:, 80:88])
                for tt in range(8):
                    p.dma("sp", xt7[tt % 2][:], x1D[tt * 128:(tt + 1) * 128, :], reads=["x1D"], writes=[("xt7", tt % 2)])
                    o = ot7[tt % 2]
                    p.op("dve", lambda e, tt=tt, o=o: e.scalar_tensor_tensor(
                        out=o[:], in0=fo[:, tt, :], scalar=st[:, 72 + tt:73 + tt], in1=gg7[:],
                        op0=ALU.mult, op1=ALU.mult), reads=[("fo", tt), "rs_f", "gg7"], writes=[("ot", tt % 2)])
                    p.op("dve", lambda e, tt=tt, o=o: e.tensor_tensor(out=o[:], in0=o[:], in1=xt7[tt % 2][:], op=ALU.add),
                         reads=[("ot", tt % 2), ("xt7", tt % 2)], writes=[("ot", tt % 2)])
                    final.append(p.dma("sp", y[tt * 128:(tt + 1) * 128, :], o[:], reads=[("ot", tt % 2)]))
                return finish()


def own_blocks(ty):
    return [2 * j + ty if j < 4 else 2 * j + 1 - ty for j in range(8)]


def _pool_mats(first_is_block0):
    wins = (2, 4, 8, 16)
    Mm = np.zeros((128, 2, 4, 128), np.float32)
    for var in range(2):
        blk0 = (var == 0 and first_is_block0)
        for g, w in enumerate(wins):
            for t in range(128):
                cnt = min(t + 1, w) if blk0 else w
                lo = max(t + 1 - w, 0)
                Mm[lo:t + 1, var, g, t] = 1.0 / cnt
                Mm[t, var, g, t] -= 1.0
    Hf = np.zeros((128, 8, 4, 16), np.float32)
    for j in range(8):
        if j == 0 and first_is_block0:
            continue
        for g, w in enumerate(wins):
            for t in range(16):
                for r in range(16):
                    off = r - 16
                    if t - w + 1 <= off:
                        Hf[16 * j + r, j, g, t] = 1.0 / w
    return Mm, Hf


def host_inputs(inputs):
    x = np.asarray(inputs["x"], np.float32)
    c = np.asarray(inputs["c"], np.float32)
    f = lambda k: np.ascontiguousarray(np.asarray(inputs[k], np.float32)[0])
    shared = {
        "w_ada": f("w_ada"), "b_ada": f("b_ada")[None, :],
        "g_post_mix": f("g_post_mix")[None, :], "g_post_ffn": f("g_post_ffn")[None, :],
        "w_in": f("w_in"), "w_pool": f("w_pool"), "pool_scale": f("pool_scale")[None, :],
        "g_pool_out": f("g_pool_out")[None, :], "g_attn_out": f("g_attn_out")[None, :],
        "w_out": f("w_out"), "w_ff1": f("w_ff1"), "w_ff2": f("w_ff2"),
        "I4": np.ascontiguousarray(np.tile(np.eye(128, dtype=np.float32), (1, 4))),
        "pw2": np.ascontiguousarray(np.tile((0.5 ** np.arange(1, 33, dtype=np.float64)).astype(np.float32)[None, :], (128, 1))),
    }
    gpre1 = f("g_pre_mix").reshape(16, 128).T
    gpre2 = f("g_pre_ffn").reshape(16, 128).T
    shared["vcols"] = np.ascontiguousarray(np.concatenate([gpre1, gpre2], axis=1))
    in_maps = []
    for core in range(NCORES):
        b, ty = core // 2, core % 2
        own = own_blocks(ty)
        oth = own_blocks(1 - ty)
        xb = x[b].reshape(16, 128, D)
        m = dict(shared)
        m["x_own"] = np.ascontiguousarray(xb[own].reshape(1024, D))
        m["x_oth"] = np.ascontiguousarray(xb[oth].reshape(1024, D))
        halo = np.zeros((128, D), np.float32)
        for j, blk in enumerate(own):
            if blk > 0:
                halo[16 * j:16 * j + 16] = x[b, blk * 128 - 16:blk * 128]
        m["x_halo"] = halo
        m["c_col"] = np.ascontiguousarray(c[b].reshape(16, 128).T)
        kpos = np.zeros(S, np.float64)
        for j in range(8):
            kpos[(2 * j) * 128:(2 * j + 1) * 128] = own[j] * 128 + np.arange(128)
            kpos[(2 * j + 1) * 128:(2 * j + 2) * 128] = oth[j] * 128 + np.arange(128)
        m["nkB"] = np.ascontiguousarray(np.tile((-kpos * BIG).astype(np.float32)[None, :], (128, 1)))
        qpos = np.array(own, np.float64)[None, :] * 128 + np.arange(128)[:, None]
        m["qB"] = np.ascontiguousarray(((qpos + 0.5) * BIG).astype(np.float32))
        Mm, Hf = _pool_mats(own[0] == 0)
        m["Mm"] = np.ascontiguousarray(Mm.reshape(128, -1))
        m["Hf"] = np.ascontiguousarray(Hf.reshape(128, -1))
        in_maps.append(m)
    return in_maps


_NC_CACHE = {}


def kernel(**inputs):
    in_maps = host_inputs(inputs)
    if "nc" not in _NC_CACHE:
        _NC_CACHE["nc"] = build_program()
    nc = _NC_CACHE["nc"]
    res = run_bass_kernel_spmd(nc, in_maps, core_ids=list(range(NCORES)))
    out = np.zeros((4, 16, 128, D), np.float32)
    for core in range(NCORES):
        b, ty = core // 2, core % 2
        yc = np.asarray(res.results[core]["y"], np.float32).reshape(8, 128, D)
        for j, blk in enumerate(own_blocks(ty)):
            out[b, blk] = yc[j]
    return out.reshape(4, S, D)
```

```python
import bisect
from contextlib import ExitStack

import numpy as np
import concourse.bass as bass
import concourse.mybir as mybir
from concourse.bass_utils import run_bass_kernel_spmd

F32 = mybir.dt.float32
BF16 = mybir.dt.bfloat16
ALU = mybir.AluOpType
AF = mybir.ActivationFunctionType
AX = mybir.AxisListType

D = 2048
S = 2048
DFF = 8192
IN_W = 4752
NCORES = 8
NIT = 24
TOPK = 256.0
EPS = 1e-6
MASKV = -30000.0
BIG = 1e30
IDX_SCALE = (16 ** -0.5) * (128 ** -0.5)
ATT_SCALE = 128 ** -0.5
NDSEM = 24
NRING = 2


class Prog:
    ENG = ("pe", "act", "dve", "pool", "sp")

    def __init__(self, nc):
        self.nc = nc
        self.ops = {e: [] for e in self.ENG}
        self.sig_seq = {e: [] for e in self.ENG}
        self.nseq = {e: 0 for e in self.ENG}
        self.wr = {}
        self.rd = {}
        self.dslot_next = {"sp": 0, "pool": 0}
        self.dslot_val = [0] * NDSEM
        self.floor = []

    def _deps(self, eng, reads, writes):
        deps = list(self.floor)
        for k in reads:
            deps.extend(self.wr.get(k, {}).values())
        for k in writes:
            deps.extend(self.wr.get(k, {}).values())
            deps.extend(self.rd.get(k, {}).values())
        return [t for t in deps if not (t[0] == "c" and t[1] == eng and eng == "pe")]

    def _record(self, tok, who, reads, writes):
        for k in reads:
            self.rd.setdefault(k, {})[who] = tok
        for k in writes:
            self.wr.setdefault(k, {})[who] = tok

    def op(self, eng, fn, reads=(), writes=(), signal=True):
        deps = self._deps(eng, reads, writes)
        seq = self.nseq[eng]
        self.nseq[eng] += 1
        if signal:
            self.sig_seq[eng].append(seq)
        tok = ("c", eng, seq)
        self.ops[eng].append(dict(kind="c", fn=fn, deps=deps, seq=seq, signal=signal))
        self._record(tok, eng, reads, writes)
        return tok

    def dma(self, eng, out, in_, reads=(), writes=(), **kw):
        deps = self._deps(eng, reads, writes)
        half = NDSEM // 2
        base = 0 if eng == "sp" else half
        slot = base + self.dslot_next[eng]
        self.dslot_next[eng] = (self.dslot_next[eng] + 1) % half
        prev = self.dslot_val[slot]
        self.dslot_val[slot] = prev + 16
        tok = ("d", slot, prev + 16)
        if prev:
            deps.append(("d", slot, prev))
        self.ops[eng].append(dict(kind="d", out=out, in_=in_, deps=deps, slot=slot, kw=kw,
                                  seq=self.nseq[eng]))
        self._record(tok, "dma%d" % slot, reads, writes)
        return tok

    def barrier(self):
        fl = []
        for e in ("pe", "act", "dve", "pool"):
            if self.nseq[e]:
                assert self.sig_seq[e] and self.sig_seq[e][-1] == self.nseq[e] - 1, e
                fl.append(("c", e, self.nseq[e] - 1))
        for s in range(NDSEM):
            if self.dslot_val[s]:
                fl.append(("d", s, self.dslot_val[s]))
        self.floor = fl

    def emit(self, final_tokens=()):
        nc = self.nc
        with ExitStack() as es:
            csem = {e: es.enter_context(nc.semaphore("s_" + e)) for e in self.ENG if e != "sp"}
            dsem = [es.enter_context(nc.semaphore("d%d" % i)) for i in range(NDSEM)]
            block = es.enter_context(nc.Block())
            sig_seq = self.sig_seq

            def resolve(tok):
                if tok[0] == "d":
                    return dsem[tok[1]], tok[2]
                _, e, seq = tok
                i = bisect.bisect_left(sig_seq[e], seq)
                assert i < len(sig_seq[e]), ("no signalling op after", tok)
                return csem[e], i + 1

            def run(ename):
                def body(engine):
                    waited = {}
                    for o in self.ops[ename]:
                        for t in o["deps"]:
                            if t[0] == "c" and t[1] == ename:
                                i = bisect.bisect_left(sig_seq[ename], t[2])
                                assert i < len(sig_seq[ename]) and sig_seq[ename][i] < o["seq"], \
                                    ("same-engine dep on unsignalled op", ename, t)
                            sem, val = resolve(t)
                            if waited.get(id(sem), 0) >= val:
                                continue
                            waited[id(sem)] = val
                            engine.wait_ge(sem, val)
                        if o["kind"] == "c":
                            ins = o["fn"](engine)
                            if o["signal"]:
                                ins.then_inc(csem[ename], 1)
                        else:
                            engine.dma_start(out=o["out"], in_=o["in_"], **o["kw"]).then_inc(
                                dsem[o["slot"]], 16)
                    if ename == "sp":
                        for t in final_tokens:
                            sem, val = resolve(t)
                            engine.wait_ge(sem, val)
                return body

            block.tensor(run("pe"))
            block.scalar(run("act"))
            block.vector(run("dve"))
            block.gpsimd(run("pool"))
            block.sync(run("sp"))


def build_program(debug=False, stage=None):
    nc = bass.Bass("TRN2", target_bir_lowering=False)
    dt_in = lambda name, shape: nc.dram_tensor(name, shape, F32, kind="ExternalInput").ap()
    x_own = dt_in("x_own", [1024, D])
    x_oth = dt_in("x_oth", [1024, D])
    x_halo = dt_in("x_halo", [128, D])
    c_col = dt_in("c_col", [128, 16])
    vcols = dt_in("vcols", [128, 32])
    nkB_d = dt_in("nkB", [128, S])
    qB_d = dt_in("qB", [128, 8])
    pw2_d = dt_in("pw2", [128, 32])
    Mm_d = dt_in("Mm", [128, 2 * 4 * 128])
    Hf_d = dt_in("Hf", [128, 8 * 4 * 16])
    I4_d = dt_in("I4", [128, 512])
    w_ada = dt_in("w_ada", [D, 6 * D])
    b_ada = dt_in("b_ada", [1, 6 * D])
    g_post_mix = dt_in("g_post_mix", [1, D])
    g_post_ffn = dt_in("g_post_ffn", [1, D])
    w_in = dt_in("w_in", [D, IN_W])
    w_pool = dt_in("w_pool", [4, 256, 256])
    pool_scale = dt_in("pool_scale", [1, 1024])
    g_pool_out = dt_in("g_pool_out", [1, 1024])
    g_attn_out = dt_in("g_attn_out", [1, 1024])
    w_out = dt_in("w_out", [D, D])
    w_ff1 = dt_in("w_ff1", [D, DFF])
    w_ff2 = dt_in("w_ff2", [DFF, D])
    y = nc.dram_tensor("y", [1024, D], F32, kind="ExternalOutput").ap()
    modD = nc.dram_tensor("modD", [1, 6 * D], F32).ap()
    x1D = nc.dram_tensor("x1D", [1024, D], F32).ap()
    dbg = {}
    if debug:
        for nm, shp in debug.items():
            dbg[nm] = nc.dram_tensor("dbg_" + nm, shp, F32, kind="ExternalOutput").ap()

    p = Prog(nc)
    final = []

    def finish():
        p.emit(final_tokens=final)
        return nc

    ARENA_BYTES = 210944
    arena_cm = nc.sbuf_tensor("arena", [128, ARENA_BYTES // 2], BF16)
    arena_t = arena_cm.__enter__()
    astate = dict(off=0, peak=0)

    def T(es, name, shape, dt):
        n = 1
        for s_ in shape[1:]:
            n *= s_
        nb = n * (4 if dt == F32 else 2)
        off = (astate["off"] + 63) // 64 * 64
        assert off + nb <= ARENA_BYTES, ("SBUF arena overflow", name, off + nb)
        astate["off"] = off + nb
        astate["peak"] = max(astate["peak"], off + nb)
        ap = arena_t[:, off // 2:(off + nb) // 2]
        if dt == F32:
            ap = ap.bitcast(F32)
        if len(shape) == 3:
            ap = ap.rearrange("p (a b) -> p a b", a=shape[1])
        elif len(shape) == 4:
            ap = ap.rearrange("p (a b c) -> p a b c", a=shape[1], b=shape[2])
        return ap

    def scope():
        es = ExitStack()
        m = astate["off"]

        def rel():
            astate["off"] = m
        es.callback(rel)
        return es

    def bank(b, n=1):
        return ["pb%d" % i for i in range(b, b + n)]

    with ExitStack() as G:
        ps = G.enter_context(nc.psum_tensor("ps", [128, 8, 512], F32))
        psf = ps[:].rearrange("p a b -> p (a b)")
        psb = ps[:].bitcast(BF16).rearrange("p a b -> p (a b)")
        ring = [T(G, "ring%d" % i, [128, 16, 512], BF16) for i in range(NRING)]
        st = T(G, "st", [128, 256], F32)
        cols = T(G, "cols", [128, 96], F32)
        identb = T(G, "identb", [128, 128], BF16)
        p.op("dve", lambda e: e.memset(st[:], 0.0), writes=["st"])
        p.dma("sp", cols[:, 0:32], vcols[:, :], writes=["cols_g"])
        p.dma("pool", identb[:], I4_d[:, 0:128], writes=["identb"])

        sched = []
        for cg in range(8):
            sched.append(("ada", w_ada[:, cg * 512:(cg + 1) * 512], 512))
        sched.append(("inA", w_in[:, 2048:2560], 512))
        sched.append(("inB", w_in[:, 4608:4752], 144))
        for i in range(2):
            sched.append(("q", w_in[:, 1024 + 512 * i:1536 + 512 * i], 512))
        for i in range(4):
            sched.append(("iq", w_in[:, 2560 + 512 * i:3072 + 512 * i], 512))
        for i in range(2):
            sched.append(("pool", w_in[:, 512 * i:512 * (i + 1)], 512))
        for cg in range(8, 24):
            sched.append(("ada", w_ada[:, cg * 512:(cg + 1) * 512], 512))
        for cg in range(4):
            sched.append(("wout", w_out[:, cg * 512:(cg + 1) * 512], 512))
        for fg in range(16):
            sched.append(("ff1", w_ff1[:, fg * 512:(fg + 1) * 512], 512))
        for cg in range(4):
            for jg in range(4):
                sched.append(("ff2", w_ff2[jg * 2048:(jg + 1) * 2048, cg * 512:(cg + 1) * 512], 512))
        rstate = dict(loaded=0, used=0)

        def ring_load():
            i = rstate["loaded"]
            if i >= len(sched):
                return
            name, src, ncol = sched[i]
            buf = ring[i % NRING]
            p.dma("pool", buf[:, :, 0:ncol], src.rearrange("(k p) c -> p k c", p=128),
                  writes=[("ring", i % NRING)])
            rstate["loaded"] += 1

        def ring_get(name):
            i = rstate["used"]
            assert sched[i][0] == name, (sched[i][0], name)
            while rstate["loaded"] <= i:
                ring_load()
            return ring[i % NRING], ("ring", i % NRING)

        def ring_done():
            rstate["used"] += 1
            ring_load()

        for _ in range(NRING):
            ring_load()

        def mm(out, lhsT, rhs, start, stop, reads, writes, signal, sgc=False):
            if sgc:
                p.op("pe", lambda e: e.matmul(out, lhsT=lhsT, rhs=rhs, start=start, stop=stop,
                                              skip_group_check=True),
                     reads=reads, writes=writes, signal=signal)
            else:
                p.op("pe", lambda e: e.matmul(out, lhsT=lhsT, rhs=rhs, start=start, stop=stop),
                     reads=reads, writes=writes, signal=signal)

        def rstd_batch(ss_ap, out_ap, n, key_in, key_out, tmp_ap):
            p.op("dve", lambda e: e.tensor_scalar(out=tmp_ap, in0=ss_ap, scalar1=1.0 / n, scalar2=EPS,
                                                   op0=ALU.mult, op1=ALU.add),
                 reads=(key_in if isinstance(key_in, list) else [key_in]), writes=[key_out + "_t"])
            p.op("act", lambda e: e.activation(out=tmp_ap, in_=tmp_ap, func=AF.Sqrt),
                 reads=[key_out + "_t"], writes=[key_out + "_t"])
            p.op("dve", lambda e: e.reciprocal(out=out_ap, in_=tmp_ap),
                 reads=[key_out + "_t"], writes=[key_out])

        def norm_transpose_pair(srcs, xt, xnb, dst, gcol, scol, tagbase, pbase, rstd_known=None):
            n = len(srcs)
            pv = psb[:, pbase * 1024:(pbase + 4) * 1024].rearrange("p (k t) -> p k t", k=16)
            for i, (kind, src, sc) in enumerate(srcs):
                if kind == "dram":
                    xa = xt[i]
                    xkey = ("xt", i)
                    p.dma("sp", xa[:], src, writes=[xkey])
                    xin = xa[:]
                else:
                    xin, xkey = src
                nb = xnb[i]
                nkey = ("xnb", i)
                if rstd_known is None:
                    p.op("act", lambda e, xin=xin, nb=nb, sc=sc: e.activation(
                        out=nb[:], in_=xin, func=AF.Square, accum_out=st[:, sc:sc + 1]),
                        reads=[xkey], writes=[nkey, ("st", sc)])
                    rstd_batch(st[:, sc:sc + 1], st[:, sc + 32:sc + 33], D, ("st", sc), "rs%s%d" % (tagbase, sc),
                               st[:, sc + 64:sc + 65])
                    rkey = "rs%s%d" % (tagbase, sc)
                    rap = st[:, sc + 32:sc + 33]
                else:
                    rap, rkey = rstd_known[i]
                p.op("dve", lambda e, xin=xin, nb=nb, rap=rap: e.tensor_scalar(
                    out=nb[:], in0=xin, scalar1=rap, scalar2=None, op0=ALU.mult),
                    reads=[xkey, rkey], writes=[nkey])
                for k in range(16):
                    p.op("pe", lambda e, k=k, i=i, nb=nb: e.transpose(
                        out=pv[:, k, i * 128:(i + 1) * 128], in_=nb[:, k * 128:(k + 1) * 128], identity=identb[:]),
                        reads=[nkey, "identb"], writes=bank(pbase, 4), signal=(k == 15))
            w = n * 128
            if gcol is None:
                for kb in range(4):
                    o_ = dst[:, 4 * kb:4 * kb + 4, 0:w]
                    i_ = pv[:, 4 * kb:4 * kb + 4, 0:w]
                    if kb % 2 == 0:
                        p.op("act", lambda e, o_=o_, i_=i_: e.activation(out=o_, in_=i_, func=AF.Copy),
                             reads=bank(pbase + kb), writes=[dst_key(dst)])
                    else:
                        p.op("dve", lambda e, o_=o_, i_=i_: e.tensor_copy(out=o_, in_=i_),
                             reads=bank(pbase + kb), writes=[dst_key(dst)])
                return
            for k in range(16):
                if (k // 4) % 2 == 0:
                    p.op("act", lambda e, k=k: e.activation(
                        out=dst[:, k, 0:w], in_=pv[:, k, 0:w], func=AF.Identity,
                        scale=cols[:, gcol + k:gcol + k + 1], bias=cols[:, scol + k:scol + k + 1]),
                        reads=bank(pbase, 4) + ["cols_m"], writes=[dst_key(dst)])
                else:
                    p.op("dve", lambda e, k=k: e.tensor_scalar(
                        out=dst[:, k, 0:w], in0=pv[:, k, 0:w], scalar1=cols[:, gcol + k:gcol + k + 1],
                        scalar2=cols[:, scol + k:scol + k + 1], op0=ALU.mult, op1=ALU.add),
                        reads=bank(pbase, 4) + ["cols_m"], writes=[dst_key(dst)])

        dkeys = {}

        def dst_key(ap):
            return dkeys[id(ap)]

        def dbg_dump(name, ap_sb, key):
            if debug and name in dbg:
                final.append(p.dma("pool", dbg[name], ap_sb, reads=[key]))

        brow = [T(G, "brow%d" % i, [1, 512], F32)[0:1, :] for i in range(1)]
        mrow = [T(G, "mrow%d" % i, [1, 512], F32)[0:1, :] for i in range(1)]
        ctmp = T(G, "ctmp", [128, 32], F32)
        caT = T(G, "caT", [128, 16], BF16)
        p.dma("sp", ctmp[:, 0:16], c_col[:, :], writes=["ccol"])
        p.op("act", lambda e: e.activation(out=ctmp[:, 16:32], in_=ctmp[:, 0:16], func=AF.Exp, scale=-1.0),
             reads=["ccol"], writes=["cexp"])
        p.op("dve", lambda e: e.tensor_scalar(out=ctmp[:, 16:32], in0=ctmp[:, 16:32], scalar1=1.0, scalar2=None,
                                               op0=ALU.add), reads=["cexp"], writes=["cexp"])
        p.op("dve", lambda e: e.reciprocal(out=ctmp[:, 16:32], in_=ctmp[:, 16:32]), reads=["cexp"], writes=["cexp"])
        p.op("dve", lambda e: e.tensor_tensor(out=caT[:], in0=ctmp[:, 0:16], in1=ctmp[:, 16:32], op=ALU.mult),
             reads=["cexp", "ccol"], writes=["caT"])
        ada_n = [0]

        def ada_group(b):
            cg = ada_n[0]
            ada_n[0] += 1
            W, wkey = ring_get("ada")
            i = 0
            p.dma("sp", brow[i], b_ada[0:1, cg * 512:(cg + 1) * 512], writes=[("brow", i)])
            for k in range(16):
                mm(ps[0:1, b, :], caT[:, k:k + 1], W[:, k, :], k == 0, k == 15,
                   [wkey, "caT"], bank(b), k == 15)
            ring_done()
            p.op("dve", lambda e: e.tensor_tensor(out=mrow[i], in0=ps[0:1, b, :], in1=brow[i], op=ALU.add),
                 reads=bank(b) + [("brow", i)], writes=[("mrow", i)])
            p.dma("sp", modD[0:1, cg * 512:(cg + 1) * 512], mrow[i], reads=[("mrow", i)], writes=["modD"])

        def colload(dstc, off, key):
            p.dma("sp", cols[:, dstc:dstc + 16],
                  modD[0, off:off + D].rearrange("(k p) -> p k", p=128),
                  reads=["modD"], writes=[key], allow_slow_non_contiguous=True)

        with scope() as S1:
            big16 = T(S1, "big16", [128, 16, 1024], BF16)
            dkeys[id(big16)] = "big16"
            with scope() as S2:
                KT = T(S2, "KT", [128, 2, S], BF16)
                Vaug = T(S2, "Vaug", [128, 16, 2, 129], BF16)
                kiT = T(S2, "kiT", [128, S], BF16)
                wq = T(S2, "wq", [128, 8, 16], F32)
                with scope() as S4:
                    qT = T(S4, "qT", [128, 8, 8, 128], BF16)
                    u = T(S4, "u", [128, 8, 1024], BF16)
                    uh = T(S4, "uh", [128, 1024], BF16)
                    with scope() as S3:
                        hT_own = T(S3, "hT_own", [128, 16, 1024], BF16)
                        hT_halo = T(S3, "hT_halo", [128, 16, 128], BF16)
                        dkeys[id(hT_own)] = "hT_own"
                        dkeys[id(hT_halo)] = "hT_halo"
                        xt = [T(S3, "xt%d" % i, [128, D], F32) for i in range(2)]
                        xnb = [T(S3, "xnb%d" % i, [128, D], BF16) for i in range(2)]
                        grp = 0
                        for which, src, dstT in (("own", x_own, hT_own), ("oth", x_oth, big16)):
                            for j0 in range(0, 8, 2):
                                srcs = [("dram", src[(j0 + i) * 128:(j0 + i + 1) * 128, :],
                                         (0 if which == "own" else 8) + j0 + i) for i in range(2)]
                                dview = dstT[:, :, j0 * 128:(j0 + 2) * 128]
                                dkeys[id(dview)] = dkeys[id(dstT)]
                                ada_group(4 * ((grp + 1) % 2))
                                norm_transpose_pair(srcs, xt, xnb, dview, None, None, "a", 4 * (grp % 2))
                                grp += 1
                        dview = hT_halo[:, :, :]
                        dkeys[id(dview)] = "hT_halo"
                        norm_transpose_pair([("dram", x_halo[:, :], 16)], xt, xnb, dview, None, None, "a", 4 * (grp % 2))
                        colload(48, 0, "c_sh1")
                        colload(32, D, "c_sc1")
                        p.op("dve", lambda e: e.scalar_tensor_tensor(out=cols[:, 32:48], in0=cols[:, 32:48], scalar=1.0,
                                                                      in1=cols[:, 0:16], op0=ALU.add, op1=ALU.mult),
                             reads=["c_sc1", "cols_g", "c_sh1"], writes=["c_sc1", "cols_m1"])
                        for (tile_, tkey) in ((hT_own, "hT_own"), (big16, "big16"), (hT_halo, "hT_halo")):
                            for k in range(16):
                                p.op("dve", lambda e, tile_=tile_, k=k: e.tensor_scalar(
                                    out=tile_[:, k, :], in0=tile_[:, k, :], scalar1=cols[:, 32 + k:33 + k],
                                    scalar2=cols[:, 48 + k:49 + k], op0=ALU.mult, op1=ALU.add),
                                    reads=[tkey, "cols_m1"], writes=[tkey])
                        if debug:
                            dbg_dump("hT_own", hT_own[:], "hT_own")
                            dbg_dump("hT_oth", big16[:], "big16")
                            dbg_dump("hT_halo", hT_halo[:], "hT_halo")
                        if stage == "p1":
                            return finish()
                        WA, keyA = ring_get("inA")
                        ring_done_A = False
                        pb = [0]

                        def nextbank():
                            b = pb[0]
                            pb[0] = (b + 1) % 8
                            return b

                        p.op("dve", lambda e: e.memset(Vaug[:, :, :, 128:129], 1.0), writes=["Vaug"])
                        KTv = KT[:].rearrange("p g (j two t) -> p g j two t", two=2, t=128)
                        kiTv = kiT[:].rearrange("p (j two t) -> p j two t", two=2, t=128)
                        cpy = [0]

                        def evac_copy(out, in_, reads, writes):
                            cpy[0] += 1
                            if cpy[0] % 2:
                                p.op("act", lambda e: e.activation(out=out, in_=in_, func=AF.Copy),
                                     reads=reads, writes=writes)
                            else:
                                p.op("dve", lambda e: e.tensor_copy(out=out, in_=in_), reads=reads, writes=writes)

                        for par, hsrc, hkey in ((0, hT_own, "hT_own"), (1, big16, "big16")):
                            for tg in range(2):
                                for c in range(2):
                                    b = nextbank()
                                    for k in range(16):
                                        mm(ps[:, b, :], WA[:, k, c * 128:(c + 1) * 128], hsrc[:, k, tg * 512:(tg + 1) * 512],
                                           k == 0, k == 15, [keyA, hkey], bank(b), k == 15)
                                    evac_copy(KTv[:, c, tg * 4:(tg + 1) * 4, par, :],
                                              ps[:, b, :].rearrange("p (j t) -> p j t", t=128), bank(b), ["KT"])
                                for tt in range(4):
                                    jt = tg * 4 + tt
                                    b = nextbank()
                                    for k in range(16):
                                        mm(ps[:, b, 0:256], hsrc[:, k, jt * 128:(jt + 1) * 128], WA[:, k, 256:512],
                                           k == 0, k == 15, [keyA, hkey], bank(b), k == 15)
                                    evac_copy(Vaug[:, 2 * jt + par, :, 0:128],
                                              ps[:, b, 0:256].rearrange("p (g d) -> p g d", d=128), bank(b), ["Vaug"])
                        ring_done()
                        WB, keyB = ring_get("inB")
                        for par, hsrc, hkey in ((0, hT_own, "hT_own"), (1, big16, "big16")):
                            for tg in range(2):
                                b = nextbank()
                                for k in range(16):
                                    mm(ps[:, b, :], WB[:, k, 0:128], hsrc[:, k, tg * 512:(tg + 1) * 512],
                                       k == 0, k == 15, [keyB, hkey], bank(b), k == 15)
                                evac_copy(kiTv[:, tg * 4:(tg + 1) * 4, par, :],
                                          ps[:, b, :].rearrange("p (j t) -> p j t", t=128), bank(b), ["kiT"])
                        for j in range(8):
                            b = nextbank()
                            for k in range(16):
                                mm(ps[:, b, 0:16], hT_own[:, k, j * 128:(j + 1) * 128], WB[:, k, 128:144],
                                   k == 0, k == 15, [keyB, "hT_own"], bank(b), k == 15)
                            p.op("dve", lambda e, b=b, j=j: e.tensor_scalar(
                                out=wq[:, j, :], in0=ps[:, b, 0:16], scalar1=IDX_SCALE, scalar2=None, op0=ALU.mult),
                                reads=bank(b), writes=["wq"])
                        ring_done()
                        if debug:
                            dbg_dump("KT", KT[:], "KT")
                            dbg_dump("kiT", kiT[:], "kiT")
                            dbg_dump("Vaug", Vaug[:], "Vaug")
                            dbg_dump("wq", wq[:], "wq")
                        p.barrier()
                        if stage == "p2a":
                            return finish()
                        for i in range(2):
                            W, wkey = ring_get("q")
                            for hh in range(4):
                                h = 4 * i + hh
                                for tg in range(2):
                                    b = nextbank()
                                    for k in range(16):
                                        mm(ps[:, b, :], W[:, k, hh * 128:(hh + 1) * 128], hT_own[:, k, tg * 512:(tg + 1) * 512],
                                           k == 0, k == 15, [wkey, "hT_own"], bank(b), k == 15)
                                    evac_copy(qT[:, tg * 4:(tg + 1) * 4, h, :],
                                              ps[:, b, :].rearrange("p (j t) -> p j t", t=128), bank(b), ["qT"])
                            ring_done()
                        for i in range(4):
                            W, wkey = ring_get("iq")
                            for hh in range(4):
                                h = 4 * i + hh
                                for tg in range(2):
                                    b = nextbank()
                                    for k in range(16):
                                        mm(ps[:, b, :], W[:, k, hh * 128:(hh + 1) * 128], hT_own[:, k, tg * 512:(tg + 1) * 512],
                                           k == 0, k == 15, [wkey, "hT_own"], bank(b), k == 15)
                                    evac_copy(big16[:, h, tg * 512:(tg + 1) * 512], ps[:, b, :], bank(b),
                                              [("b16", tg * 4 + jj) for jj in range(4)])
                            ring_done()
                        for i in range(2):
                            W, wkey = ring_get("pool")
                            for j in range(9):
                                b = nextbank()
                                for k in range(16):
                                    lhsT = hT_own[:, k, j * 128:(j + 1) * 128] if j < 8 else hT_halo[:, k, :]
                                    mm(ps[:, b, :], lhsT, W[:, k, :], k == 0, k == 15,
                                       [wkey, "hT_own", "hT_halo"], bank(b), k == 15)
                                dst = u[:, j, i * 512:(i + 1) * 512] if j < 8 else uh[:, i * 512:(i + 1) * 512]
                                evac_copy(dst, ps[:, b, :], bank(b), ["u"])
                            ring_done()
                        if debug:
                            dbg_dump("qT", qT[:], "qT")
                            dbg_dump("qiT", big16[:], ("b16", 7))
                            dbg_dump("u", u[:], "u")
                            dbg_dump("uh", uh[:], "u")
                        p.barrier()
                        if stage == "p2b":
                            return finish()
                    with scope() as P3:
                        nkB = T(P3, "nkB", [128, S], F32)
                        qB = T(P3, "qB", [128, 8], F32)
                        pw2 = T(P3, "pw2", [128, 32], F32)
                        Mm = T(P3, "Mm", [128, 2, 4, 128], BF16)
                        Hf = T(P3, "Hf", [128, 8, 4, 16], BF16)
                        I4 = T(P3, "I4", [128, 512], BF16)
                        wps = T(P3, "wps", [128, 4, 2, 256], BF16)
                        gb = T(P3, "gb", [128, 2048], F32)
                        p.dma("sp", nkB[:], nkB_d[:, :], writes=["nkB"])
                        p.dma("sp", qB[:], qB_d[:, :], writes=["qB"])
                        p.dma("sp", pw2[:], pw2_d[:, :], writes=["pw2"])
                        p.dma("pool", Mm[:].rearrange("p a g t -> p (a g t)"), Mm_d[:, :], writes=["Mm"])
                        p.dma("pool", Hf[:].rearrange("p a g t -> p (a g t)"), Hf_d[:, :], writes=["Hf"])
                        p.dma("pool", I4[:], I4_d[:, :], writes=["I4"])
                        p.dma("sp", gb[:, 0:1024], g_pool_out[0:1, :].partition_broadcast(128), writes=["gb0"])
                        p.dma("sp", gb[:, 1024:2048], g_attn_out[0:1, :].partition_broadcast(128), writes=["gb1"])
                        with scope() as P3s:
                            wpf = T(P3s, "wpf", [128, 4, 2, 256], F32)
                            psb_t = T(P3s, "psb_t", [128, 1024], F32)
                            p.dma("sp", wpf[:], w_pool.rearrange("g (cc p) d -> p g cc d", p=128), writes=["wpf"])
                            p.dma("sp", psb_t[:], pool_scale[0:1, :].partition_broadcast(128), writes=["psb_t"])
                            for g in range(4):
                                p.op("dve", lambda e, g=g, wpf=wpf, psb_t=psb_t: e.tensor_tensor(
                                    out=wps[:, g, :, :], in0=wpf[:, g, :, :],
                                    in1=psb_t[:, g * 256:(g + 1) * 256].unsqueeze(1).to_broadcast([128, 2, 256]),
                                    op=ALU.mult), reads=["wpf", "psb_t"], writes=["wps"])
                            p.barrier()
                        scoreS = [[T(P3, "scs%d" % s, [128, 1024], F32), T(P3, "scb%d" % s, [128, S], F32)] for s in range(2)]
                        maskS = [[T(P3, "mks%d" % s, [128, 1024], BF16), T(P3, "mkb%d" % s, [128, S], BF16)] for s in range(2)]
                        bsS = [[T(P3, "bs%d_%d" % (s, i), [128, 64], F32) for i in range(2)] for s in range(2)]
                        cntS = [T(P3, "cnt%d" % s, [128, 32], F32) for s in range(2)]
                        Rb = [T(P3, "Rb%d" % i, [128, 1024], F32) for i in range(2)]
                        PT = [T(P3, "PT%d" % i, [128, 512], BF16) for i in range(2)]
                        attn32 = T(P3, "attn32", [128, 1024], F32)
                        mg = T(P3, "mg", [128, 2048], BF16)
                        pooledT = T(P3, "pooledT", [128, 8, 128], BF16)
                        rcT = T(P3, "rcT", [128, 8], F32)
                        cnt_ci = [0]
                        cnt_ui = [0]

                        def stage_A(j, score, bs, tag):
                            L = 2 * j + 2
                            N = 128 * L
                            skey = ("score", tag)
                            chunks = [(c0, min(1024, N - c0)) for c0 in range(0, N, 1024)]
                            for h in range(16):
                                for (c0, cw) in chunks:
                                    ci = cnt_ci[0]
                                    cnt_ci[0] += 1
                                    bb = 2 * (ci % 2)
                                    rb = Rb[ci % 2]
                                    rkey = ("Rb", ci % 2)
                                    nmm = (cw + 511) // 512
                                    for m in range(nmm):
                                        w = min(512, cw - m * 512)
                                        mm(ps[:, bb + m, 0:w], big16[:, h, j * 128:(j + 1) * 128],
                                           kiT[:, c0 + m * 512:c0 + m * 512 + w], True, True,
                                           [("b16", j), "kiT"], bank(bb + m), m == nmm - 1)
                                    pin = psf[:, bb * 512:bb * 512 + cw]
                                    p.op("act", lambda e, rb=rb, pin=pin, cw=cw: e.activation(
                                        out=rb[:, 0:cw], in_=pin, func=AF.Relu),
                                        reads=bank(bb, nmm), writes=[rkey])
                                    if h == 0:
                                        p.op("dve", lambda e, rb=rb, c0=c0, cw=cw: e.tensor_scalar(
                                            out=score[:, c0:c0 + cw], in0=rb[:, 0:cw], scalar1=wq[:, j, 0:1],
                                            scalar2=None, op0=ALU.mult), reads=[rkey, "wq"], writes=[skey])
                                    else:
                                        p.op("dve", lambda e, rb=rb, c0=c0, cw=cw, h=h: e.scalar_tensor_tensor(
                                            out=score[:, c0:c0 + cw], in0=rb[:, 0:cw], scalar=wq[:, j, h:h + 1],
                                            in1=score[:, c0:c0 + cw], op0=ALU.mult, op1=ALU.add),
                                            reads=[rkey, "wq", skey], writes=[skey])
                                    yield
                            if debug and j == 1:
                                dbg_dump("score1", score[:, 0:512], skey)
                            p.op("dve", lambda e: e.tensor_reduce(out=bs[:, 0:1], in_=score[:, 0:N], axis=AX.X, op=ALU.max),
                                 reads=[skey], writes=[("bsM", tag)])
                            yield
                            p.op("dve", lambda e: e.tensor_reduce(out=bs[:, 1:2], in_=score[:, 0:N], axis=AX.X, op=ALU.min),
                                 reads=[skey], writes=[("bsm", tag)])
                            yield
                            p.op("dve", lambda e: e.scalar_tensor_tensor(
                                out=score[:, 0:N], in0=nkB[:, 0:N], scalar=qB[:, j:j + 1], in1=score[:, 0:N],
                                op0=ALU.add, op1=ALU.min), reads=["nkB", "qB", skey], writes=[skey])
                            yield
                            p.op("dve", lambda e: e.tensor_scalar(out=bs[:, 2:3], in0=bs[:, 1:2], scalar1=-1.0, scalar2=None,
                                                                   op0=ALU.add), reads=[("bsm", tag)], writes=[("bslo", tag)])
                            yield
                            p.op("dve", lambda e: e.tensor_tensor(out=bs[:, 3:4], in0=bs[:, 0:1], in1=bs[:, 2:3], op=ALU.subtract),
                                 reads=[("bsM", tag), ("bslo", tag)], writes=[("bsW", tag)])
                            yield
                            p.op("dve", lambda e: e.tensor_scalar(out=bs[:, 32:64], in0=pw2[:, :], scalar1=bs[:, 3:4], scalar2=None,
                                                                   op0=ALU.mult), reads=[("bsW", tag), "pw2"], writes=[("bswd", tag)])
                            yield
                            p.op("dve", lambda e: e.tensor_tensor(out=bs[:, 4:5], in0=bs[:, 2:3], in1=bs[:, 32:33], op=ALU.add),
                                 reads=[("bslo", tag), ("bswd", tag)], writes=[("bsmid", tag)])
                            yield
                            if tag[1] == 1:
                                p.op("dve", lambda e: e.tensor_scalar(out=bs[:, 7:8], in0=bs[:, 4:5], scalar1=-1.0, scalar2=None,
                                                                       op0=ALU.mult), reads=[("bsmid", tag)], writes=[("bsnmid", tag)])
                                yield
                                p.op("dve", lambda e: e.tensor_scalar(out=bs[:, 32:64], in0=bs[:, 32:64], scalar1=-1.0, scalar2=None,
                                                                       op0=ALU.mult), reads=[("bswd", tag), ("bsmid", tag)],
                                     writes=[("bswd", tag)])
                                yield
                                p.op("dve", lambda e: e.memset(cntS[tag[0]][:], 0.0),
                                     reads=[("bscnt", tag)], writes=[("bscnt", tag)])
                                yield

                        def stage_B_act(j, score, maskb, bs, tag):
                            N = 128 * (2 * j + 2)
                            skey = ("score", tag)
                            mkey = ("maskb", tag)
                            for it in range(NIT):
                                p.op("act", lambda e, it=it: e.activation(
                                    out=maskb[:, 0:N], in_=score[:, 0:N], func=AF.Sign, bias=bs[:, 7:8], scale=1.0,
                                    accum_out=cntS[tag[0]][:, it:it + 1]),
                                    reads=[skey, ("bsnmid", tag), mkey], writes=[mkey, ("bscnt", tag)])
                                last = it == NIT - 1
                                p.op("dve", lambda e, last=last, it=it: e.tensor_scalar(
                                    out=bs[:, 6:7], in0=cntS[tag[0]][:, it:it + 1], scalar1=2.0 * TOPK - N, scalar2=(-1.0 if last else -0.5),
                                    op0=ALU.is_ge, op1=ALU.add), reads=[("bscnt", tag)], writes=[("bsge", tag)])
                                yield
                                p.op("dve", lambda e, it=it: e.scalar_tensor_tensor(
                                    out=bs[:, 7:8], in0=bs[:, 6:7], scalar=bs[:, 32 + it:33 + it], in1=bs[:, 7:8],
                                    op0=ALU.mult, op1=ALU.add),
                                    reads=[("bsge", tag), ("bswd", tag), ("bsnmid", tag)], writes=[("bsnmid", tag)])
                                yield
                            p.op("dve", lambda e: e.tensor_scalar(out=bs[:, 4:5], in0=bs[:, 7:8], scalar1=-1.0, scalar2=None,
                                                                   op0=ALU.mult), reads=[("bsnmid", tag)], writes=[("bsmid", tag)])
                            yield
                            p.op("dve", lambda e: e.tensor_scalar(
                                out=maskb[:, 0:N], in0=score[:, 0:N], scalar1=bs[:, 4:5], scalar2=MASKV,
                                op0=ALU.is_lt, op1=ALU.mult), reads=[skey, ("bsmid", tag), mkey], writes=[mkey])
                            yield

                        def stage_B(j, score, maskb, bs, tag):
                            N = 128 * (2 * j + 2)
                            skey = ("score", tag)
                            mkey = ("maskb", tag)
                            for it in range(NIT):
                                p.op("dve", lambda e: e.tensor_scalar(
                                    out=maskb[:, 0:N], in0=score[:, 0:N], scalar1=bs[:, 4:5], scalar2=None,
                                    op0=ALU.is_ge, op1=ALU.add, accum_out=bs[:, 5:6]),
                                    reads=[skey, ("bsmid", tag), mkey], writes=[mkey, ("bscnt", tag)])
                                yield
                                last = it == NIT - 1
                                p.op("dve", lambda e, last=last: e.tensor_scalar(
                                    out=bs[:, 6:7], in0=bs[:, 5:6], scalar1=TOPK, scalar2=(-1.0 if last else -0.5),
                                    op0=ALU.is_ge, op1=ALU.add), reads=[("bscnt", tag)], writes=[("bsge", tag)])
                                yield
                                p.op("dve", lambda e, it=it: e.scalar_tensor_tensor(
                                    out=bs[:, 4:5], in0=bs[:, 6:7], scalar=bs[:, 32 + it:33 + it], in1=bs[:, 4:5],
                                    op0=ALU.mult, op1=ALU.add),
                                    reads=[("bsge", tag), ("bswd", tag), ("bsmid", tag)], writes=[("bsmid", tag)])
                                yield
                            if debug and j == 1:
                                dbg_dump("tau1", bs[:, 0:8], ("bsmid", tag))
                            p.op("dve", lambda e: e.tensor_scalar(
                                out=maskb[:, 0:N], in0=score[:, 0:N], scalar1=bs[:, 4:5], scalar2=MASKV,
                                op0=ALU.is_lt, op1=ALU.mult), reads=[skey, ("bsmid", tag), mkey], writes=[mkey])
                            yield

                        def oacc(h, n=129):
                            return ps[:, 5 + h // 3, (h % 3) * 129:(h % 3) * 129 + n]

                        def stage_C(j, maskb, tag):
                            L = 2 * j + 2
                            mkey = ("maskb", tag)
                            for kt in range(L):
                                for g in range(2):
                                    mm(ps[:, 4, :], KT[:, g, kt * 128:(kt + 1) * 128],
                                       qT[:, j, 4 * g:4 * g + 4, :], True, False,
                                       ["KT", "qT"], bank(4), False)
                                    mm(ps[:, 4, :], maskb[:, kt * 128:(kt + 1) * 128], I4[:, :], False, True,
                                       [mkey, "I4"], bank(4), True)
                                    ui = cnt_ui[0]
                                    cnt_ui[0] += 1
                                    pt = PT[ui % 2]
                                    pkey = ("PT", ui % 2)
                                    p.op("act", lambda e, pt=pt: e.activation(out=pt[:], in_=ps[:, 4, :], func=AF.Exp,
                                                                              scale=ATT_SCALE),
                                         reads=bank(4), writes=[pkey])
                                    for hh in range(4):
                                        h = 4 * g + hh
                                        lastmm = (kt == L - 1) and (g == 1) and (hh == 3)
                                        mm(oacc(h), pt[:, hh * 128:(hh + 1) * 128], Vaug[:, kt, g, :],
                                           kt == 0 and h % 3 == 0, kt == L - 1, [pkey, "Vaug"], bank(5, 3),
                                           lastmm or hh == 3, sgc=True)
                                    yield
                            for bk in range(3):
                                nh = 3 if bk < 2 else 2
                                den = ps[:, 5 + bk, 0:nh * 129].rearrange("p (h c) -> p h c", c=129)
                                p.op("dve", lambda e, den=den, bk=bk, nh=nh: e.reciprocal(
                                    out=rcT[:, 3 * bk:3 * bk + nh], in_=den[:, :, 128]),
                                    reads=bank(5, 3), writes=[("rc", bk)])
                                yield
                                p.op("dve", lambda e, den=den, bk=bk, nh=nh: e.tensor_tensor(
                                    out=attn32[:, 384 * bk:384 * bk + 128 * nh].rearrange("p (h d) -> p h d", d=128),
                                    in0=den[:, :, 0:128],
                                    in1=rcT[:, 3 * bk:3 * bk + nh].unsqueeze(2).to_broadcast([128, nh, 128]),
                                    op=ALU.mult), reads=bank(5, 3) + [("rc", bk)], writes=["attn32"])
                                yield
                            p.op("act", lambda e: e.activation(out=mg[:, 1024:2048], in_=attn32[:], func=AF.Square,
                                                               accum_out=st[:, 128 + j:129 + j]),
                                 reads=["attn32"], writes=["mg1", ("st", 128 + j)])
                            p.op("dve", lambda e: e.tensor_tensor(out=mg[:, 1024:2048], in0=attn32[:], in1=gb[:, 1024:2048],
                                                                  op=ALU.mult), reads=["attn32", "gb1"], writes=["mg1"])
                            yield
                            if debug and j == 1:
                                dbg_dump("attn1", attn32[:], "attn32")
                            var = 0 if j == 0 else 1
                            psP = psf[:, 2048:3072].rearrange("p (c t) -> p c t", t=128)
                            for cch in range(8):
                                g = cch // 2
                                mm(psP[:, cch, :], u[:, j, cch * 128:(cch + 1) * 128], Mm[:, var, g, :], True, False,
                                   ["u", "Mm"], bank(4, 2), False)
                                mm(psP[:, cch, 0:16], uh[:, cch * 128:(cch + 1) * 128], Hf[:, j, g, :], False, True,
                                   ["u", "Hf"], bank(4, 2), cch == 7)
                            p.op("act", lambda e: e.activation(out=pooledT[:].rearrange("p c t -> p (c t)"), in_=psf[:, 2048:3072],
                                                               func=AF.Copy), reads=bank(4, 2), writes=["pooledT"])
                            for g in range(4):
                                for cc in range(2):
                                    mm(psf[:, 3072 + g * 256:3072 + (g + 1) * 256], pooledT[:, 2 * g + cc, :], wps[:, g, cc, :],
                                       cc == 0, cc == 1, ["pooledT", "wps"], bank(6, 2), (g == 3 and cc == 1))
                            p.op("act", lambda e: e.activation(out=mg[:, 0:1024], in_=psf[:, 3072:4096], func=AF.Square,
                                                               accum_out=st[:, 136 + j:137 + j]),
                                 reads=bank(6, 2), writes=["mg0", ("st", 136 + j)])
                            p.op("dve", lambda e: e.tensor_tensor(out=mg[:, 0:1024], in0=psf[:, 3072:4096], in1=gb[:, 0:1024],
                                                                  op=ALU.mult), reads=bank(6, 2) + ["gb0"], writes=["mg0"])
                            yield
                            pv = psb[:, 4096:6144].rearrange("p (c t) -> p c t", t=128)
                            for c in range(16):
                                p.op("pe", lambda e, c=c: e.transpose(out=pv[:, c, :], in_=mg[:, c * 128:(c + 1) * 128],
                                                                      identity=identb[:]),
                                     reads=["mg0", "mg1", "identb"], writes=bank(4, 2), signal=(c == 15))
                            p.op("act", lambda e: e.activation(out=big16[:, :, j * 128:(j + 1) * 128], in_=pv,
                                                               func=AF.Copy),
                                 reads=bank(4, 2), writes=[("b16", j)])
                            yield

                        def run_rr(gens):
                            gens = [g for g in gens if g is not None]
                            while gens:
                                for g in list(gens):
                                    try:
                                        next(g)
                                    except StopIteration:
                                        gens.remove(g)

                        def chain(*gs):
                            for g in gs:
                                yield from g

                        pairs = [(0, 7), (1, 6), (2, 5), (3, 4)]

                        def bufs(q, i):
                            s = q % 2
                            return scoreS[s][i], maskS[s][i], bsS[s][i], (s, i)

                        def A_pair(q):
                            return chain(*[stage_A(pairs[q][i], bufs(q, i)[0], bufs(q, i)[2], bufs(q, i)[3]) for i in range(2)])

                        def C_pair(q):
                            return chain(*[stage_C(pairs[q][i], bufs(q, i)[1], bufs(q, i)[3]) for i in range(2)])

                        def B_one(q, i):
                            sc, mk, bs_, tag = bufs(q, i)
                            if i == 1:
                                return stage_B_act(pairs[q][i], sc, mk, bs_, tag)
                            return stage_B(pairs[q][i], sc, mk, bs_, tag)

                        def ada_rest(n):
                            for _ in range(n):
                                ci = cnt_ci[0]
                                cnt_ci[0] += 1
                                ada_group(2 * (ci % 2))
                                for _ in range(3):
                                    yield

                        run_rr([A_pair(0), ada_rest(2)])
                        for q in range(4):
                            run_rr([B_one(q, 0), B_one(q, 1),
                                    A_pair(q + 1) if q + 1 < 4 else None,
                                    C_pair(q - 1) if q >= 1 else None,
                                    ada_rest(4 if q < 3 else 2)])
                        run_rr([C_pair(3)])
                        assert ada_n[0] == 24
                        if debug:
                            dbg_dump("mergedT", big16[:], ("b16", 7))
                            dbg_dump("st", st[:], ("st", 143))
                        p.barrier()
                        if stage == "p3":
                            return finish()
            with scope() as P4:
                mix = T(P4, "mix", [128, 8, D], F32)
                gg = T(P4, "gg", [128, D], F32)
                gt = T(P4, "gt", [128, D], F32)
                xt = [T(P4, "xt4_%d" % i, [128, D], F32) for i in range(2)]
                xnb = [T(P4, "xnb4_%d" % i, [128, D], BF16) for i in range(2)]
                colload(80, 3 * D, "c_sh2")
                colload(64, 4 * D, "c_sc2")
                p.op("dve", lambda e: e.scalar_tensor_tensor(out=cols[:, 64:80], in0=cols[:, 64:80], scalar=1.0,
                                                              in1=cols[:, 16:32], op0=ALU.add, op1=ALU.mult),
                     reads=["c_sc2", "cols_g", "c_sh2"], writes=["c_sc2", "cols_m"])
                p.dma("sp", gg[:], modD[0:1, 2 * D:3 * D].partition_broadcast(128), reads=["modD"], writes=["gg"])
                p.dma("sp", gt[:], g_post_mix[0:1, :].partition_broadcast(128), writes=["gt"])
                p.op("dve", lambda e: e.tensor_tensor(out=gg[:], in0=gg[:], in1=gt[:], op=ALU.mult),
                     reads=["gg", "gt"], writes=["gg"])
                rstd_batch(st[:, 128:144], st[:, 144:160], 1024, [("st", 128 + i) for i in range(16)], "rs_pa", st[:, 160:176])
                for cg in range(4):
                    W, wkey = ring_get("wout")
                    for j in range(8):
                        bA = (2 * j) % 8
                        bB = (2 * j + 1) % 8
                        for k in range(8):
                            mm(ps[:, bA, :], big16[:, k, j * 128:(j + 1) * 128], W[:, k, :], k == 0, k == 7,
                               [wkey, ("b16", j)], bank(bA), k == 7)
                        for k in range(8, 16):
                            mm(ps[:, bB, :], big16[:, k, j * 128:(j + 1) * 128], W[:, k, :], k == 8, k == 15,
                               [wkey, ("b16", j)], bank(bB), k == 15)
                        p.op("act", lambda e, j=j, cg=cg, bA=bA: e.activation(
                            out=mix[:, j, cg * 512:(cg + 1) * 512], in_=ps[:, bA, :], func=AF.Identity,
                            scale=st[:, 152 + j:153 + j]), reads=bank(bA) + ["rs_pa"], writes=[("mix", j)])
                        p.op("dve", lambda e, j=j, cg=cg, bB=bB: e.scalar_tensor_tensor(
                            out=mix[:, j, cg * 512:(cg + 1) * 512], in0=ps[:, bB, :], scalar=st[:, 144 + j:145 + j],
                            in1=mix[:, j, cg * 512:(cg + 1) * 512], op0=ALU.mult, op1=ALU.add),
                            reads=bank(bB) + ["rs_pa", ("mix", j)], writes=[("mix", j)])
                    ring_done()
                p.barrier()
                for j in range(8):
                    p.op("act", lambda e, j=j: e.activation(out=xnb[j % 2][:], in_=mix[:, j, :], func=AF.Square,
                                                            accum_out=st[:, 176 + j:177 + j]),
                         reads=[("mix", j)], writes=[("xnb", j % 2), ("st", 176 + j)])
                rstd_batch(st[:, 176:184], st[:, 184:192], D, [("st", 176 + i) for i in range(8)], "rs_m", st[:, 192:200])
                for j in range(8):
                    p.dma("sp", xt[j % 2][:], x_own[j * 128:(j + 1) * 128, :], writes=[("xt", j % 2)])
                    p.op("dve", lambda e, j=j: e.scalar_tensor_tensor(
                        out=mix[:, j, :], in0=mix[:, j, :], scalar=st[:, 184 + j:185 + j], in1=gg[:],
                        op0=ALU.mult, op1=ALU.mult), reads=[("mix", j), "rs_m", "gg"], writes=[("mix", j)])
                    p.op("dve", lambda e, j=j: e.tensor_tensor(out=mix[:, j, :], in0=mix[:, j, :], in1=xt[j % 2][:], op=ALU.add),
                         reads=[("mix", j), ("xt", j % 2)], writes=[("mix", j)])
                    p.dma("sp", x1D[j * 128:(j + 1) * 128, :], mix[:, j, :], reads=[("mix", j)], writes=["x1D"])
                    p.op("act", lambda e, j=j: e.activation(out=xnb[j % 2][:], in_=mix[:, j, :], func=AF.Square,
                                                            accum_out=st[:, 200 + j:201 + j]),
                         reads=[("mix", j)], writes=[("xnb", j % 2), ("st", 200 + j)])
                rstd_batch(st[:, 200:208], st[:, 208:216], D, [("st", 200 + i) for i in range(8)], "rs_2", st[:, 216:224])
                if debug:
                    dbg_dump("x1", mix[:], ("mix", 7))
                for j0 in range(0, 8, 2):
                    srcs = [("sbuf", (mix[:, j0 + i, :], ("mix", j0 + i)), 0) for i in range(2)]
                    dview = big16[:, :, j0 * 128:(j0 + 2) * 128]
                    dkeys[id(dview)] = "h2T"
                    norm_transpose_pair(srcs, None, xnb, dview, 64, 80, "b", 4 * ((j0 // 2) % 2),
                                        rstd_known=[(st[:, 208 + j0 + i:209 + j0 + i], "rs_2") for i in range(2)])
                if debug:
                    dbg_dump("h2T", big16[:], "h2T")
                p.barrier()
                if stage == "p4":
                    return finish()
            with scope() as P5:
                fT = T(P5, "fT", [128, 64, 1024], BF16)
                r32 = [T(P5, "r32_%d" % i, [128, 512], F32) for i in range(2)]
                ei = 0
                for fg in range(16):
                    W, wkey = ring_get("ff1")
                    for fc in range(4):
                        jc = fg * 4 + fc
                        for tg in range(2):
                            b = ei % 4
                            for k in range(16):
                                mm(ps[:, b, :], W[:, k, fc * 128:(fc + 1) * 128], big16[:, k, tg * 512:(tg + 1) * 512],
                                   k == 0, k == 15, [wkey, "h2T"], bank(b), k == 15)
                            r = r32[ei % 2]
                            rkey = ("r32", ei % 2)
                            ei += 1
                            p.op("act", lambda e, r=r, b=b: e.activation(out=r[:], in_=ps[:, b, :], func=AF.Relu),
                                 reads=bank(b), writes=[rkey])
                            p.op("dve", lambda e, r=r, jc=jc, tg=tg: e.tensor_tensor(
                                out=fT[:, jc, tg * 512:(tg + 1) * 512], in0=r[:], in1=r[:], op=ALU.mult),
                                reads=[rkey], writes=["fT"])
                    ring_done()
                if debug:
                    dbg_dump("fT", fT[:, 0:4, :], "fT")
                p.barrier()
                if stage == "p5":
                    return finish()
                fo = big16[:].rearrange("p a b -> p (a b)").rearrange("p (j d) -> p j d", d=D)
                jk = T(P5, "jk", [128, 512], BF16)
                for cg in range(4):
                    for jg in range(4):
                        W, wkey = ring_get("ff2")
                        for jj in range(16):
                            jc = jg * 16 + jj
                            for tt in range(8):
                                mm(ps[:, tt, :], fT[:, jc, tt * 128:(tt + 1) * 128], W[:, jj, :], jc == 0, jc == 63,
                                   [wkey, "fT"], bank(tt), jc == 63 or (jj == 15 and tt == 7))
                        ring_done()
                    for tt in range(8):
                        if tt % 2 == 0:
                            p.op("act", lambda e, tt=tt, cg=cg: e.activation(
                                out=fo[:, tt, cg * 512:(cg + 1) * 512], in_=ps[:, tt, :], func=AF.Copy),
                                reads=bank(tt), writes=[("fo", tt)])
                        else:
                            p.op("dve", lambda e, tt=tt, cg=cg: e.tensor_copy(
                                out=fo[:, tt, cg * 512:(cg + 1) * 512], in_=ps[:, tt, :]),
                                reads=bank(tt), writes=[("fo", tt)])
                if debug:
                    dbg_dump("fo", big16[:], ("fo", 7))
                p.barrier()
                if stage == "p6":
                    return finish()
            with scope() as P7:
                gg7 = T(P7, "gg7", [128, D], F32)
                gt7 = T(P7, "gt7", [128, D], F32)
                NB7 = 4
                xt7 = [T(P7, "xt7_%d" % i, [128, D], F32) for i in range(NB7)]
                ot7 = [T(P7, "ot7_%d" % i, [128, D], F32) for i in range(NB7)]
                jk7 = T(P7, "jk7", [128, D], BF16)
                fo = big16[:].rearrange("p a b -> p (a b)").rearrange("p (j d) -> p j d", d=D)
                p.dma("sp", gg7[:], modD[0:1, 5 * D:6 * D].partition_broadcast(128), reads=["modD"], writes=["gg7"])
                p.dma("sp", gt7[:], g_post_ffn[0:1, :].partition_broadcast(128), writes=["gt7"])
                p.op("dve", lambda e: e.tensor_tensor(out=gg7[:], in0=gg7[:], in1=gt7[:], op=ALU.mult),
                     reads=["gg7", "gt7"], writes=["gg7"])
                for tt in range(NB7):
                    p.dma("sp", xt7[tt][:], x1D[tt * 128:(tt + 1) * 128, :], reads=["x1D"], writes=[("xt7", tt)])
                for tt in range(8):
                    p.op("act", lambda e, tt=tt: e.activation(out=jk7[:], in_=fo[:, tt, :], func=AF.Square,
                                                              accum_out=st[:, 224 + tt:225 + tt]),
                         reads=[("fo", tt)], writes=["jk7", ("st", 224 + tt)])
                rstd_batch(st[:, 224:232], st[:, 72:80], D, [("st", 224 + i) for i in range(8)], "rs_f", st[:, 80:88])
                for tt in range(8):
                    bi = tt % NB7
                    o = ot7[bi]
                    p.op("dve", lambda e, tt=tt, o=o: e.scalar_tensor_tensor(
                        out=o[:], in0=fo[:, tt, :], scalar=st[:, 72 + tt:73 + tt], in1=gg7[:],
                        op0=ALU.mult, op1=ALU.mult), reads=[("fo", tt), "rs_f", "gg7"], writes=[("ot", bi)])
                    p.op("dve", lambda e, o=o, bi=bi: e.tensor_tensor(out=o[:], in0=o[:], in1=xt7[bi][:], op=ALU.add),
                         reads=[("ot", bi), ("xt7", bi)], writes=[("ot", bi)])
                    final.append(p.dma("sp", y[tt * 128:(tt + 1) * 128, :], o[:], reads=[("ot", bi)]))
                    if tt + NB7 < 8:
                        p.dma("sp", xt7[bi][:], x1D[(tt + NB7) * 128:(tt + NB7 + 1) * 128, :], reads=["x1D"],
                              writes=[("xt7", bi)])
                return finish()


def own_blocks(ty):
    return [2 * j + ty if j < 4 else 2 * j + 1 - ty for j in range(8)]


def _pool_mats(first_is_block0):
    wins = (2, 4, 8, 16)
    Mm = np.zeros((128, 2, 4, 128), np.float32)
    for var in range(2):
        blk0 = (var == 0 and first_is_block0)
        for g, w in enumerate(wins):
            for t in range(128):
                cnt = min(t + 1, w) if blk0 else w
                lo = max(t + 1 - w, 0)
                Mm[lo:t + 1, var, g, t] = 1.0 / cnt
                Mm[t, var, g, t] -= 1.0
    Hf = np.zeros((128, 8, 4, 16), np.float32)
    for j in range(8):
        if j == 0 and first_is_block0:
            continue
        for g, w in enumerate(wins):
            for t in range(16):
                for r in range(16):
                    off = r - 16
                    if t - w + 1 <= off:
                        Hf[16 * j + r, j, g, t] = 1.0 / w
    return Mm, Hf


def host_inputs(inputs):
    x = np.asarray(inputs["x"], np.float32)
    c = np.asarray(inputs["c"], np.float32)
    f = lambda k: np.ascontiguousarray(np.asarray(inputs[k], np.float32)[0])
    shared = {
        "w_ada": f("w_ada"), "b_ada": f("b_ada")[None, :],
        "g_post_mix": f("g_post_mix")[None, :], "g_post_ffn": f("g_post_ffn")[None, :],
        "w_in": f("w_in"), "w_pool": f("w_pool"), "pool_scale": f("pool_scale")[None, :],
        "g_pool_out": f("g_pool_out")[None, :], "g_attn_out": f("g_attn_out")[None, :],
        "w_out": f("w_out"), "w_ff1": f("w_ff1"), "w_ff2": f("w_ff2"),
        "I4": np.ascontiguousarray(np.tile(np.eye(128, dtype=np.float32), (1, 4))),
        "pw2": np.ascontiguousarray(np.tile((0.5 ** np.arange(1, 33, dtype=np.float64)).astype(np.float32)[None, :], (128, 1))),
    }
    gpre1 = f("g_pre_mix").reshape(16, 128).T
    gpre2 = f("g_pre_ffn").reshape(16, 128).T
    shared["vcols"] = np.ascontiguousarray(np.concatenate([gpre1, gpre2], axis=1))
    in_maps = []
    for core in range(NCORES):
        b, ty = core // 2, core % 2
        own = own_blocks(ty)
        oth = own_blocks(1 - ty)
        xb = x[b].reshape(16, 128, D)
        m = dict(shared)
        m["x_own"] = np.ascontiguousarray(xb[own].reshape(1024, D))
        m["x_oth"] = np.ascontiguousarray(xb[oth].reshape(1024, D))
        halo = np.zeros((128, D), np.float32)
        for j, blk in enumerate(own):
            if blk > 0:
                halo[16 * j:16 * j + 16] = x[b, blk * 128 - 16:blk * 128]
        m["x_halo"] = halo
        m["c_col"] = np.ascontiguousarray(c[b].reshape(16, 128).T)
        kpos = np.zeros(S, np.float64)
        for j in range(8):
            kpos[(2 * j) * 128:(2 * j + 1) * 128] = own[j] * 128 + np.arange(128)
            kpos[(2 * j + 1) * 128:(2 * j + 2) * 128] = oth[j] * 128 + np.arange(128)
        m["nkB"] = np.ascontiguousarray(np.tile((-kpos * BIG).astype(np.float32)[None, :], (128, 1)))
        qpos = np.array(own, np.float64)[None, :] * 128 + np.arange(128)[:, None]
        m["qB"] = np.ascontiguousarray(((qpos + 0.5) * BIG).astype(np.float32))
        Mm, Hf = _pool_mats(own[0] == 0)
        m["Mm"] = np.ascontiguousarray(Mm.reshape(128, -1))
        m["Hf"] = np.ascontiguousarray(Hf.reshape(128, -1))
        in_maps.append(m)
    return in_maps


_NC_CACHE = {}


def kernel(**inputs):
    in_maps = host_inputs(inputs)
    if "nc" not in _NC_CACHE:
        _NC_CACHE["nc"] = build_program()
    nc = _NC_CACHE["nc"]
    res = run_bass_kernel_spmd(nc, in_maps, core_ids=list(range(NCORES)))
    out = np.zeros((4, 16, 128, D), np.float32)
    for core in range(NCORES):
        b, ty = core // 2, core % 2
        yc = np.asarray(res.results[core]["y"], np.float32).reshape(8, 128, D)
        for j, blk in enumerate(own_blocks(ty)):
            out[b, blk] = yc[j]
    return out.reshape(4, S, D)
```
